# Optimizing a Trainium2 kernel written in Bass

```python
import math
import jax, jax.numpy as jnp
from jax import lax
import numpy as np

D_MODEL = 1024
BATCH = 32
SEQ = 2048
DEPTH = 4

N_MEM = 256
HEAD_DIM = 64
SWA_HEADS = 8
SWA_KV_HEADS = 2
SWA_GROUP = SWA_HEADS // SWA_KV_HEADS
WINDOW = 128
SB_HEADS = 8
SB_BLOCK = 128
HGRN_HEAD_DIM = 128
HGRN_HEADS = D_MODEL // HGRN_HEAD_DIM
HGRN_CHUNK = 32
XA_HEADS = 4
XA_HEAD_DIM = D_MODEL // XA_HEADS
D_FF = 128 * ((8 * D_MODEL // 3 + 127) // 128)
CONV_WIDTH = 3
ROPE_THETA = 10000.0
EPS = 1e-6
N_EVEN = (DEPTH + 1) // 2
N_ODD = DEPTH // 2
SWA_Q_W = SWA_HEADS * HEAD_DIM
SWA_KV_W = SWA_KV_HEADS * HEAD_DIM
SB_W = SB_HEADS * HEAD_DIM
AB_IN = SWA_Q_W + 2 * SWA_KV_W + 3 * SB_W
AB_OUT = SWA_Q_W + SB_W

kernel_name = "hybrid_swa_stickbreak_hgrn2_trunk"


def rms_norm(x, gain):
    x32 = x.astype(jnp.float32)
    y = x32 * lax.rsqrt(jnp.mean(x32 * x32, axis=-1, keepdims=True) + EPS)
    return (y * gain.astype(jnp.float32)).astype(x.dtype)


def rope(x, positions):
    d = x.shape[-1]
    inv_freq = ROPE_THETA ** (-jnp.arange(0, d, 2, dtype=jnp.float32) / d)
    ang = positions.astype(jnp.float32)[..., None] * inv_freq
    cos, sin = jnp.cos(ang)[:, :, None, :], jnp.sin(ang)[:, :, None, :]
    x1, x2 = x[..., : d // 2], x[..., d // 2:]
    return jnp.concatenate([x1 * cos - x2 * sin, x2 * cos + x1 * sin], axis=-1)


def sliding_window_attention(q, k, v, sinks):
    B, S = q.shape[:2]
    nb = S // WINDOW
    qb = q.reshape(B, nb, WINDOW, SWA_KV_HEADS, SWA_GROUP, HEAD_DIM)

    def band(t):
        prev = jnp.pad(t, ((0, 0), (WINDOW, 0), (0, 0), (0, 0)))[:, :S]
        return jnp.concatenate([prev.reshape(B, nb, WINDOW, SWA_KV_HEADS, HEAD_DIM),
                                t.reshape(B, nb, WINDOW, SWA_KV_HEADS, HEAD_DIM)], axis=2)

    kb, vb = band(k), band(v)
    s = jnp.einsum('bnqhgd,bnkhd->bnhgqk', qb, kb) * (HEAD_DIM ** -0.5)
    i = jnp.arange(WINDOW)[:, None]
    j = jnp.arange(2 * WINDOW)[None, :]
    blk = jnp.arange(nb)[:, None, None]
    mask = (j > i) & (j <= i + WINDOW) & (blk * WINDOW - WINDOW + j >= 0)
    s = jnp.where(mask[None, :, None, None], s, -jnp.inf)
    sink = sinks.astype(jnp.float32).reshape(SWA_KV_HEADS, SWA_GROUP)[None, None, :, :, None, None]
    m = jnp.maximum(jnp.max(s, axis=-1, keepdims=True), sink)
    p = jnp.exp(s - m)
    p = p / (jnp.sum(p, axis=-1, keepdims=True) + jnp.exp(sink - m))
    o = jnp.einsum('bnhgqk,bnkhd->bnqhgd', p, vb)
    return o.reshape(B, S, SWA_Q_W)


def stick_breaking_attention(q, k, v):
    B, S = q.shape[:2]
    outs = []
    for n in range(S // SB_BLOCK):
        t0, t1 = n * SB_BLOCK, (n + 1) * SB_BLOCK
        z = jnp.einsum('bqhd,bkhd->bhqk', q[:, t0:t1], k[:, :t1]) * (HEAD_DIM ** -0.5)
        causal = jnp.arange(t1)[None, :] < (t0 + jnp.arange(SB_BLOCK))[:, None]
        log_fail = jnp.where(causal, -jax.nn.softplus(z), 0.0)
        log_after = lax.cumsum(log_fail, axis=3, reverse=True) - log_fail
        a = jnp.where(causal, jnp.exp(jax.nn.log_sigmoid(z) + log_after), 0.0)
        outs.append(jnp.einsum('bhqk,bkhd->bqhd', a, v[:, :t1]))
    return jnp.concatenate(outs, axis=1).reshape(B, S, SB_W)


def hgrn2_chunkwise(q, k, v, log_f):
    B, S, H, dk = q.shape
    dv = v.shape[-1]
    nc = S // HGRN_CHUNK

    def chunks(t):
        return t.reshape(B, nc, HGRN_CHUNK, H, t.shape[-1]).transpose(1, 0, 3, 2, 4)

    qc, kc, vc = chunks(q), chunks(k), chunks(v)
    bc = jnp.cumsum(chunks(log_f), axis=3)
    tril = jnp.tril(jnp.ones((HGRN_CHUNK, HGRN_CHUNK), dtype=bool))

    def step(state, inp):
        qi, ki, vi, bi = inp
        b_last = bi[:, :, -1:, :]
        rel = jnp.where(tril[None, None, :, :, None],
                        bi[:, :, :, None, :] - bi[:, :, None, :, :], -jnp.inf)
        scores = jnp.einsum('bhtd,bhsd,bhtsd->bhts', qi, ki, jnp.exp(rel))
        o = (jnp.einsum('bhts,bhse->bhte', scores, vi)
             + jnp.einsum('bhtd,bhde->bhte', qi * jnp.exp(bi), state))
        state = (jnp.exp(b_last)[:, :, 0, :, None] * state
                 + jnp.einsum('bhsd,bhse->bhde', ki * jnp.exp(b_last - bi), vi))
        return state, o

    s0 = jnp.zeros((B, H, dk, dv), jnp.float32)
    _, o = lax.scan(step, s0, (qc, kc, vc, bc))
    return o.transpose(1, 0, 3, 2, 4).reshape(B, S, H, dv)


def swa_sb_mixer(h, positions, w_in, w_out, q_norm, k_norm, sinks):
    B, S, _ = h.shape
    f32 = jnp.float32
    cuts = (SWA_Q_W, SWA_Q_W + SWA_KV_W, SWA_Q_W + 2 * SWA_KV_W,
            SWA_Q_W + 2 * SWA_KV_W + SB_W, SWA_Q_W + 2 * SWA_KV_W + 2 * SB_W)
    qa, ka, va, qb, kb, vb = jnp.split((h @ w_in).astype(f32), cuts, axis=-1)
    qa = rope(rms_norm(qa.reshape(B, S, SWA_HEADS, HEAD_DIM), q_norm), positions)
    ka = rope(rms_norm(ka.reshape(B, S, SWA_KV_HEADS, HEAD_DIM), k_norm), positions)
    va = va.reshape(B, S, SWA_KV_HEADS, HEAD_DIM)
    out_a = sliding_window_attention(qa, ka, va, sinks)
    heads_b = lambda t: t.reshape(B, S, SB_HEADS, HEAD_DIM)
    out_b = stick_breaking_attention(heads_b(qb), heads_b(kb), heads_b(vb))
    return jnp.concatenate([out_a, out_b], axis=-1).astype(h.dtype) @ w_out


def hgrn2_mixer(h, w_in, w_out, o_norm, lower_bound):
    B, S, _ = h.shape
    f32 = jnp.float32
    q, f_logit, i_in, g = jnp.split((h @ w_in).astype(f32), 4, axis=-1)
    lb = lower_bound.astype(f32)
    log_f = jnp.log(lb + (1.0 - lb) * jax.nn.sigmoid(f_logit))
    k = (1.0 - lb) * jax.nn.sigmoid(-f_logit)
    heads = lambda t: t.reshape(B, S, HGRN_HEADS, HGRN_HEAD_DIM)
    o = hgrn2_chunkwise(heads(q), heads(k), heads(i_in), heads(log_f))
    o = rms_norm(o, o_norm) * jax.nn.silu(heads(g))
    return o.reshape(B, S, HGRN_HEADS * HGRN_HEAD_DIM).astype(h.dtype) @ w_out


def memory_cross_attention(h, mem_n, w_q, w_kv, w_o, q_norm, k_norm):
    B, S, _ = h.shape
    M = mem_n.shape[1]
    f32 = jnp.float32
    q = rms_norm((h @ w_q).astype(f32).reshape(B, S, XA_HEADS, XA_HEAD_DIM), q_norm)
    kv = (mem_n @ w_kv).astype(f32).reshape(B, M, 2, XA_HEADS, XA_HEAD_DIM)
    k = rms_norm(kv[:, :, 0], k_norm)
    v = kv[:, :, 1]
    s = jnp.einsum('bshd,bmhd->bhsm', q, k) * (XA_HEAD_DIM ** -0.5)
    p = jax.nn.softmax(s, axis=-1)
    o = jnp.einsum('bhsm,bmhd->bshd', p, v).reshape(B, S, D_MODEL)
    return o.astype(h.dtype) @ w_o


def conv_ffn(h, w_up, conv_w, conv_b, w_down):
    S = h.shape[1]
    u = h @ w_up
    shift = lambda t, n: jnp.pad(t, ((0, 0), (n, 0), (0, 0)))[:, :S]
    u = sum(conv_w[CONV_WIDTH - 1 - n] * shift(u, n) for n in range(CONV_WIDTH)) + conv_b
    gate, up = jnp.split(u, 2, axis=-1)
    return (jax.nn.silu(gate) * up) @ w_down


def setup_inputs(seed: int = 0) -> dict:
    key = jax.random.key(seed)
    ks = jax.random.split(key, 24)
    f32 = jnp.float32

    def dense(k, shape):
        return jax.random.normal(k, shape, f32) * (shape[-2] ** -0.5)

    def gain(k, shape):
        return 1.0 + 0.02 * jax.random.normal(k, shape, f32)

    x = jax.random.normal(ks[0], (BATCH, SEQ, D_MODEL), f32)
    mem = jax.random.normal(ks[1], (BATCH, N_MEM, D_MODEL), f32)
    positions = (jax.random.randint(ks[2], (BATCH, 1), 0, 1024, dtype=jnp.int32)
                 + jnp.arange(SEQ, dtype=jnp.int32)[None, :])
    return {
        "x": x,
        "mem": mem,
        "positions": positions,
        "norm_mix": gain(ks[3], (DEPTH, D_MODEL)),
        "norm_cross": gain(ks[4], (DEPTH, D_MODEL)),
        "norm_mem": gain(ks[5], (DEPTH, D_MODEL)),
        "norm_ffn": gain(ks[6], (DEPTH, D_MODEL)),
        "ab_w_in": dense(ks[7], (N_EVEN, D_MODEL, AB_IN)),
        "ab_w_out": dense(ks[8], (N_EVEN, AB_OUT, D_MODEL)),
        "swa_q_norm": gain(ks[9], (N_EVEN, HEAD_DIM)),
        "swa_k_norm": gain(ks[10], (N_EVEN, HEAD_DIM)),
        "swa_sinks": 0.5 * jax.random.normal(ks[11], (N_EVEN, SWA_HEADS), f32),
        "hgrn_w_in": dense(ks[12], (N_ODD, D_MODEL, 4 * HGRN_HEADS * HGRN_HEAD_DIM)),
        "hgrn_w_out": dense(ks[13], (N_ODD, HGRN_HEADS * HGRN_HEAD_DIM, D_MODEL)),
        "hgrn_o_norm": gain(ks[14], (N_ODD, HGRN_HEAD_DIM)),
        "hgrn_lb": jax.random.normal(ks[15], (DEPTH, HGRN_HEADS * HGRN_HEAD_DIM), f32),
        "xa_w_q": dense(ks[16], (DEPTH, D_MODEL, D_MODEL)),
        "xa_w_kv": dense(ks[17], (DEPTH, D_MODEL, 2 * D_MODEL)),
        "xa_w_o": dense(ks[18], (DEPTH, D_MODEL, D_MODEL)),
        "xa_q_norm": gain(ks[19], (DEPTH, XA_HEAD_DIM)),
        "xa_k_norm": gain(ks[20], (DEPTH, XA_HEAD_DIM)),
        "ffn_w_up": dense(ks[21], (DEPTH, D_MODEL, 2 * D_FF)),
        "ffn_conv_w": jax.random.normal(ks[22], (DEPTH, CONV_WIDTH, 2 * D_FF), f32) * (CONV_WIDTH ** -0.5),
        "ffn_conv_b": 0.02 * jax.random.normal(jax.random.fold_in(ks[22], 1), (DEPTH, 2 * D_FF), f32),
        "ffn_w_down": dense(ks[23], (DEPTH, D_FF, D_MODEL)),
    }


def reference(x, mem, positions, norm_mix, norm_cross, norm_mem, norm_ffn,
              ab_w_in, ab_w_out, swa_q_norm, swa_k_norm, swa_sinks,
              hgrn_w_in, hgrn_w_out, hgrn_o_norm, hgrn_lb,
              xa_w_q, xa_w_kv, xa_w_o, xa_q_norm, xa_k_norm,
              ffn_w_up, ffn_conv_w, ffn_conv_b, ffn_w_down):
    p_lb = jax.nn.softmax(hgrn_lb.astype(jnp.float32), axis=0)
    lower_bounds = jnp.cumsum(p_lb, axis=0) - p_lb[0]
    for l in range(DEPTH):
        h = rms_norm(x, norm_mix[l])
        if l % 2 == 0:
            e = l // 2
            x = x + swa_sb_mixer(h, positions, ab_w_in[e], ab_w_out[e],
                                 swa_q_norm[e], swa_k_norm[e], swa_sinks[e])
        else:
            o = l // 2
            x = x + hgrn2_mixer(h, hgrn_w_in[o], hgrn_w_out[o], hgrn_o_norm[o], lower_bounds[l])
        mem_n = rms_norm(mem, norm_mem[l])
        x = x + memory_cross_attention(rms_norm(x, norm_cross[l]), mem_n, xa_w_q[l], xa_w_kv[l],
                                       xa_w_o[l], xa_q_norm[l], xa_k_norm[l])
        x = x + conv_ffn(rms_norm(x, norm_ffn[l]), ffn_w_up[l], ffn_conv_w[l], ffn_conv_b[l], ffn_w_down[l])
    return x
```

```python
import contextlib
import numpy as np
import concourse.bass as bass
import concourse.mybir as mybir
from concourse.bass_utils import run_bass_kernel_spmd

F32 = mybir.dt.float32
BF16 = mybir.dt.bfloat16
I32 = mybir.dt.int32
AF = mybir.ActivationFunctionType
ALU = mybir.AluOpType
AX = mybir.AxisListType

D = 1024
NCH = 8
DFF = 2816
NFC = 22
EPS = 1e-6
N_MEM = 256
TWO_PI = 6.283185307179586
CW1 = 6.28125
CW2 = TWO_PI - 6.28125
MAGIC = 12582912.0
PI_LO = 3.1415925


class Res:
    __slots__ = ("name", "w", "rd", "dsem", "excl")

    def __init__(self, name="", excl=False):
        self.name = name
        self.w = None
        self.rd = {}
        self.dsem = None
        self.excl = excl


class DSem:
    __slots__ = ("h", "cnt", "key")

    def __init__(self, h, key):
        self.h = h
        self.cnt = 0
        self.key = key


class K:
    def __init__(self, nc, es):
        self.nc = nc
        self.es = es
        self.eng = {"pe": nc.tensor, "act": nc.scalar, "dve": nc.vector, "pool": nc.gpsimd, "sp": nc.sync}
        self.sem = {}
        self.semobj = {}
        for k in ("pe", "act", "dve", "pool"):
            h = es.enter_context(nc.semaphore("s_" + k))
            self.sem[k] = h
            self.semobj[k] = h
        self.cnt = {k: 0 for k in self.eng}
        self.seen = {k: {} for k in self.eng}
        self.free_dsems = {"sp": [], "pool": []}
        self.ndsem = 0
        self.bank_rr = 0
        self.reserved = set()
        self.n_ins = 0

    def _dsem(self, q):
        fl = self.free_dsems[q]
        if fl:
            return fl.pop()
        key = "d%d" % self.ndsem
        self.ndsem += 1
        h = self.es.enter_context(self.nc.semaphore(key))
        ds = DSem(h, key)
        self.semobj[key] = h
        return ds

    def release(self, q, res_list):
        for r in res_list:
            if r.dsem is not None and r.dsem[0] == q:
                self.free_dsems[q].append(r.dsem[1])
                r.dsem = None

    def _waits(self, e, reads, writes):
        raw = {}
        oth = {}
        for r in reads:
            if r.w is not None:
                k, v = r.w
                if raw.get(k, 0) < v:
                    raw[k] = v
            if r.excl:
                for k, v in r.rd.items():
                    if k != e and oth.get(k, 0) < v:
                        oth[k] = v
        for w in writes:
            if w.w is not None:
                k, v = w.w
                if oth.get(k, 0) < v:
                    oth[k] = v
            for k, v in w.rd.items():
                if oth.get(k, 0) < v:
                    oth[k] = v
        deps = dict(raw)
        for k, v in oth.items():
            if k == e and e == "pe":
                continue
            if deps.get(k, 0) < v:
                deps[k] = v
        if e == "pe":
            deps.pop("pe", None)
        eng = self.eng[e]
        seen = self.seen[e]
        for k, v in deps.items():
            if seen.get(k, 0) >= v:
                continue
            eng.wait_ge(self.semobj[k], v)
            seen[k] = v
            self.n_ins += 1

    def op(self, e, fn, reads=(), writes=()):
        self._waits(e, reads, writes)
        ins = fn(self.eng[e])
        self.cnt[e] += 1
        c = self.cnt[e]
        ins.then_inc(self.sem[e], 1)
        self.n_ins += 1
        for r in reads:
            if r.rd.get(e, 0) < c:
                r.rd[e] = c
        for w in writes:
            w.w = (e, c)
            w.rd = {}
        return ins

    def dma(self, q, out, in_, reads=(), writes=(), owner=None):
        self._waits(q, reads, writes)
        if owner is None:
            owner = writes[0] if writes else reads[0]
        if owner.dsem is None or owner.dsem[0] != q:
            assert owner.dsem is None
            owner.dsem = (q, self._dsem(q))
        ds = owner.dsem[1]
        ins = self.eng[q].dma_start(out=out, in_=in_)
        ins.then_inc(ds.h, 16)
        ds.cnt += 16
        self.n_ins += 1
        tok = (ds.key, ds.cnt)
        for r in reads:
            if r.rd.get(ds.key, 0) < ds.cnt:
                r.rd[ds.key] = ds.cnt
        for w in writes:
            w.w = tok
            w.rd = {}
        return ins

    def barrier(self):
        ce = ("pe", "act", "dve", "pool")
        for a in ce + ("sp",):
            for b in ce:
                if a == b:
                    continue
                v = self.cnt[b]
                if v and self.seen[a].get(b, 0) < v:
                    self.eng[a].wait_ge(self.sem[b], v)
                    self.seen[a][b] = v
                    self.n_ins += 1

    def wait_all(self, e, res_list):
        self._waits(e, [], res_list)

    def bank(self, k=1):
        for _ in range(16):
            b = self.bank_rr
            if b % k:
                b += k - (b % k)
            if b + k > 8:
                b = 0
            self.bank_rr = (b + k) % 8
            if not any((b + j) in self.reserved for j in range(k)):
                return b
        raise RuntimeError("no free PSUM bank")


class Phase:
    uid = 0

    def __init__(self, kk):
        self.kk = kk
        self.es = contextlib.ExitStack()
        self.res = []

    def __enter__(self):
        self.es.__enter__()
        return self

    def sb(self, name, shape, dtype):
        Phase.uid += 1
        return self.es.enter_context(self.kk.nc.sbuf_tensor("%s_u%d" % (name, Phase.uid), list(shape), dtype))

    def r(self, name=""):
        x = Res(name)
        self.res.append(x)
        return x

    def rs(self, n, name=""):
        return [self.r(name + str(i)) for i in range(n)]

    def __exit__(self, *a):
        kk = self.kk
        kk.barrier()
        for q in ("sp", "pool"):
            kk.release(q, self.res)
        return self.es.__exit__(*a)


def wblk(w2d, kc0, nkc, c0, ncols):
    return w2d.rearrange("(kc p) n -> p kc n", p=128)[:, kc0:kc0 + nkc, c0:c0 + ncols]


class Prog:
    def __init__(self, S=2048, NSEQ=4, layers=(0, 1, 2, 3), dbg=False, parts=("mix", "xa", "ffn")):
        self.S = S
        self.NT = S // 128
        self.NG = S // 512
        self.NSEQ = NSEQ
        self.layers = layers
        self.dbg = dbg
        self.parts = parts
        self.nc = bass.Bass("TRN2", target_bir_lowering=False)
        self.build()

    def din(self, name, shape, dt=F32):
        return self.nc.dram_tensor(name, list(shape), dt, kind="ExternalInput").ap()

    def build(self):
        nc, S, NT, NSEQ = self.nc, self.S, self.NT, self.NSEQ
        self.x_d = self.din("x", [NSEQ, S, D])
        self.mem_d = self.din("mem", [NSEQ, N_MEM, D])
        self.pos_d = self.din("positions", [NSEQ, S], I32)
        self.norm_mix = self.din("norm_mix", [4, D])
        self.norm_cross = self.din("norm_cross", [4, D])
        self.norm_mem = self.din("norm_mem", [4, D])
        self.norm_ffn = self.din("norm_ffn", [4, D])
        self.ab_w_in = self.din("ab_w_in", [2, D, 2304])
        self.ab_w_out = self.din("ab_w_out", [2, D, D])
        self.swa_q_norm = self.din("swa_q_norm", [2, 64])
        self.swa_k_norm = self.din("swa_k_norm", [2, 64])
        self.swa_sinks = self.din("swa_sinks", [2, 8])
        self.hgrn_w_in = self.din("hgrn_w_in", [2, D, 4096])
        self.hgrn_w_out = self.din("hgrn_w_out", [2, D, D])
        self.hgrn_o_norm = self.din("hgrn_o_norm", [2, 128])
        self.hgrn_lb = self.din("hgrn_lb", [4, D])
        self.xa_w_q = self.din("xa_w_q", [4, D, D])
        self.xa_w_kv = self.din("xa_w_kv", [4, D, 2 * D])
        self.xa_w_o = self.din("xa_w_o", [4, D, D])
        self.xa_q_norm = self.din("xa_q_norm", [4, 256])
        self.xa_k_norm = self.din("xa_k_norm", [4, 256])
        self.ffn_w_up = self.din("ffn_w_up", [4, D, 2 * DFF])
        self.ffn_conv_w = self.din("ffn_conv_w", [4, 3, 2 * DFF])
        self.ffn_conv_b = self.din("ffn_conv_b", [4, 2 * DFF])
        self.ffn_w_down = self.din("ffn_w_down", [4, DFF, D])
        self.c_ident = self.din("c_ident", [128, 128])
        self.c_swamask = self.din("c_swamask", [128, 512])
        self.c_sbmask = self.din("c_sbmask", [128, 128])
        self.c_hmask = self.din("c_hmask", [128, 128])
        self.c_invf = self.din("c_invf", [128, 32])
        self.c_cmask4 = self.din("c_cmask4", [128, 512])
        self.c_rmask4 = self.din("c_rmask4", [128, 512])
        self.out_d = nc.dram_tensor("out", [NSEQ, S, D], F32, kind="ExternalOutput").ap()
        if self.dbg:
            self.dbg_d = nc.dram_tensor("dbg", [16, S, D], F32, kind="ExternalOutput").ap()
            self.ndbg = 0

        with contextlib.ExitStack() as es:
            self.es = es
            kk = self.kk = K(nc, es)
            sb = lambda name, shape, dt: es.enter_context(nc.sbuf_tensor(name, list(shape), dt))
            self.X = sb("X", [128, NT, D], F32)
            self.XR = [Res("X%d" % i) for i in range(NT)]
            self.P = es.enter_context(nc.psum_tensor("P", [128, 8, 512], F32))
            self.PB = [Res("B%d" % i, excl=True) for i in range(8)]
            self.identf = sb("identf", [128, 128], F32)
            self.identb = sb("identb", [128, 128], BF16)
            self.onesb = sb("onesb", [128, 128], BF16)
            self.swamask = sb("swamask", [128, 512], BF16)
            self.sbmask = sb("sbmask", [128, 128], F32)
            self.hmask = sb("hmask", [128, 128], F32)
            self.invf = sb("invf", [128, 32], F32)
            self.lbt = sb("lbt", [128, 32], F32)
            self.omlt = sb("omlt", [128, 32], F32)
            self.nomlt = sb("nomlt", [128, 32], F32)
            self.CR = Res("consts")
            cr = [self.CR]
            kk.dma("sp", self.identf[:], self.c_ident, writes=cr)
            kk.dma("sp", self.sbmask[:], self.c_sbmask, writes=cr)
            kk.dma("sp", self.hmask[:], self.c_hmask, writes=cr)
            kk.dma("sp", self.invf[:], self.c_invf, writes=cr)
            self.CR2 = Res("consts2")
            kk.dma("pool", self.identb[:], self.c_ident, writes=[self.CR2])
            kk.dma("pool", self.swamask[:], self.c_swamask, writes=[self.CR2])
            self.cmask4 = sb("cmask4", [128, 512], BF16)
            self.rmask4 = sb("rmask4", [128, 512], BF16)
            kk.dma("pool", self.cmask4[:], self.c_cmask4, writes=[self.CR2])
            kk.dma("pool", self.rmask4[:], self.c_rmask4, writes=[self.CR2])
            kk.op("dve", lambda e: e.memset(self.onesb[:], 1.0), writes=[self.CR2])
            self.setup_lb()
            for s in range(NSEQ):
                self.run_seq(s)
            kk.wait_all("sp", self.XR)
            kk.barrier()

    def pb(self, b, k=1):
        if k == 1:
            return self.P[:, b, :]
        return self.P[:, b:b + k, :].rearrange("p b f -> p (b f)")

    def pbb(self, b):
        return self.P[:, b, :].bitcast(BF16)

    def setup_lb(self):
        kk = self.kk
        with Phase(kk) as ph:
            raw = ph.sb("lbraw", [32, 128], F32)
            rr = ph.r("lbraw")
            kk.dma("sp", raw[:], self.hgrn_lb.rearrange("l (c p) -> (l c) p", p=128), writes=[rr])
            b = kk.bank()
            kk.op("pe", lambda e: e.transpose(out=self.P[:, b, 0:32], in_=raw[:], identity=self.identf[0:32, 0:32]),
                  reads=[rr, self.CR], writes=[self.PB[b]])
            xs = ph.sb("lbx", [128, 32], F32)
            r2 = ph.r("lbx")
            kk.op("act", lambda e: e.copy(out=xs[:], in_=self.P[:, b, 0:32]), reads=[self.PB[b]], writes=[r2])
            mx = ph.sb("lbmx", [128, 8], F32)
            kk.op("dve", lambda e: e.tensor_max(out=mx[:], in0=xs[:, 0:8], in1=xs[:, 8:16]), reads=[r2], writes=[r2])
            kk.op("dve", lambda e: e.tensor_max(out=mx[:], in0=mx[:], in1=xs[:, 16:24]), reads=[r2], writes=[r2])
            kk.op("dve", lambda e: e.tensor_max(out=mx[:], in0=mx[:], in1=xs[:, 24:32]), reads=[r2], writes=[r2])
            ex = ph.sb("lbex", [128, 32], F32)
            for l in range(4):
                kk.op("dve", lambda e, l=l: e.tensor_sub(out=ex[:, l * 8:(l + 1) * 8], in0=xs[:, l * 8:(l + 1) * 8], in1=mx[:]),
                      reads=[r2], writes=[r2])
            kk.op("act", lambda e: e.activation(out=ex[:], in_=ex[:], func=AF.Exp), reads=[r2], writes=[r2])
            sm = ph.sb("lbsm", [128, 8], F32)
            kk.op("dve", lambda e: e.tensor_add(out=sm[:], in0=ex[:, 0:8], in1=ex[:, 8:16]), reads=[r2], writes=[r2])
            kk.op("dve", lambda e: e.tensor_add(out=sm[:], in0=sm[:], in1=ex[:, 16:24]), reads=[r2], writes=[r2])
            kk.op("dve", lambda e: e.tensor_add(out=sm[:], in0=sm[:], in1=ex[:, 24:32]), reads=[r2], writes=[r2])
            kk.op("dve", lambda e: e.reciprocal(out=sm[:], in_=sm[:]), reads=[r2], writes=[r2])
            for l in range(4):
                kk.op("dve", lambda e, l=l: e.tensor_mul(out=ex[:, l * 8:(l + 1) * 8], in0=ex[:, l * 8:(l + 1) * 8], in1=sm[:]),
                      reads=[r2], writes=[r2])
            lr = self.LBR = Res("lb")
            kk.op("dve", lambda e: e.memset(self.lbt[:, 0:8], 0.0), reads=[r2], writes=[lr])
            kk.op("dve", lambda e: e.tensor_copy(out=self.lbt[:, 8:16], in_=ex[:, 8:16]), reads=[r2, lr], writes=[lr])
            kk.op("dve", lambda e: e.tensor_add(out=self.lbt[:, 16:24], in0=self.lbt[:, 8:16], in1=ex[:, 16:24]), reads=[r2, lr], writes=[lr])
            kk.op("dve", lambda e: e.tensor_add(out=self.lbt[:, 24:32], in0=self.lbt[:, 16:24], in1=ex[:, 24:32]), reads=[r2, lr], writes=[lr])
            kk.op("dve", lambda e: e.tensor_scalar(out=self.omlt[:], in0=self.lbt[:], scalar1=-1.0, scalar2=1.0, op0=ALU.mult, op1=ALU.add),
                  reads=[lr], writes=[lr])
            kk.op("dve", lambda e: e.tensor_scalar(out=self.nomlt[:], in0=self.lbt[:], scalar1=1.0, scalar2=-1.0, op0=ALU.mult, op1=ALU.add),
                  reads=[lr], writes=[lr])

    def load_gain_bc(self, ph, name, row_ap, n):
        t = ph.sb(name, [128, n], F32)
        r = ph.r(name)
        self.kk.dma("sp", t[:], row_ap.partition_broadcast(128), writes=[r])
        return t, r

    def rstd_from_ss(self, ss_ap, rstd_ap, n, res):
        kk = self.kk
        kk.op("act", lambda e: e.activation(out=rstd_ap, in_=ss_ap, func=AF.Ln, bias=self.epsb[:, 0:1], scale=1.0 / n),
              reads=[res, self.CR3], writes=[res])
        kk.op("act", lambda e: e.activation(out=rstd_ap, in_=rstd_ap, func=AF.Exp, scale=-0.5), reads=[res], writes=[res])

    def norm_T(self, ph, tag, srcs, gain, gain_r, dstT, dst_rs, col0=0):
        kk = self.kk
        n = len(srcs)
        ss = ph.sb(tag + "ss", [128, n], F32)
        ssr = ph.r(tag + "ss")
        junk = ph.sb(tag + "junk", [128, D], BF16)
        jr = ph.r(tag + "junk")
        hn = [ph.sb(tag + "hn%d" % j, [128, D], BF16) for j in range(2)]
        hnr = ph.rs(2, tag + "hn")
        for i, (ap, r) in enumerate(srcs):
            kk.op("act", lambda e, ap=ap, i=i: e.activation(out=junk[:], in_=ap, func=AF.Square, accum_out=ss[:, i:i + 1]),
                  reads=[r], writes=[jr, ssr])
        self.rstd_from_ss(ss[:], ss[:], D, ssr)
        for i, (ap, r) in enumerate(srcs):
            h, hr = hn[i % 2], hnr[i % 2]
            kk.op("dve", lambda e, ap=ap, i=i, h=h: e.scalar_tensor_tensor(out=h[:], in0=ap, scalar=ss[:, i:i + 1], in1=gain[:],
                                                                         op0=ALU.mult, op1=ALU.mult),
                  reads=[r, ssr, gain_r], writes=[hr])
            b = kk.bank()
            pv = self.pbb(b)
            for c in range(NCH):
                kk.op("pe", lambda e, c=c, h=h, pv=pv: e.transpose(out=pv[:, c * 128:(c + 1) * 128], in_=h[:, c * 128:(c + 1) * 128],
                                                                  identity=self.identb[:]),
                      reads=[hr, self.CR2], writes=[self.PB[b]])
            kk.op("act", lambda e, i=i, pv=pv: e.copy(out=dstT[:, :, col0 + i * 128: col0 + (i + 1) * 128],
                                                      in_=pv.rearrange("p (c t) -> p c t", c=NCH)),
                  reads=[self.PB[b]], writes=[dst_rs[i]])

    def resid_add(self, i, b, hf):
        kk = self.kk
        xs = self.X[:, i, hf * 512:(hf + 1) * 512]
        kk.op("dve", lambda e: e.tensor_tensor(out=xs, in0=xs, in1=self.P[:, b, :], op=ALU.add),
              reads=[self.PB[b], self.XR[i]], writes=[self.XR[i]])

    def dump(self):
        if not self.dbg:
            return
        k = self.ndbg
        self.ndbg += 1
        for i in range(self.NT):
            self.kk.dma("sp", self.dbg_d[k, i * 128:(i + 1) * 128, :], self.X[:, i, :], reads=[self.XR[i]], owner=self.XR[i])

    def run_seq(self, s):
        kk, NT = self.kk, self.NT
        if s == 0:
            self.epsb = self.es.enter_context(self.nc.sbuf_tensor("epsb", [128, 1], F32))
            self.CR3 = Res("eps")
            kk.op("dve", lambda e: e.memset(self.epsb[:], EPS), writes=[self.CR3])
        for i in range(NT):
            kk.dma("sp", self.X[:, i, :], self.x_d[s, i * 128:(i + 1) * 128, :], writes=[self.XR[i]])
        for l in self.layers:
            if "mix" in self.parts:
                if l % 2 == 0:
                    self.even_mixer(s, l)
                else:
                    self.odd_mixer(s, l)
                self.dump()
            if "xa" in self.parts:
                self.xattn(s, l)
                self.dump()
            if "ffn" in self.parts:
                self.ffn(s, l)
                self.dump()
        for i in range(NT):
            kk.dma("sp", self.out_d[s, i * 128:(i + 1) * 128, :], self.X[:, i, :], reads=[self.XR[i]], owner=self.XR[i])

    def ffn(self, s, l):
        kk, S, NT, NG = self.kk, self.S, self.NT, self.NG
        with Phase(kk) as ph:
            gain, gr = self.load_gain_bc(ph, "fg", self.norm_ffn[l], D)
            hT = ph.sb("fhT", [128, NCH, S], BF16)
            hTr = ph.rs(NT, "fhT")
            self.norm_T(ph, "fn", [(self.X[:, i, :], self.XR[i]) for i in range(NT)], gain, gr, hT, hTr)
            cst = ph.sb("cst", [128, 3, 128], F32)
            cstr = ph.r("cst")
            cw = self.ffn_conv_w[l].rearrange("t (c p) -> (t c) p", p=128)
            cb = self.ffn_conv_b[l].rearrange("(c p) -> c p", p=128)
            kk.dma("sp", cst[:, 0, :], cw[0:128, :], writes=[cstr])
            kk.dma("sp", cst[0:4, 1, :], cw[128:132, :], writes=[cstr])
            kk.dma("sp", cst[0:44, 2, :], cb, writes=[cstr])
            cwb = ph.sb("cwb", [128, 176], F32)
            cwr = ph.r("cwb")
            b = kk.bank()
            kk.op("pe", lambda e: e.transpose(out=self.P[:, b, 0:128], in_=cst[:, 0, :], identity=self.identf[:]),
                  reads=[cstr, self.CR], writes=[self.PB[b]])
            kk.op("pe", lambda e: e.transpose(out=self.P[:, b, 128:132], in_=cst[0:4, 1, :], identity=self.identf[0:4, 0:4]),
                  reads=[cstr, self.CR], writes=[self.PB[b]])
            kk.op("pe", lambda e: e.transpose(out=self.P[:, b, 132:176], in_=cst[0:44, 2, :], identity=self.identf[0:44, 0:44]),
                  reads=[cstr, self.CR], writes=[self.PB[b]])
            kk.op("act", lambda e: e.copy(out=cwb[:], in_=self.P[:, b, 0:176]), reads=[self.PB[b]], writes=[cwr])

            NQ = 4
            qchunks = [list(range(0, 6)), list(range(6, 11)), list(range(11, 17)), list(range(17, 22))]
            yT = ph.sb("yT", [128, 6, S], BF16)
            yTr = [ph.rs(NG, "yT%d_" % j) for j in range(6)]
            NWB = 3
            wup = [ph.sb("wup%d" % j, [128, NCH, 256], BF16) for j in range(NWB)]
            wupr = ph.rs(NWB, "wup")
            wdn = [ph.sb("wdn%d" % j, [128, 6, D], BF16) for j in range(2)]
            wdnr = ph.rs(2, "wdn")
            NU = 3
            U = [ph.sb("U%d" % j, [128, 2, 514], F32) for j in range(NU)]
            Ur = ph.rs(NU, "U")
            TG = [ph.sb("TG%d" % j, [128, 2, 512], F32) for j in range(2)]
            TGr = ph.rs(2, "TG")
            SG = [ph.sb("SG%d" % j, [128, 512], F32) for j in range(2)]
            SGr = ph.rs(2, "SG")
            wup_d = self.ffn_w_up[l]
            wdn_d = self.ffn_w_down[l]
            ui = 0
            wi = 0
            ti = 0
            for qi, chunks in enumerate(qchunks):
                nq = len(chunks)
                wd, wdr = wdn[qi % 2], wdnr[qi % 2]
                kk.dma("pool", wd[:, 0:nq, :], wblk(wdn_d, chunks[0], nq, 0, D), writes=[wdr])
                for ci, c in enumerate(chunks):
                    w, wr = wup[wi % NWB], wupr[wi % NWB]
                    wi += 1
                    kk.dma("pool", w[:, :, 0:128], wblk(wup_d, 0, NCH, c * 128, 128), writes=[wr])
                    kk.dma("pool", w[:, :, 128:256], wblk(wup_d, 0, NCH, DFF + c * 128, 128), writes=[wr])
                    prevU = None
                    for st in range(NG):
                        bg = kk.bank()
                        bu = kk.bank()
                        for (bb, co) in ((bg, 0), (bu, 128)):
                            for kc in range(NCH):
                                kk.op("pe", lambda e, bb=bb, co=co, kc=kc, w=w, st=st: e.matmul(
                                    self.P[:, bb, :], lhsT=w[:, kc, co:co + 128], rhs=hT[:, kc, st * 512:(st + 1) * 512],
                                    start=(kc == 0), stop=(kc == NCH - 1)),
                                    reads=[wr] + hTr[st * 4:(st + 1) * 4], writes=[self.PB[bb]])
                        u, ur = U[ui % NU], Ur[ui % NU]
                        ui += 1
                        kk.op("act", lambda e, u=u, bg=bg: e.copy(out=u[:, 0, 2:514], in_=self.P[:, bg, :]),
                              reads=[self.PB[bg]], writes=[ur])
                        kk.op("act", lambda e, u=u, bu=bu: e.copy(out=u[:, 1, 2:514], in_=self.P[:, bu, :]),
                              reads=[self.PB[bu]], writes=[ur])
                        if prevU is not None:
                            pu, pur = prevU
                            kk.op("act", lambda e, u=u, pu=pu: e.copy(out=u[:, :, 0:2], in_=pu[:, :, 512:514]),
                                  reads=[pur, ur], writes=[ur])
                        else:
                            kk.op("dve", lambda e, u=u: e.memset(u[:, :, 0:2], 0.0), reads=[ur], writes=[ur])
                        prevU = (u, ur)
                        tg, tgr = TG[ti % 2], TGr[ti % 2]
                        sg, sgr = SG[ti % 2], SGr[ti % 2]
                        ti += 1
                        for gi, fc in ((0, c), (1, NFC + c)):
                            w2 = cwb[:, 2 * 44 + fc:2 * 44 + fc + 1]
                            w1 = cwb[:, 1 * 44 + fc:1 * 44 + fc + 1]
                            w0 = cwb[:, 0 * 44 + fc:0 * 44 + fc + 1]
                            bb_ = cwb[:, 3 * 44 + fc:3 * 44 + fc + 1]
                            kk.op("dve", lambda e, u=u, tg=tg, gi=gi, w2=w2, bb_=bb_: e.tensor_scalar(
                                out=tg[:, gi, :], in0=u[:, gi, 2:514], scalar1=w2, scalar2=bb_, op0=ALU.mult, op1=ALU.add),
                                reads=[ur, cwr], writes=[tgr])
                            kk.op("dve", lambda e, u=u, tg=tg, gi=gi, w1=w1: e.scalar_tensor_tensor(
                                out=tg[:, gi, :], in0=u[:, gi, 1:513], scalar=w1, in1=tg[:, gi, :], op0=ALU.mult, op1=ALU.add),
                                reads=[ur, cwr, tgr], writes=[tgr])
                            kk.op("dve", lambda e, u=u, tg=tg, gi=gi, w0=w0: e.scalar_tensor_tensor(
                                out=tg[:, gi, :], in0=u[:, gi, 0:512], scalar=w0, in1=tg[:, gi, :], op0=ALU.mult, op1=ALU.add),
                                reads=[ur, cwr, tgr], writes=[tgr])
                        kk.op("act", lambda e, sg=sg, tg=tg: e.activation(out=sg[:], in_=tg[:, 0, :], func=AF.Silu),
                              reads=[tgr], writes=[sgr])
                        kk.op("dve", lambda e, sg=sg, tg=tg, ci=ci, st=st: e.tensor_tensor(
                            out=yT[:, ci, st * 512:(st + 1) * 512], in0=sg[:], in1=tg[:, 1, :], op=ALU.mult),
                            reads=[sgr, tgr], writes=[yTr[ci][st]])
                for i in range(NT):
                    for hf in range(2):
                        b = kk.bank()
                        for ci in range(nq):
                            kk.op("pe", lambda e, b=b, ci=ci, i=i, hf=hf, wd=wd: e.matmul(
                                self.P[:, b, :], lhsT=yT[:, ci, i * 128:(i + 1) * 128], rhs=wd[:, ci, hf * 512:(hf + 1) * 512],
                                start=(ci == 0), stop=(ci == nq - 1)),
                                reads=[wdr, yTr[ci][i // 4]], writes=[self.PB[b]])
                        self.resid_add(i, b, hf)

    def headnorm(self, ph, tag, bank0, nh, hd, gain, gain_r, out, out_r, extra_reads=()):
        kk = self.kk
        nb = (nh * hd) // 512
        src = self.pb(bank0, nb)
        pbr = [self.PB[bank0 + j] for j in range(nb)]
        ss = ph.sb(tag + "ss", [128, nh], F32)
        ssr = ph.r(tag + "ss")
        junk = ph.sb(tag + "jk", [128, hd], BF16)
        jr = ph.r(tag + "jk")
        for h in range(nh):
            kk.op("act", lambda e, h=h: e.activation(out=junk[:], in_=src[:, h * hd:(h + 1) * hd], func=AF.Square,
                                                     accum_out=ss[:, h:h + 1]),
                  reads=pbr, writes=[jr, ssr])
        self.rstd_from_ss(ss[:], ss[:], hd, ssr)
        for h in range(nh):
            kk.op("dve", lambda e, h=h: e.scalar_tensor_tensor(out=out[:, h * hd:(h + 1) * hd], in0=src[:, h * hd:(h + 1) * hd],
                                                               scalar=ss[:, h:h + 1], in1=gain[:], op0=ALU.mult, op1=ALU.mult),
                  reads=pbr + [ssr, gain_r], writes=[out_r])

    def xattn(self, s, l):
        kk, S, NT, NG = self.kk, self.S, self.NT, self.NG
        with Phase(kk) as ph0:
            kT = ph0.sb("xkT", [128, NCH, N_MEM], BF16)
            kTr = ph0.r("xkT")
            Vt = ph0.sb("xV", [128, 2, D], BF16)
            Vr = ph0.r("xV")
            with Phase(kk) as ph:
                gm, gmr = self.load_gain_bc(ph, "gm", self.norm_mem[l], D)
                gk, gkr = self.load_gain_bc(ph, "gk", self.xa_k_norm[l], 256)
                mem = ph.sb("mem", [128, 2, D], F32)
                memr = ph.rs(2, "mem")
                for mt in range(2):
                    kk.dma("sp", mem[:, mt, :], self.mem_d[s, mt * 128:(mt + 1) * 128, :], writes=[memr[mt]])
                memT = ph.sb("memT", [128, NCH, N_MEM], BF16)
                memTr = ph.rs(2, "memT")
                self.norm_T(ph, "mn", [(mem[:, mt, :], memr[mt]) for mt in range(2)], gm, gmr, memT, memTr)
                wkv = [ph.sb("wkv%d" % j, [128, NCH, 512], BF16) for j in range(4)]
                wkvr = ph.rs(4, "wkv")
                for j in range(4):
                    kk.dma("pool", wkv[j][:], wblk(self.xa_w_kv[l], 0, NCH, j * 512, 512), writes=[wkvr[j]])
                kn = ph.sb("kn", [128, D], BF16)
                knr = ph.r("kn")
                for mt in range(2):
                    b0 = kk.bank(2)
                    for hf in range(2):
                        for kc in range(NCH):
                            kk.op("pe", lambda e, hf=hf, kc=kc, mt=mt, b0=b0: e.matmul(
                                self.P[:, b0 + hf, :], lhsT=memT[:, kc, mt * 128:(mt + 1) * 128], rhs=wkv[hf][:, kc, :],
                                start=(kc == 0), stop=(kc == NCH - 1)),
                                reads=[memTr[mt], wkvr[hf]], writes=[self.PB[b0 + hf]])
                    self.headnorm(ph, "kh%d" % mt, b0, 4, 256, gk, gkr, kn, knr)
                    b = kk.bank()
                    pv = self.pbb(b)
                    for c in range(NCH):
                        kk.op("pe", lambda e, c=c, pv=pv: e.transpose(out=pv[:, c * 128:(c + 1) * 128], in_=kn[:, c * 128:(c + 1) * 128],
                                                                      identity=self.identb[:]),
                              reads=[knr, self.CR2], writes=[self.PB[b]])
                    kk.op("act", lambda e, mt=mt, pv=pv: e.copy(out=kT[:, :, mt * 128:(mt + 1) * 128],
                                                              in_=pv.rearrange("p (c t) -> p c t", c=NCH)),
                          reads=[self.PB[b]], writes=[kTr])
                    b1 = kk.bank(2)
                    for hf in range(2):
                        for kc in range(NCH):
                            kk.op("pe", lambda e, hf=hf, kc=kc, mt=mt, b1=b1: e.matmul(
                                self.P[:, b1 + hf, :], lhsT=memT[:, kc, mt * 128:(mt + 1) * 128], rhs=wkv[2 + hf][:, kc, :],
                                start=(kc == 0), stop=(kc == NCH - 1)),
                                reads=[memTr[mt], wkvr[2 + hf]], writes=[self.PB[b1 + hf]])
                    kk.op("act", lambda e, mt=mt, b1=b1: e.copy(out=Vt[:, mt, :], in_=self.pb(b1, 2)),
                          reads=[self.PB[b1], self.PB[b1 + 1]], writes=[Vr])
            with Phase(kk) as ph:
                gc, gcr = self.load_gain_bc(ph, "gc", self.norm_cross[l], D)
                gq, gqr = self.load_gain_bc(ph, "gq", self.xa_q_norm[l], 256)
                wq = ph.sb("wq", [128, NCH, D], BF16)
                wqr = ph.r("wq")
                wo = ph.sb("wo", [128, NCH, D], BF16)
                wor = ph.r("wo")
                for hf in range(2):
                    kk.dma("pool", wq[:, :, hf * 512:(hf + 1) * 512], wblk(self.xa_w_q[l], 0, NCH, hf * 512, 512), writes=[wqr])
                for hf in range(2):
                    kk.dma("pool", wo[:, :, hf * 512:(hf + 1) * 512], wblk(self.xa_w_o[l], 0, NCH, hf * 512, 512), writes=[wor])
                hTg = [ph.sb("xh%d" % j, [128, NCH, 512], BF16) for j in range(2)]
                hTgr = [ph.rs(4, "xh%d_" % j) for j in range(2)]
                qTg = [ph.sb("xq%d" % j, [128, NCH, 512], BF16) for j in range(2)]
                qTgr = [ph.rs(4, "xq%d_" % j) for j in range(2)]
                oTg = [ph.sb("xo%d" % j, [128, NCH, 512], BF16) for j in range(2)]
                oTgr = [ph.rs(NCH, "xo%d_" % j) for j in range(2)]
                qn = [ph.sb("xqn%d" % j, [128, D], BF16) for j in range(2)]
                qnr = ph.rs(2, "xqn")
                PT = [ph.sb("xPT%d" % j, [128, 2, 512], BF16) for j in range(2)]
                PTr = ph.rs(2, "xPT")
                rden = [ph.sb("xrd%d" % j, [128, 512], F32) for j in range(2)]
                rdr = ph.rs(2, "xrd")
                pi = 0
                for g in range(NG):
                    hT, hTr = hTg[g % 2], hTgr[g % 2]
                    qT, qTr = qTg[g % 2], qTgr[g % 2]
                    oT, oTr = oTg[g % 2], oTgr[g % 2]
                    self.norm_T(ph, "xn%d_" % g, [(self.X[:, g * 4 + j, :], self.XR[g * 4 + j]) for j in range(4)], gc, gcr, hT, hTr)
                    for j in range(4):
                        b0 = kk.bank(2)
                        for hf in range(2):
                            for kc in range(NCH):
                                kk.op("pe", lambda e, hf=hf, kc=kc, j=j, b0=b0, hT=hT: e.matmul(
                                    self.P[:, b0 + hf, :], lhsT=hT[:, kc, j * 128:(j + 1) * 128], rhs=wq[:, kc, hf * 512:(hf + 1) * 512],
                                    start=(kc == 0), stop=(kc == NCH - 1)),
                                    reads=[hTr[j], wqr], writes=[self.PB[b0 + hf]])
                        q_, q_r = qn[j % 2], qnr[j % 2]
                        self.headnorm(ph, "qh%d_%d" % (g, j), b0, 4, 256, gq, gqr, q_, q_r)
                        b = kk.bank()
                        pv = self.pbb(b)
                        for c in range(NCH):
                            kk.op("pe", lambda e, c=c, pv=pv, q_=q_: e.transpose(out=pv[:, c * 128:(c + 1) * 128],
                                                                              in_=q_[:, c * 128:(c + 1) * 128], identity=self.identb[:]),
                                  reads=[q_r, self.CR2], writes=[self.PB[b]])
                        kk.op("act", lambda e, j=j, pv=pv, qT=qT: e.copy(out=qT[:, :, j * 128:(j + 1) * 128],
                                                                      in_=pv.rearrange("p (c t) -> p c t", c=NCH)),
                              reads=[self.PB[b]], writes=[qTr[j]])
                    for h in range(4):
                        pt, ptr = PT[pi % 2], PTr[pi % 2]
                        rd, rdr_ = rden[pi % 2], rdr[pi % 2]
                        pi += 1
                        b0 = kk.bank(2)
                        for mt in range(2):
                            for hh in range(2):
                                kk.op("pe", lambda e, mt=mt, hh=hh, h=h, b0=b0, qT=qT: e.matmul(
                                    self.P[:, b0 + mt, :], lhsT=kT[:, h * 2 + hh, mt * 128:(mt + 1) * 128], rhs=qT[:, h * 2 + hh, :],
                                    start=(hh == 0), stop=(hh == 1)),
                                    reads=[kTr] + qTr, writes=[self.PB[b0 + mt]])
                        kk.op("act", lambda e, b0=b0, pt=pt: e.activation(out=pt[:].rearrange("p a b -> p (a b)"), in_=self.pb(b0, 2),
                                                                        func=AF.Exp, scale=1.0 / 16.0),
                              reads=[self.PB[b0], self.PB[b0 + 1]], writes=[ptr])
                        bd = kk.bank()
                        for mt in range(2):
                            kk.op("pe", lambda e, mt=mt, bd=bd, pt=pt: e.matmul(self.P[:, bd, :], lhsT=self.onesb[:], rhs=pt[:, mt, :],
                                                                             start=(mt == 0), stop=(mt == 1)),
                                  reads=[ptr, self.CR2], writes=[self.PB[bd]])
                        kk.op("dve", lambda e, bd=bd, rd=rd: e.reciprocal(out=rd[:], in_=self.P[:, bd, :]),
                              reads=[self.PB[bd]], writes=[rdr_])
                        for hh in range(2):
                            bo = kk.bank()
                            for mt in range(2):
                                kk.op("pe", lambda e, mt=mt, bo=bo, pt=pt, h=h, hh=hh: e.matmul(
                                    self.P[:, bo, :], lhsT=Vt[:, mt, h * 256 + hh * 128:h * 256 + (hh + 1) * 128], rhs=pt[:, mt, :],
                                    start=(mt == 0), stop=(mt == 1)),
                                    reads=[ptr, Vr], writes=[self.PB[bo]])
                            kk.op("dve", lambda e, bo=bo, rd=rd, oT=oT, h=h, hh=hh: e.tensor_tensor(
                                out=oT[:, h * 2 + hh, :], in0=self.P[:, bo, :], in1=rd[:], op=ALU.mult),
                                reads=[self.PB[bo], rdr_], writes=[oTr[h * 2 + hh]])
                    for j in range(4):
                        i = g * 4 + j
                        for hf in range(2):
                            b = kk.bank()
                            for c in range(NCH):
                                kk.op("pe", lambda e, c=c, b=b, j=j, hf=hf, oT=oT: e.matmul(
                                    self.P[:, b, :], lhsT=oT[:, c, j * 128:(j + 1) * 128], rhs=wo[:, c, hf * 512:(hf + 1) * 512],
                                    start=(c == 0), stop=(c == NCH - 1)),
                                    reads=[oTr[c], wor], writes=[self.PB[b]])
                            self.resid_add(i, b, hf)

    def odd_mixer(self, s, l):
        kk, S, NT, NG = self.kk, self.S, self.NT, self.NG
        o = l // 2
        NCK = NT * 4
        with Phase(kk) as ph:
            gain, gr = self.load_gain_bc(ph, "og", self.norm_mix[l], D)
            go, gor = self.load_gain_bc(ph, "ogo", self.hgrn_o_norm[o], 128)
            hT = ph.sb("ohT", [128, NCH, S], BF16)
            hTr = ph.rs(NT, "ohT")
            self.norm_T(ph, "on", [(self.X[:, i, :], self.XR[i]) for i in range(NT)], gain, gr, hT, hTr)
            rm = ph.sb("orm", [128, 512], BF16)
            rmr = ph.r("orm")
            kk.op("dve", lambda e: e.memset(rm[:], 1.0), writes=[rmr])
            kk.op("dve", lambda e: e.memset(rm[:, 0:512:32], 0.0), writes=[rmr])
            win = [ph.sb("owin%d" % j, [128, NCH, 512], BF16) for j in range(2)]
            winr = ph.rs(2, "owin")
            wout = [ph.sb("owout%d" % j, [128, D], BF16) for j in range(2)]
            woutr = ph.rs(2, "owout")
            qt = ph.sb("oqt", [128, S], BF16)
            qtr = ph.rs(NG, "oqt")
            kt = ph.sb("okt", [128, S], BF16)
            ktr = ph.rs(NG, "okt")
            khT = ph.sb("okhT", [128, NT, 128], BF16)
            khTr = ph.rs(NT, "okhT")
            vtok = ph.sb("ovt", [128, NT, 128], BF16)
            vtr = ph.rs(NT, "ovt")
            sgt = ph.sb("osg", [128, NT, 128], BF16)
            sgr = ph.rs(NT, "osg")
            adec = ph.sb("oadec", [128, NCK], F32)
            adr = ph.rs(NG, "oadec")
            Sbf = ph.sb("oSbf", [128, NCK, 128], BF16)
            Sbfr = ph.rs(NCK, "oSbf")
            Sst = [ph.sb("oSst%d" % j, [128, 128], F32) for j in range(2)]
            Sstr = ph.rs(2, "oSst")
            T1 = [[ph.sb("ot%d_%d" % (a, j), [128, 512], F32) for j in range(2)] for a in range(6)]
            T1r = [ph.rs(2, "ot%d_" % a) for a in range(6)]
            kh = [ph.sb("okh%d" % j, [128, 512], BF16) for j in range(2)]
            khr = ph.rs(2, "okh")
            vm = [ph.sb("ovm%d" % j, [128, 4, 128], BF16) for j in range(2)]
            vmr = ph.rs(2, "ovm")
            qm = [ph.sb("oqm%d" % j, [128, 4, 128], BF16) for j in range(2)]
            qmr = ph.rs(2, "oqm")
            for j in range(2):
                kk.op("dve", lambda e, j=j: e.memset(qm[j][:], 0.0), writes=[qmr[j]])
            scm = [ph.sb("oscm%d" % j, [128, 128], BF16) for j in range(2)]
            scmr = ph.rs(2, "oscm")
            yf = [ph.sb("oyf%d" % j, [128, 128], F32) for j in range(2)]
            yfr = ph.rs(2, "oyf")
            yb = [ph.sb("oyb%d" % j, [128, 128], BF16) for j in range(2)]
            ybr = ph.rs(2, "oyb")
            yT = [ph.sb("oyT%d" % j, [128, 128], BF16) for j in range(2)]
            yTr = ph.rs(2, "oyT")
            oss = ph.sb("oss", [128, 2], F32)
            ossr = ph.rs(2, "oss")
            ojk = ph.sb("ojk", [128, 128], BF16)
            ojr = ph.r("ojk")
            w_in = self.hgrn_w_in[o]
            t1i = 0
            import os
            STG = int(os.environ.get("ODD_STAGE", "9"))
            for hd in range(int(os.environ.get("ODD_HEADS", "8"))):
                if STG < 1:
                    break
                w, wr = win[hd % 2], winr[hd % 2]
                for j in range(4):
                    kk.dma("pool", w[:, :, j * 128:(j + 1) * 128], wblk(w_in, 0, NCH, j * 1024 + hd * 128, 128), writes=[wr])
                wo_, wor_ = wout[hd % 2], woutr[hd % 2]
                kk.dma("pool", wo_[:], self.hgrn_w_out[o][hd * 128:(hd + 1) * 128, :], writes=[wor_])
                col = l * 8 + hd
                lb = self.lbt[:, col:col + 1]
                oml = self.omlt[:, col:col + 1]
                noml = self.nomlt[:, col:col + 1]
                for g in range(NG):
                    tsl = slice(g * 512, (g + 1) * 512)
                    bq = kk.bank()
                    bf = kk.bank()
                    for (bb, co) in ((bq, 0), (bf, 128)):
                        for kc in range(NCH):
                            kk.op("pe", lambda e, bb=bb, co=co, kc=kc, w=w, tsl=tsl: e.matmul(
                                self.P[:, bb, :], lhsT=w[:, kc, co:co + 128], rhs=hT[:, kc, tsl],
                                start=(kc == 0), stop=(kc == NCH - 1)),
                                reads=[wr] + hTr[g * 4:(g + 1) * 4], writes=[self.PB[bb]])
                    p = t1i % 2
                    t1i += 1
                    sig, lf, kf, bb_, eb, enb = [T1[a][p] for a in range(6)]
                    sigr, lfr, kfr, bbr, ebr, enbr = [T1r[a][p] for a in range(6)]
                    kk.op("act", lambda e, sig=sig, bf=bf: e.activation(out=sig[:], in_=self.P[:, bf, :], func=AF.Sigmoid),
                          reads=[self.PB[bf]], writes=[sigr])
                    kk.op("act", lambda e, sig=sig, lf=lf: e.activation(out=lf[:], in_=sig[:], func=AF.Ln, bias=lb, scale=oml),
                          reads=[sigr, self.LBR], writes=[lfr])
                    kk.op("dve", lambda e, sig=sig, kf=kf: e.tensor_scalar(out=kf[:], in0=sig[:], scalar1=noml, scalar2=oml,
                                                                         op0=ALU.mult, op1=ALU.add),
                          reads=[sigr, self.LBR], writes=[kfr])
                    kk.op("dve", lambda e, lf=lf, bb_=bb_: e.tensor_tensor_scan(out=bb_[:], data0=rm[:], data1=lf[:], initial=0.0,
                                                                               op0=ALU.mult, op1=ALU.add),
                          reads=[lfr, rmr], writes=[bbr])
                    kk.op("act", lambda e, bb_=bb_, eb=eb: e.activation(out=eb[:], in_=bb_[:], func=AF.Exp), reads=[bbr], writes=[ebr])
                    kk.op("act", lambda e, bb_=bb_, enb=enb: e.activation(out=enb[:], in_=bb_[:], func=AF.Exp, scale=-1.0),
                          reads=[bbr], writes=[enbr])
                    kk.op("dve", lambda e, eb=eb, bq=bq, tsl=tsl: e.tensor_tensor(out=qt[:, tsl], in0=self.P[:, bq, :], in1=eb[:], op=ALU.mult),
                          reads=[self.PB[bq], ebr], writes=[qtr[g]])
                    kk.op("dve", lambda e, kf=kf, enb=enb, tsl=tsl: e.tensor_tensor(out=kt[:, tsl], in0=kf[:], in1=enb[:], op=ALU.mult),
                          reads=[kfr, enbr], writes=[ktr[g]])
                    kh_, khr_ = kh[p], khr[p]
                    for cc in range(16):
                        kk.op("dve", lambda e, eb=eb, kh_=kh_, cc=cc, g=g: e.tensor_scalar(
                            out=kh_[:, cc * 32:(cc + 1) * 32], in0=kt[:, g * 512 + cc * 32:g * 512 + (cc + 1) * 32],
                            scalar1=eb[:, cc * 32 + 31:cc * 32 + 32], scalar2=None, op0=ALU.mult),
                            reads=[ktr[g], ebr], writes=[khr_])
                    kk.op("act", lambda e, eb=eb, g=g: e.copy(out=adec[:, g * 16:(g + 1) * 16], in_=eb[:, 31:512:32]),
                          reads=[ebr], writes=[adr[g]])
                    b = kk.bank()
                    pv = self.pbb(b)
                    for j in range(4):
                        kk.op("pe", lambda e, j=j, pv=pv, kh_=kh_: e.transpose(out=pv[:, j * 128:(j + 1) * 128],
                                                                            in_=kh_[:, j * 128:(j + 1) * 128], identity=self.identb[:]),
                              reads=[khr_, self.CR2], writes=[self.PB[b]])
                    kk.op("act", lambda e, pv=pv, g=g: e.copy(out=khT[:, g * 4:(g + 1) * 4, :],
                                                              in_=pv[:, 0:512].rearrange("p (c t) -> p c t", c=4)),
                          reads=[self.PB[b]], writes=khTr[g * 4:(g + 1) * 4])
                if STG < 2:
                    continue
                for i in range(NT):
                    b = kk.bank()
                    for kc in range(NCH):
                        kk.op("pe", lambda e, kc=kc, b=b, i=i, w=w: e.matmul(
                            self.P[:, b, 0:256], lhsT=hT[:, kc, i * 128:(i + 1) * 128], rhs=w[:, kc, 256:512],
                            start=(kc == 0), stop=(kc == NCH - 1)),
                            reads=[wr, hTr[i]], writes=[self.PB[b]])
                    ODV = int(os.environ.get("ODD_V", "9"))
                    if ODV < 2:
                        continue
                    if os.environ.get("ODD_VACT"):
                        kk.op("act", lambda e, b=b, i=i: e.copy(out=vtok[:, i, :], in_=self.P[:, b, 0:128]),
                              reads=[self.PB[b]], writes=[vtr[i]])
                    else:
                        kk.op("dve", lambda e, b=b, i=i: e.tensor_scalar(out=vtok[:, i, :], in0=self.P[:, b, 0:128], scalar1=1.0, scalar2=None, op0=ALU.mult),
                              reads=[self.PB[b]], writes=[vtr[i]])
                    if ODV < 3:
                        continue
                    sgs, sgsr = yf[i % 2], yfr[i % 2]
                    kk.op("act", lambda e, b=b, sgs=sgs: e.activation(out=sgs[:], in_=self.P[:, b, 128:256], func=AF.Sigmoid),
                          reads=[self.PB[b]], writes=[sgsr])
                    kk.op("dve", lambda e, b=b, i=i, sgs=sgs: e.tensor_tensor(out=sgt[:, i, :], in0=self.P[:, b, 128:256], in1=sgs[:], op=ALU.mult),
                          reads=[self.PB[b], sgsr], writes=[sgr[i]])
                if STG < 3:
                    continue
                kk.op("dve", lambda e: e.memset(Sst[0][:], 0.0), writes=[Sstr[0]])
                kk.op("dve", lambda e: e.memset(Sbf[:, 0, :], 0.0), writes=[Sbfr[0]])
                for cj in range(NCK - 1):
                    i, j = divmod(cj, 4)
                    if j == 0:
                        vm_, vm_r = vm[i % 2], vmr[i % 2]
                        for jj in range(4):
                            kk.op("dve", lambda e, i=i, vm_=vm_, jj=jj: e.tensor_scalar(
                                out=vm_[:, jj, :], in0=vtok[:, i, :], scalar1=self.rmask4[:, jj * 128:jj * 128 + 1], scalar2=None, op0=ALU.mult),
                                reads=[vtr[i], self.CR2], writes=[vm_r])
                    bd = kk.bank()
                    kk.op("pe", lambda e, bd=bd, i=i, j=j, vm_=vm_: e.matmul(
                        self.P[:, bd, 0:128], lhsT=khT[:, i, :], rhs=vm_[:, j, :], start=True, stop=True),
                        reads=[khTr[i], vm_r], writes=[self.PB[bd]])
                    kk.op("dve", lambda e, bd=bd, j=j, cj=cj: e.scalar_tensor_tensor(
                        out=Sst[(cj + 1) % 2][:], in0=Sst[cj % 2][:], scalar=adec[:, cj:cj + 1], in1=self.P[:, bd, 0:128],
                        op0=ALU.mult, op1=ALU.add),
                        reads=[Sstr[cj % 2], adr[cj // 16], self.PB[bd]], writes=[Sstr[(cj + 1) % 2]])
                    kk.op("act", lambda e, cj=cj: e.copy(out=Sbf[:, cj + 1, :], in_=Sst[(cj + 1) % 2][:]), reads=[Sstr[(cj + 1) % 2]], writes=[Sbfr[cj + 1]])
                if STG < 4:
                    continue
                bobox = {}

                def odd_X(i):
                    p = i % 2
                    bs = kk.bank()
                    kk.op("pe", lambda e, bs=bs, i=i: e.matmul(self.P[:, bs, 0:128], lhsT=kt[:, i * 128:(i + 1) * 128],
                                                              rhs=qt[:, i * 128:(i + 1) * 128], start=True, stop=True),
                          reads=[ktr[i // 4], qtr[i // 4]], writes=[self.PB[bs]])
                    kk.op("dve", lambda e, bs=bs, p=p: e.tensor_tensor(out=scm[p][:], in0=self.P[:, bs, 0:128], in1=self.hmask[:], op=ALU.mult),
                          reads=[self.PB[bs], self.CR], writes=[scmr[p]])
                    bo = kk.bank()
                    kk.op("pe", lambda e, bo=bo, i=i, p=p: e.matmul(self.P[:, bo, 0:128], lhsT=scm[p][:], rhs=vtok[:, i, :],
                                                                 start=True, stop=False),
                          reads=[scmr[p], vtr[i]], writes=[self.PB[bo]])
                    qm_, qm_r = qm[p], qmr[p]
                    for jj in range(4):
                        kk.op("dve", lambda e, i=i, qm_=qm_, jj=jj: e.tensor_copy(
                            out=qm_[:, jj, jj * 32:(jj + 1) * 32], in_=qt[:, i * 128 + jj * 32:i * 128 + (jj + 1) * 32]),
                            reads=[qtr[i // 4]], writes=[qm_r])
                    for j in range(4):
                        kk.op("pe", lambda e, bo=bo, i=i, j=j, qm_=qm_: e.matmul(
                            self.P[:, bo, 0:128], lhsT=qm_[:, j, :], rhs=Sbf[:, 4 * i + j, :], start=False, stop=(j == 3)),
                            reads=[qm_r, Sbfr[4 * i + j]], writes=[self.PB[bo]])
                    bobox[i] = bo
                    kk.reserved.add(bo)

                def odd_Y(i, wo_=wo_, wor_=wor_):
                    p = i % 2
                    bo = bobox.pop(i)
                    kk.op("act", lambda e, bo=bo, p=p: e.activation(out=ojk[:], in_=self.P[:, bo, 0:128], func=AF.Square,
                                                                    accum_out=oss[:, p:p + 1]),
                          reads=[self.PB[bo]], writes=[ojr, ossr[p]])
                    self.rstd_from_ss(oss[:, p:p + 1], oss[:, p:p + 1], 128, ossr[p])
                    kk.op("dve", lambda e, bo=bo, p=p: e.scalar_tensor_tensor(out=yf[p][:], in0=self.P[:, bo, 0:128], scalar=oss[:, p:p + 1],
                                                                             in1=go[:], op0=ALU.mult, op1=ALU.mult),
                          reads=[self.PB[bo], ossr[p], gor], writes=[yfr[p]])
                    kk.reserved.discard(bo)
                    kk.op("dve", lambda e, p=p, i=i: e.tensor_tensor(out=yb[p][:], in0=yf[p][:], in1=sgt[:, i, :], op=ALU.mult),
                          reads=[yfr[p], sgr[i]], writes=[ybr[p]])
                    bt = kk.bank()
                    pv = self.pbb(bt)
                    kk.op("pe", lambda e, pv=pv, p=p: e.transpose(out=pv[:, 0:128], in_=yb[p][:], identity=self.identb[:]),
                          reads=[ybr[p], self.CR2], writes=[self.PB[bt]])
                    kk.op("act", lambda e, pv=pv, p=p: e.copy(out=yT[p][:], in_=pv[:, 0:128]), reads=[self.PB[bt]], writes=[yTr[p]])
                    for hf in range(2):
                        b = kk.bank()
                        kk.op("pe", lambda e, b=b, p=p, hf=hf, wo_=wo_: e.matmul(self.P[:, b, :], lhsT=yT[p][:], rhs=wo_[:, hf * 512:(hf + 1) * 512],
                                                                              start=True, stop=True),
                              reads=[yTr[p], wor_], writes=[self.PB[b]])
                        self.resid_add(i, b, hf)

                odd_X(0)
                for i in range(NT):
                    if i + 1 < NT:
                        odd_X(i + 1)
                    odd_Y(i)

    def rope_tables(self, ph, s):
        kk, NT = self.kk, self.NT
        n = NT * 32
        posi = ph.sb("posi", [128, NT], I32)
        pr = ph.r("pos")
        with self.nc.allow_non_contiguous_dma(reason="positions token-major gather"):
            kk.dma("sp", posi[:], self.pos_d[s].rearrange("(n p) -> p n", p=128), writes=[pr])
        posf = ph.sb("posf", [128, NT], F32)
        kk.op("dve", lambda e: e.tensor_copy(out=posf[:], in_=posi[:]), reads=[pr], writes=[pr])
        ang = ph.sb("ang", [128, NT, 32], F32)
        for t in range(NT):
            kk.op("dve", lambda e, t=t: e.tensor_scalar(out=ang[:, t, :], in0=self.invf[:], scalar1=posf[:, t:t + 1], scalar2=None,
                                                       op0=ALU.mult),
                  reads=[pr, self.CR], writes=[pr])
        a2 = ang[:].rearrange("p a b -> p (a b)")
        kf = ph.sb("ropek", [128, n], F32)
        r_ = ph.sb("roper", [128, n], F32)
        rc = ph.sb("roperc", [128, n], F32)
        m_ = ph.sb("ropem", [128, n], F32)
        kk.op("dve", lambda e: e.tensor_scalar(out=kf[:], in0=a2, scalar1=1.0 / TWO_PI, scalar2=MAGIC, op0=ALU.mult, op1=ALU.add),
              reads=[pr], writes=[pr])
        kk.op("dve", lambda e: e.tensor_scalar(out=kf[:], in0=kf[:], scalar1=-MAGIC, scalar2=None, op0=ALU.add), reads=[pr], writes=[pr])
        kk.op("dve", lambda e: e.scalar_tensor_tensor(out=r_[:], in0=kf[:], scalar=-CW1, in1=a2, op0=ALU.mult, op1=ALU.add),
              reads=[pr], writes=[pr])
        kk.op("dve", lambda e: e.scalar_tensor_tensor(out=r_[:], in0=kf[:], scalar=-CW2, in1=r_[:], op0=ALU.mult, op1=ALU.add),
              reads=[pr], writes=[pr])
        kk.op("dve", lambda e: e.tensor_scalar(out=rc[:], in0=r_[:], scalar1=0.5 * np.pi, scalar2=None, op0=ALU.add), reads=[pr], writes=[pr])
        kk.op("dve", lambda e: e.tensor_scalar(out=m_[:], in0=rc[:], scalar1=float(np.pi), scalar2=None, op0=ALU.is_gt), reads=[pr], writes=[pr])
        kk.op("dve", lambda e: e.scalar_tensor_tensor(out=rc[:], in0=m_[:], scalar=-TWO_PI, in1=rc[:], op0=ALU.mult, op1=ALU.add),
              reads=[pr], writes=[pr])
        for t_ in (r_, rc):
            kk.op("dve", lambda e, t_=t_: e.tensor_scalar(out=t_[:], in0=t_[:], scalar1=-PI_LO, scalar2=PI_LO, op0=ALU.max, op1=ALU.min),
                  reads=[pr], writes=[pr])
        cos3 = ph.sb("cos3", [128, NT, 3, 32], F32)
        sin3 = ph.sb("sin3", [128, NT, 3, 32], F32)
        tr = ph.r("ropetab")
        kk.op("act", lambda e: e.activation(out=sin3[:, :, 0, :], in_=r_[:].rearrange("p (a b) -> p a b", b=32), func=AF.Sin),
              reads=[pr], writes=[tr])
        kk.op("act", lambda e: e.activation(out=cos3[:, :, 0, :], in_=rc[:].rearrange("p (a b) -> p a b", b=32), func=AF.Sin),
              reads=[pr], writes=[tr])
        for j in (1, 2):
            kk.op("dve", lambda e, j=j: e.tensor_copy(out=sin3[:, :, j, :], in_=sin3[:, :, 0, :]), reads=[tr], writes=[tr])
            kk.op("dve", lambda e, j=j: e.tensor_copy(out=cos3[:, :, j, :], in_=cos3[:, :, 0, :]), reads=[tr], writes=[tr])
        return cos3, sin3, tr

    def outproj_partial(self, i, aT_ap, aT_r, wo_, wor_):
        kk = self.kk
        for hf in range(2):
            b = kk.bank()
            kk.op("pe", lambda e, b=b, hf=hf: e.matmul(self.P[:, b, :], lhsT=aT_ap, rhs=wo_[:, hf * 512:(hf + 1) * 512], start=True, stop=True),
                  reads=[aT_r, wor_], writes=[self.PB[b]])
            self.resid_add(i, b, hf)

    def even_mixer(self, s, l):
        kk, S, NT, NG = self.kk, self.S, self.NT, self.NG
        ei = l // 2
        w_in = self.ab_w_in[ei]
        w_out = self.ab_w_out[ei]
        with Phase(kk) as ph:
            hT = ph.sb("ehT", [128, NCH, S], BF16)
            hTr = ph.rs(NT, "ehT")
            with Phase(kk) as pn:
                gain, gr = self.load_gain_bc(pn, "eg", self.norm_mix[l], D)
                self.norm_T(pn, "en", [(self.X[:, i, :], self.XR[i]) for i in range(NT)], gain, gr, hT, hTr)
            wout = [ph.sb("ewout%d" % j, [128, D], BF16) for j in range(2)]
            woutr = ph.rs(2, "ewout")
            Oc = [ph.sb("eOc%d" % j, [128, 128], BF16) for j in range(2)]
            Ocr = ph.rs(2, "eOc")
            aT = [ph.sb("eaT%d" % j, [128, 128], BF16) for j in range(2)]
            aTr = ph.rs(2, "eaT")
            oi = 0
            with Phase(kk) as pa:
                cos3, sin3, tabr = self.rope_tables(pa, s)
                g3 = pa.sb("g3", [128, 3, 64], F32)
                g3r = pa.r("g3")
                kk.dma("sp", g3[:, 0, :], self.swa_q_norm[ei].partition_broadcast(128), writes=[g3r])
                kk.dma("sp", g3[:, 1, :], self.swa_q_norm[ei].partition_broadcast(128), writes=[g3r])
                kk.dma("sp", g3[:, 2, :], self.swa_k_norm[ei].partition_broadcast(128), writes=[g3r])
                esk = pa.sb("esk", [128, 8], F32)
                eskr = pa.r("esk")
                kk.dma("sp", esk[:], self.swa_sinks[ei].partition_broadcast(128), writes=[eskr])
                kk.op("act", lambda e: e.activation(out=esk[:], in_=esk[:], func=AF.Exp), reads=[eskr], writes=[eskr])
                win = [pa.sb("ewa%d" % j, [128, NCH, 256], BF16) for j in range(2)]
                winr = pa.rs(2, "ewa")
                raw = [pa.sb("eraw%d" % j, [128, 256], F32) for j in range(2)]
                rawr = pa.rs(2, "eraw")
                ss3 = [pa.sb("ess%d" % j, [128, 3], F32) for j in range(2)]
                ss3r = pa.rs(2, "ess")
                jk = pa.sb("ejk", [128, 64], BF16)
                jkr = pa.r("ejk")
                xn = [pa.sb("exn%d" % j, [128, 3, 64], F32) for j in range(2)]
                xnr = pa.rs(2, "exn")
                tt = [pa.sb("ett%d" % j, [128, 4, 3, 32], F32) for j in range(2)]
                ttr = pa.rs(2, "ett")
                rp = [pa.sb("erp%d" % j, [128, 3, 64], F32) for j in range(2)]
                rpr = pa.rs(2, "erp")
                stage = [pa.sb("est%d" % j, [128, 2, 128], BF16) for j in range(2)]
                stager = pa.rs(2, "est")
                vst = [pa.sb("evs%d" % j, [128, 65], BF16) for j in range(3)]
                vstr = pa.rs(3, "evs")
                for j in range(3):
                    kk.op("dve", lambda e, j=j: e.memset(vst[j][:, 64:65], 1.0), writes=[vstr[j]])
                qkT = [pa.sb("eqk%d" % j, [128, 2, 128], BF16) for j in range(3)]
                qkTr = pa.rs(3, "eqk")
                PT = [pa.sb("ePT%d" % j, [128, 512], BF16) for j in range(2)]
                PTr = pa.rs(2, "ePT")
                den = [pa.sb("eden%d" % j, [128, 2], F32) for j in range(2)]
                denr = pa.rs(2, "eden")
                for c in range(4):
                    g = c // 2
                    w, wr = win[c % 2], winr[c % 2]
                    kk.dma("pool", w[:, :, 0:128], wblk(w_in, 0, NCH, c * 128, 128), writes=[wr])
                    kk.dma("pool", w[:, :, 128:192], wblk(w_in, 0, NCH, 512 + g * 64, 64), writes=[wr])
                    kk.dma("pool", w[:, :, 192:256], wblk(w_in, 0, NCH, 640 + g * 64, 64), writes=[wr])
                    wo_, wor_ = wout[oi % 2], woutr[oi % 2]
                    kk.dma("pool", wo_[:], w_out[c * 128:(c + 1) * 128, :], writes=[wor_])
                    def swa_A(i, c=c, g=g, w=w, wr=wr):
                        p2, p3 = i % 2, i % 3
                        b = kk.bank()
                        for kc in range(NCH):
                            kk.op("pe", lambda e, kc=kc, b=b, i=i, w=w: e.matmul(
                                self.P[:, b, 0:256], lhsT=hT[:, kc, i * 128:(i + 1) * 128], rhs=w[:, kc, :],
                                start=(kc == 0), stop=(kc == NCH - 1)),
                                reads=[wr, hTr[i]], writes=[self.PB[b]])
                        rw, rwr = raw[p2], rawr[p2]
                        kk.op("act", lambda e, b=b, rw=rw: e.copy(out=rw[:], in_=self.P[:, b, 0:256]), reads=[self.PB[b]], writes=[rwr])
                        s3, s3r = ss3[p2], ss3r[p2]
                        for h in range(3):
                            kk.op("act", lambda e, h=h, rw=rw, s3=s3: e.activation(out=jk[:], in_=rw[:, h * 64:(h + 1) * 64], func=AF.Square,
                                                                              accum_out=s3[:, h:h + 1]),
                                  reads=[rwr], writes=[jkr, s3r])
                        self.rstd_from_ss(s3[:], s3[:], 64, s3r)
                        x_, x_r = xn[p2], xnr[p2]
                        for h in range(3):
                            kk.op("dve", lambda e, h=h, rw=rw, s3=s3, x_=x_: e.scalar_tensor_tensor(
                                out=x_[:, h, :], in0=rw[:, h * 64:(h + 1) * 64], scalar=s3[:, h:h + 1], in1=g3[:, h, :],
                                op0=ALU.mult, op1=ALU.mult),
                                reads=[rwr, s3r, g3r], writes=[x_r])
                        t_, t_r = tt[p2], ttr[p2]
                        r2, r2r = rp[p2], rpr[p2]
                        x1, x2 = x_[:, :, 0:32], x_[:, :, 32:64]
                        cs_, sn_ = cos3[:, i, :, :], sin3[:, i, :, :]
                        for k_, (a_, b_) in enumerate(((x1, cs_), (x2, sn_), (x2, cs_), (x1, sn_))):
                            kk.op("pool", lambda e, k_=k_, a_=a_, b_=b_, t_=t_: e.tensor_tensor(out=t_[:, k_, :, :], in0=a_, in1=b_, op=ALU.mult),
                                  reads=[x_r, tabr], writes=[t_r])
                        kk.op("pool", lambda e, t_=t_, r2=r2: e.tensor_tensor(out=r2[:, :, 0:32], in0=t_[:, 0, :, :], in1=t_[:, 1, :, :], op=ALU.subtract),
                              reads=[t_r], writes=[r2r])
                        kk.op("pool", lambda e, t_=t_, r2=r2: e.tensor_tensor(out=r2[:, :, 32:64], in0=t_[:, 2, :, :], in1=t_[:, 3, :, :], op=ALU.add),
                              reads=[t_r], writes=[r2r])
                        st_, st_r = stage[p2], stager[p2]
                        kk.op("act", lambda e, st_=st_, r2=r2: e.copy(out=st_[:, 0, :].rearrange("p (a b) -> p a b", a=2), in_=r2[:, 0:2, :]),
                              reads=[r2r], writes=[st_r])
                        kk.op("act", lambda e, st_=st_, r2=r2: e.copy(out=st_[:, 1, 0:64], in_=r2[:, 2, :]), reads=[r2r], writes=[st_r])
                        kk.op("act", lambda e, st_=st_, r2=r2: e.copy(out=st_[:, 1, 64:128], in_=r2[:, 2, :]), reads=[r2r], writes=[st_r])
                        vs, vsr = vst[p3], vstr[p3]
                        kk.op("dve", lambda e, vs=vs, rw=rw: e.tensor_copy(out=vs[:, 0:64], in_=rw[:, 192:256]), reads=[rwr], writes=[vsr])
                        bt = kk.bank()
                        pv = self.pbb(bt)
                        for j in range(2):
                            kk.op("pe", lambda e, j=j, pv=pv, st_=st_: e.transpose(out=pv[:, j * 128:(j + 1) * 128], in_=st_[:, j, :],
                                                                                identity=self.identb[:]),
                                  reads=[st_r, self.CR2], writes=[self.PB[bt]])
                        qk, qkr = qkT[p3], qkTr[p3]
                        kk.op("act", lambda e, pv=pv, qk=qk: e.copy(out=qk[:].rearrange("p a b -> p (a b)"), in_=pv[:, 0:256]),
                              reads=[self.PB[bt]], writes=[qkr])
                    def swa_B(i, c=c, wo_=wo_, wor_=wor_):
                        p2, p3 = i % 2, i % 3
                        qk, qkr = qkT[p3], qkTr[p3]
                        oi = oibox[0]
                        blks = [(1, i)] if i == 0 else [(0, i - 1), (1, i)]
                        bs = kk.bank(2)
                        lo = 128 if i == 0 else 0
                        for (blk, ti) in blks:
                            for hh in range(2):
                                kq, kqr = qkT[ti % 3], qkTr[ti % 3]
                                kk.op("pe", lambda e, blk=blk, hh=hh, kq=kq, qk=qk, bs=bs: e.matmul(
                                    self.P[:, bs + hh, blk * 128:(blk + 1) * 128],
                                    lhsT=kq[hh * 64:(hh + 1) * 64, 1, :], rhs=qk[hh * 64:(hh + 1) * 64, 0, :], start=True, stop=True),
                                    reads=[kqr, qkr], writes=[self.PB[bs + hh]])
                        pt, ptr = PT[p2], PTr[p2]
                        pt3 = pt[:].rearrange("p (a b) -> p a b", a=2)
                        mk3 = self.swamask[:].rearrange("p (a b) -> p a b", a=2)
                        kk.op("act", lambda e, pt3=pt3, bs=bs, lo=lo: e.activation(out=pt3[:, :, lo:256], in_=self.P[:, bs:bs + 2, lo:256], func=AF.Exp, scale=0.125),
                              reads=[self.PB[bs], self.PB[bs + 1]], writes=[ptr])
                        kk.op("dve", lambda e, pt3=pt3, mk3=mk3, lo=lo: e.tensor_tensor(out=pt3[:, :, lo:256], in0=pt3[:, :, lo:256], in1=mk3[:, :, lo:256], op=ALU.mult),
                              reads=[ptr, self.CR2], writes=[ptr])
                        bo = kk.bank()
                        for hh in range(2):
                            for bi, (blk, ti) in enumerate(blks):
                                kk.op("pe", lambda e, hh=hh, blk=blk, ti=ti, bi=bi, bo=bo, pt=pt: e.matmul(
                                    self.P[:, bo, hh * 65:(hh + 1) * 65], lhsT=pt[:, (hh * 2 + blk) * 128:(hh * 2 + blk + 1) * 128],
                                    rhs=vst[ti % 3][:, :], start=(bi == 0), stop=(bi == len(blks) - 1)),
                                    reads=[ptr, vstr[ti % 3]], writes=[self.PB[bo]])
                        dn, dnr = den[p2], denr[p2]
                        kk.op("dve", lambda e, dn=dn, bo=bo, c=c: e.tensor_tensor(out=dn[:], in0=self.P[:, bo, 64:130:65], in1=esk[:, 2 * c:2 * c + 2], op=ALU.add),
                              reads=[self.PB[bo], eskr], writes=[dnr])
                        kk.op("dve", lambda e, dn=dn: e.reciprocal(out=dn[:], in_=dn[:]), reads=[dnr], writes=[dnr])
                        oc, ocr = Oc[oi % 2], Ocr[oi % 2]
                        for hh in range(2):
                            kk.op("dve", lambda e, hh=hh, dn=dn, bo=bo, oc=oc: e.tensor_scalar(
                                out=oc[:, hh * 64:(hh + 1) * 64], in0=self.P[:, bo, hh * 65:hh * 65 + 64], scalar1=dn[:, hh:hh + 1], scalar2=None,
                                op0=ALU.mult),
                                reads=[self.PB[bo], dnr], writes=[ocr])
                        bt2 = kk.bank()
                        pv2 = self.pbb(bt2)
                        kk.op("pe", lambda e, pv2=pv2, oc=oc: e.transpose(out=pv2[:, 0:128], in_=oc[:], identity=self.identb[:]),
                              reads=[ocr, self.CR2], writes=[self.PB[bt2]])
                        at, atr = aT[oi % 2], aTr[oi % 2]
                        kk.op("act", lambda e, pv2=pv2, at=at: e.copy(out=at[:], in_=pv2[:, 0:128]), reads=[self.PB[bt2]], writes=[atr])
                        self.outproj_partial(i, at[:], atr, wo_, wor_)
                        oibox[0] = oi + 1

                    oibox = [oi]
                    swa_A(0)
                    for i in range(NT):
                        if i + 1 < NT:
                            swa_A(i + 1)
                        swa_B(i)
                    oi = oibox[0]
            with Phase(kk) as pb_:
                win = [pb_.sb("ewb%d" % j, [128, NCH, 384], BF16) for j in range(2)]
                winr = pb_.rs(2, "ewb")
                qbT = [pb_.sb("eqb%d" % j, [128, S], BF16) for j in range(2)]
                qbTr = [pb_.rs(NG, "eqb%d_" % j) for j in range(2)]
                kbT = [pb_.sb("ekb%d" % j, [128, S], BF16) for j in range(2)]
                kbTr = [pb_.rs(NG, "ekb%d_" % j) for j in range(2)]
                vb = [pb_.sb("evb%d" % j, [128, NT, 128], BF16) for j in range(2)]
                vbr = [pb_.rs(NT, "evb%d_" % j) for j in range(2)]
                ones = pb_.sb("eones", [128, S], BF16)
                onesr = pb_.r("eones")
                kk.op("dve", lambda e: e.memset(ones[:], 1.0), writes=[onesr])
                E = [pb_.sb("eE%d" % j, [128, S], F32) for j in range(2)]
                Er = pb_.rs(2, "eE")
                SPb = [pb_.sb("eSP%d" % j, [128, S], F32) for j in range(2)]
                SPr = pb_.rs(2, "eSP")
                CS = pb_.sb("eCS", [128, S + 1], F32)
                CSr = pb_.r("eCS")
                kk.op("dve", lambda e: e.memset(CS[:, 0:1], 0.0), writes=[CSr])
                ntot = pb_.sb("ent", [128, 1], F32)
                ntr = pb_.r("ent")
                Ab = [pb_.sb("eA%d" % j, [128, S], BF16) for j in range(2)]
                Abr = pb_.rs(2, "eA")
                AT = [pb_.sb("eAT%d" % j, [128, NT, 128], BF16) for j in range(1)]
                ATr = pb_.rs(1, "eAT")
                for c in range(4):
                    w, wr = win[c % 2], winr[c % 2]
                    kk.dma("pool", w[:, :, 0:128], wblk(w_in, 0, NCH, 768 + c * 128, 128), writes=[wr])
                    kk.dma("pool", w[:, :, 128:256], wblk(w_in, 0, NCH, 1280 + c * 128, 128), writes=[wr])
                    kk.dma("pool", w[:, :, 256:384], wblk(w_in, 0, NCH, 1792 + c * 128, 128), writes=[wr])
                    wo_, wor_ = wout[oi % 2], woutr[oi % 2]
                    kk.dma("pool", wo_[:], w_out[512 + c * 128:512 + (c + 1) * 128, :], writes=[wor_])
                    q_, q_r = qbT[c % 2], qbTr[c % 2]
                    k_, k_r = kbT[c % 2], kbTr[c % 2]
                    v_, v_r = vb[c % 2], vbr[c % 2]
                    for g in range(NG):
                        tsl = slice(g * 512, (g + 1) * 512)
                        for (dst, dstr, co) in ((q_, q_r, 0), (k_, k_r, 128)):
                            bb = kk.bank()
                            for kc in range(NCH):
                                kk.op("pe", lambda e, bb=bb, co=co, kc=kc, w=w, tsl=tsl: e.matmul(
                                    self.P[:, bb, :], lhsT=w[:, kc, co:co + 128], rhs=hT[:, kc, tsl],
                                    start=(kc == 0), stop=(kc == NCH - 1)),
                                    reads=[wr] + hTr[g * 4:(g + 1) * 4], writes=[self.PB[bb]])
                            kk.op("act", lambda e, bb=bb, dst=dst, tsl=tsl: e.copy(out=dst[:, tsl], in_=self.P[:, bb, :]),
                                  reads=[self.PB[bb]], writes=[dstr[g]])
                    for i in range(NT):
                        if i % 4 == 0:
                            bv = kk.bank()
                        for kc in range(NCH):
                            kk.op("pe", lambda e, kc=kc, bv=bv, i=i, w=w: e.matmul(
                                self.P[:, bv, (i % 4) * 128:(i % 4 + 1) * 128], lhsT=hT[:, kc, i * 128:(i + 1) * 128], rhs=w[:, kc, 256:384],
                                start=(kc == 0), stop=(kc == NCH - 1)),
                                reads=[wr, hTr[i]], writes=[self.PB[bv]])
                        kk.op("dve", lambda e, bv=bv, i=i, v_=v_: e.tensor_copy(out=v_[:, i, :], in_=self.P[:, bv, (i % 4) * 128:(i % 4 + 1) * 128]),
                              reads=[self.PB[bv]], writes=[v_r[i]])
                    its = [(n, hh) for n in range(NT) for hh in range(2)]
                    state = {}

                    def stA(k, c=c, q_=q_, q_r=q_r, k_=k_, k_r=k_r):
                        n, hh = its[k]
                        L = (n + 1) * 128
                        nb = 1 if L <= 512 else (2 if L <= 1024 else 4)
                        ps_ = slice(hh * 64, (hh + 1) * 64)
                        bz = kk.bank(nb)
                        Z = self.pb(bz, nb)
                        zr = [self.PB[bz + j] for j in range(nb)]
                        nj = (L + 511) // 512
                        for j in range(nj):
                            c0, c1 = j * 512, min(L, (j + 1) * 512)
                            kk.op("pe", lambda e, c0=c0, c1=c1: e.matmul(
                                Z[:, c0:c1], lhsT=q_[ps_, n * 128:(n + 1) * 128], rhs=k_[ps_, c0:c1], start=True, stop=True),
                                reads=[q_r[n // 4], k_r[j]], writes=[self.PB[bz + j]])
                        gi = state.setdefault("gi", 0)
                        state["gi"] = gi + 1
                        e_, e_r = E[gi % 2], Er[gi % 2]
                        sp_, sp_r = SPb[gi % 2], SPr[gi % 2]
                        ab, abr = Ab[gi % 2], Abr[gi % 2]
                        state[k] = (L, e_, e_r, sp_, sp_r, ab, abr)
                        kk.op("act", lambda e: e.activation(out=e_[:, 0:L], in_=Z[:, 0:L], func=AF.Exp, scale=0.125),
                              reads=zr[0:nj], writes=[e_r])
                        kk.op("dve", lambda e: e.tensor_tensor(out=e_[:, L - 128:L], in0=e_[:, L - 128:L], in1=self.sbmask[:], op=ALU.mult),
                              reads=[e_r, self.CR], writes=[e_r])
                        kk.op("act", lambda e: e.activation(out=sp_[:, 0:L], in_=e_[:, 0:L], func=AF.Ln, bias=1.0, scale=1.0),
                              reads=[e_r], writes=[sp_r])

                    def stB(k):
                        L, e_, e_r, sp_, sp_r, ab, abr = state[k]
                        kk.op("dve", lambda e: e.tensor_tensor_scan(out=CS[:, 1:L + 1], data0=ones[:, 0:L], data1=sp_[:, 0:L], initial=0.0,
                                                                     op0=ALU.mult, op1=ALU.add),
                              reads=[sp_r, onesr], writes=[CSr])
                        kk.op("dve", lambda e: e.tensor_scalar(out=ntot[:], in0=CS[:, L:L + 1], scalar1=-1.0, scalar2=None, op0=ALU.mult),
                              reads=[CSr], writes=[ntr])
                        kk.op("act", lambda e: e.activation(out=sp_[:, 0:L], in_=CS[:, 0:L], func=AF.Exp, bias=ntot[:, 0:1], scale=1.0),
                              reads=[CSr, ntr], writes=[sp_r])
                        kk.op("pool", lambda e: e.tensor_tensor(out=ab[:, 0:L], in0=e_[:, 0:L], in1=sp_[:, 0:L], op=ALU.mult),
                              reads=[e_r, sp_r], writes=[abr])

                    def stC(k, c=c, v_=v_, v_r=v_r, wo_=wo_, wor_=wor_):
                        n, hh = its[k]
                        L, e_, e_r, sp_, sp_r, ab, abr = state.pop(k)
                        if hh == 0:
                            state["bo"] = kk.bank()
                            kk.reserved.add(state["bo"])
                        bo = state["bo"]
                        at_, at_r = AT[0], ATr[0]
                        for k0 in range(0, n + 1, 8):
                            k1 = min(n + 1, k0 + 8)
                            bt = kk.bank()
                            while bt == bo:
                                bt = kk.bank()
                            pv = self.pbb(bt)
                            for kb in range(k0, k1):
                                kk.op("pe", lambda e, kb=kb, k0=k0, pv=pv: e.transpose(
                                    out=pv[:, (kb - k0) * 128:(kb - k0 + 1) * 128], in_=ab[:, kb * 128:(kb + 1) * 128], identity=self.identb[:]),
                                    reads=[abr, self.CR2], writes=[self.PB[bt]])
                            kk.op("act", lambda e, k0=k0, k1=k1, pv=pv: e.copy(
                                out=at_[:, k0:k1, :], in_=pv[:, 0:(k1 - k0) * 128].rearrange("p (a b) -> p a b", b=128)),
                                reads=[self.PB[bt]], writes=[at_r])
                        for kb in range(n + 1):
                            kk.op("pe", lambda e, kb=kb: e.matmul(
                                self.P[:, bo, hh * 64:(hh + 1) * 64], lhsT=at_[:, kb, :], rhs=v_[:, kb, hh * 64:(hh + 1) * 64],
                                start=(kb == 0), stop=(kb == n)),
                                reads=[at_r, v_r[kb]], writes=[self.PB[bo]])
                        if hh == 1:
                            oi = state["oi"]
                            oc, ocr = Oc[oi % 2], Ocr[oi % 2]
                            kk.op("dve", lambda e: e.tensor_copy(out=oc[:], in_=self.P[:, bo, 0:128]), reads=[self.PB[bo]], writes=[ocr])
                            kk.reserved.discard(bo)
                            bt2 = kk.bank()
                            pv2 = self.pbb(bt2)
                            kk.op("pe", lambda e: e.transpose(out=pv2[:, 0:128], in_=oc[:], identity=self.identb[:]),
                                  reads=[ocr, self.CR2], writes=[self.PB[bt2]])
                            at, atr = aT[oi % 2], aTr[oi % 2]
                            kk.op("act", lambda e: e.copy(out=at[:], in_=pv2[:, 0:128]), reads=[self.PB[bt2]], writes=[atr])
                            self.outproj_partial(n, at[:], atr, wo_, wor_)
                            state["oi"] = oi + 1

                    state["oi"] = oi
                    NI = len(its)
                    for t in range(NI + 2):
                        if 0 <= t - 2 < NI:
                            stC(t - 2)
                        if 0 <= t - 1 < NI:
                            stB(t - 1)
                        if t < NI:
                            stA(t)
                    oi = state["oi"]


def host_consts():
    p = np.arange(128)[:, None]
    i = np.arange(128)[None, :]
    ident = np.eye(128, dtype=np.float32)
    mprev = (i < p).astype(np.float32)
    mcur = (i >= p).astype(np.float32)
    swamask = np.stack([mprev, mcur, mprev, mcur], axis=1).reshape(128, 512).astype(np.float32)
    sbmask = (p > i).astype(np.float32)
    hmask = ((i >= p) & ((i // 32) == (p // 32))).astype(np.float32)
    invf = (10000.0 ** (-np.arange(0, 64, 2, dtype=np.float32) / np.float32(64))).astype(np.float32)
    invf = np.broadcast_to(invf[None, :], (128, 32)).copy()
    j4 = np.arange(4)[None, :, None]
    t4 = np.arange(128)[None, None, :]
    p4 = np.arange(128)[:, None, None]
    cmask4 = np.broadcast_to((t4 // 32) == j4, (128, 4, 128)).astype(np.float32).reshape(128, 512)
    rmask4 = np.broadcast_to((p4 // 32) == j4, (128, 4, 128)).astype(np.float32).reshape(128, 512)
    return {"c_ident": ident, "c_swamask": swamask, "c_sbmask": sbmask, "c_hmask": hmask, "c_invf": invf,
            "c_cmask4": np.ascontiguousarray(cmask4), "c_rmask4": np.ascontiguousarray(rmask4)}


_PROG = {}


def kernel(**inputs):
    n = 8
    key = "full"
    if key not in _PROG:
        _PROG[key] = Prog()
    prog = _PROG[key]
    consts = host_consts()
    per = 32 // n
    in_maps = []
    for c in range(n):
        m = {}
        for k, v in inputs.items():
            v = np.asarray(v)
            if k in ("x", "mem", "positions"):
                m[k] = np.ascontiguousarray(v[c * per:(c + 1) * per])
            else:
                m[k] = np.ascontiguousarray(v)
        m.update(consts)
        in_maps.append(m)
    res = run_bass_kernel_spmd(prog.nc, in_maps, core_ids=list(range(n)))
    return np.concatenate([r["out"] for r in res.results], axis=0).astype(np.float32)
```

```python
import contextlib
import numpy as np
import concourse.bass as bass
import concourse.mybir as mybir
from concourse.bass_utils import run_bass_kernel_spmd

F32 = mybir.dt.float32
BF16 = mybir.dt.bfloat16
I32 = mybir.dt.int32
AF = mybir.ActivationFunctionType
ALU = mybir.AluOpType
AX = mybir.AxisListType

D = 1024
NCH = 8
DFF = 2816
NFC = 22
EPS = 1e-6
N_MEM = 256
TWO_PI = 6.283185307179586
CW1 = 6.28125
CW2 = TWO_PI - 6.28125
MAGIC = 12582912.0
PI_LO = 3.1415925


class Res:
    __slots__ = ("name", "w", "rd", "dsem", "excl")

    def __init__(self, name="", excl=False):
        self.name = name
        self.w = None
        self.rd = {}
        self.dsem = None
        self.excl = excl


class DSem:
    __slots__ = ("h", "cnt", "key")

    def __init__(self, h, key):
        self.h = h
        self.cnt = 0
        self.key = key


class K:
    def __init__(self, nc, es):
        self.nc = nc
        self.es = es
        self.eng = {"pe": nc.tensor, "act": nc.scalar, "dve": nc.vector, "pool": nc.gpsimd, "sp": nc.sync}
        self.sem = {}
        self.semobj = {}
        for k in ("pe", "act", "dve", "pool"):
            h = es.enter_context(nc.semaphore("s_" + k))
            self.sem[k] = h
            self.semobj[k] = h
        self.cnt = {k: 0 for k in self.eng}
        self.seen = {k: {} for k in self.eng}
        self.free_dsems = {"sp": [], "pool": []}
        self.ndsem = 0
        self.bank_rr = 0
        self.reserved = set()
        self.n_ins = 0

    def _dsem(self, q):
        fl = self.free_dsems[q]
        if fl:
            return fl.pop()
        key = "d%d" % self.ndsem
        self.ndsem += 1
        h = self.es.enter_context(self.nc.semaphore(key))
        ds = DSem(h, key)
        self.semobj[key] = h
        return ds

    def release(self, q, res_list):
        for r in res_list:
            if r.dsem is not None and r.dsem[0] == q:
                self.free_dsems[q].append(r.dsem[1])
                r.dsem = None

    def _waits(self, e, reads, writes):
        raw = {}
        oth = {}
        for r in reads:
            if r.w is not None:
                k, v = r.w
                if raw.get(k, 0) < v:
                    raw[k] = v
            if r.excl:
                for k, v in r.rd.items():
                    if k != e and oth.get(k, 0) < v:
                        oth[k] = v
        for w in writes:
            if w.w is not None:
                k, v = w.w
                if oth.get(k, 0) < v:
                    oth[k] = v
            for k, v in w.rd.items():
                if oth.get(k, 0) < v:
                    oth[k] = v
        deps = dict(raw)
        for k, v in oth.items():
            if k == e and e == "pe":
                continue
            if deps.get(k, 0) < v:
                deps[k] = v
        if e == "pe":
            deps.pop("pe", None)
        eng = self.eng[e]
        seen = self.seen[e]
        for k, v in deps.items():
            if seen.get(k, 0) >= v:
                continue
            eng.wait_ge(self.semobj[k], v)
            seen[k] = v
            self.n_ins += 1

    def op(self, e, fn, reads=(), writes=()):
        self._waits(e, reads, writes)
        ins = fn(self.eng[e])
        self.cnt[e] += 1
        c = self.cnt[e]
        ins.then_inc(self.sem[e], 1)
        self.n_ins += 1
        for r in reads:
            if r.rd.get(e, 0) < c:
                r.rd[e] = c
        for w in writes:
            w.w = (e, c)
            w.rd = {}
        return ins

    def dma(self, q, out, in_, reads=(), writes=(), owner=None):
        self._waits(q, reads, writes)
        if owner is None:
            owner = writes[0] if writes else reads[0]
        if owner.dsem is None or owner.dsem[0] != q:
            assert owner.dsem is None
            owner.dsem = (q, self._dsem(q))
        ds = owner.dsem[1]
        ins = self.eng[q].dma_start(out=out, in_=in_)
        ins.then_inc(ds.h, 16)
        ds.cnt += 16
        self.n_ins += 1
        tok = (ds.key, ds.cnt)
        for r in reads:
            if r.rd.get(ds.key, 0) < ds.cnt:
                r.rd[ds.key] = ds.cnt
        for w in writes:
            w.w = tok
            w.rd = {}
        return ins

    def barrier(self):
        ce = ("pe", "act", "dve", "pool")
        for a in ce + ("sp",):
            for b in ce:
                if a == b:
                    continue
                v = self.cnt[b]
                if v and self.seen[a].get(b, 0) < v:
                    self.eng[a].wait_ge(self.sem[b], v)
                    self.seen[a][b] = v
                    self.n_ins += 1

    def wait_all(self, e, res_list):
        self._waits(e, [], res_list)

    def bank(self, k=1):
        for _ in range(16):
            b = self.bank_rr
            if b % k:
                b += k - (b % k)
            if b + k > 8:
                b = 0
            self.bank_rr = (b + k) % 8
            if not any((b + j) in self.reserved for j in range(k)):
                return b
        raise RuntimeError("no free PSUM bank")


class Phase:
    uid = 0

    def __init__(self, kk):
        self.kk = kk
        self.es = contextlib.ExitStack()
        self.res = []

    def __enter__(self):
        self.es.__enter__()
        return self

    def sb(self, name, shape, dtype):
        Phase.uid += 1
        return self.es.enter_context(self.kk.nc.sbuf_tensor("%s_u%d" % (name, Phase.uid), list(shape), dtype))

    def r(self, name=""):
        x = Res(name)
        self.res.append(x)
        return x

    def rs(self, n, name=""):
        return [self.r(name + str(i)) for i in range(n)]

    def __exit__(self, *a):
        kk = self.kk
        kk.barrier()
        for q in ("sp", "pool"):
            kk.release(q, self.res)
        return self.es.__exit__(*a)


def wblk(w2d, kc0, nkc, c0, ncols):
    return w2d.rearrange("(kc p) n -> p kc n", p=128)[:, kc0:kc0 + nkc, c0:c0 + ncols]


class Prog:
    def __init__(self, S=2048, NSEQ=4, layers=(0, 1, 2, 3), dbg=False, parts=("mix", "xa", "ffn")):
        self.S = S
        self.NT = S // 128
        self.NG = S // 512
        self.NSEQ = NSEQ
        self.layers = layers
        self.dbg = dbg
        self.parts = parts
        self.nc = bass.Bass("TRN2", target_bir_lowering=False)
        self.build()

    def din(self, name, shape, dt=F32):
        return self.nc.dram_tensor(name, list(shape), dt, kind="ExternalInput").ap()

    def build(self):
        nc, S, NT, NSEQ = self.nc, self.S, self.NT, self.NSEQ
        self.x_d = self.din("x", [NSEQ, S, D])
        self.mem_d = self.din("mem", [NSEQ, N_MEM, D])
        self.pos_d = self.din("positions", [NSEQ, S], I32)
        self.norm_mix = self.din("norm_mix", [4, D])
        self.norm_cross = self.din("norm_cross", [4, D])
        self.norm_mem = self.din("norm_mem", [4, D])
        self.norm_ffn = self.din("norm_ffn", [4, D])
        self.ab_w_in = self.din("ab_w_in", [2, D, 2304])
        self.ab_w_out = self.din("ab_w_out", [2, D, D])
        self.swa_q_norm = self.din("swa_q_norm", [2, 64])
        self.swa_k_norm = self.din("swa_k_norm", [2, 64])
        self.swa_sinks = self.din("swa_sinks", [2, 8])
        self.hgrn_w_in = self.din("hgrn_w_in", [2, D, 4096])
        self.hgrn_w_out = self.din("hgrn_w_out", [2, D, D])
        self.hgrn_o_norm = self.din("hgrn_o_norm", [2, 128])
        self.hgrn_lb = self.din("hgrn_lb", [4, D])
        self.xa_w_q = self.din("xa_w_q", [4, D, D])
        self.xa_w_kv = self.din("xa_w_kv", [4, D, 2 * D])
        self.xa_w_o = self.din("xa_w_o", [4, D, D])
        self.xa_q_norm = self.din("xa_q_norm", [4, 256])
        self.xa_k_norm = self.din("xa_k_norm", [4, 256])
        self.ffn_w_up = self.din("ffn_w_up", [4, D, 2 * DFF])
        self.ffn_conv_w = self.din("ffn_conv_w", [4, 3, 2 * DFF])
        self.ffn_conv_b = self.din("ffn_conv_b", [4, 2 * DFF])
        self.ffn_w_down = self.din("ffn_w_down", [4, DFF, D])
        self.c_ident = self.din("c_ident", [128, 128])
        self.c_swamask = self.din("c_swamask", [128, 512])
        self.c_sbmask = self.din("c_sbmask", [128, 128])
        self.c_hmask = self.din("c_hmask", [128, 128])
        self.c_invf = self.din("c_invf", [128, 32])
        self.c_cmask4 = self.din("c_cmask4", [128, 512])
        self.c_rmask4 = self.din("c_rmask4", [128, 512])
        self.out_d = nc.dram_tensor("out", [NSEQ, S, D], F32, kind="ExternalOutput").ap()
        if self.dbg:
            self.dbg_d = nc.dram_tensor("dbg", [16, S, D], F32, kind="ExternalOutput").ap()
            self.ndbg = 0

        with contextlib.ExitStack() as es:
            self.es = es
            kk = self.kk = K(nc, es)
            sb = lambda name, shape, dt: es.enter_context(nc.sbuf_tensor(name, list(shape), dt))
            self.X = sb("X", [128, NT, D], F32)
            self.XR = [Res("X%d" % i) for i in range(NT)]
            self.P = es.enter_context(nc.psum_tensor("P", [128, 8, 512], F32))
            self.PB = [Res("B%d" % i, excl=True) for i in range(8)]
            self.identf = sb("identf", [128, 128], F32)
            self.identb = sb("identb", [128, 128], BF16)
            self.onesb = sb("onesb", [128, 128], BF16)
            self.swamask = sb("swamask", [128, 512], BF16)
            self.sbmask = sb("sbmask", [128, 128], F32)
            self.hmask = sb("hmask", [128, 128], F32)
            self.invf = sb("invf", [128, 32], F32)
            self.lbt = sb("lbt", [128, 32], F32)
            self.omlt = sb("omlt", [128, 32], F32)
            self.nomlt = sb("nomlt", [128, 32], F32)
            self.CR = Res("consts")
            cr = [self.CR]
            kk.dma("sp", self.identf[:], self.c_ident, writes=cr)
            kk.dma("sp", self.sbmask[:], self.c_sbmask, writes=cr)
            kk.dma("sp", self.hmask[:], self.c_hmask, writes=cr)
            kk.dma("sp", self.invf[:], self.c_invf, writes=cr)
            self.CR2 = Res("consts2")
            kk.dma("pool", self.identb[:], self.c_ident, writes=[self.CR2])
            kk.dma("pool", self.swamask[:], self.c_swamask, writes=[self.CR2])
            self.cmask4 = sb("cmask4", [128, 512], BF16)
            self.rmask4 = sb("rmask4", [128, 512], BF16)
            kk.dma("pool", self.cmask4[:], self.c_cmask4, writes=[self.CR2])
            kk.dma("pool", self.rmask4[:], self.c_rmask4, writes=[self.CR2])
            kk.op("dve", lambda e: e.memset(self.onesb[:], 1.0), writes=[self.CR2])
            self.setup_lb()
            for s in range(NSEQ):
                self.run_seq(s)
            kk.wait_all("sp", self.XR)
            kk.barrier()

    def pb(self, b, k=1):
        if k == 1:
            return self.P[:, b, :]
        return self.P[:, b:b + k, :].rearrange("p b f -> p (b f)")

    def pbb(self, b):
        return self.P[:, b, :].bitcast(BF16)

    def setup_lb(self):
        kk = self.kk
        with Phase(kk) as ph:
            raw = ph.sb("lbraw", [32, 128], F32)
            rr = ph.r("lbraw")
            kk.dma("sp", raw[:], self.hgrn_lb.rearrange("l (c p) -> (l c) p", p=128), writes=[rr])
            b = kk.bank()
            kk.op("pe", lambda e: e.transpose(out=self.P[:, b, 0:32], in_=raw[:], identity=self.identf[0:32, 0:32]),
                  reads=[rr, self.CR], writes=[self.PB[b]])
            xs = ph.sb("lbx", [128, 32], F32)
            r2 = ph.r("lbx")
            kk.op("act", lambda e: e.copy(out=xs[:], in_=self.P[:, b, 0:32]), reads=[self.PB[b]], writes=[r2])
            mx = ph.sb("lbmx", [128, 8], F32)
            kk.op("dve", lambda e: e.tensor_max(out=mx[:], in0=xs[:, 0:8], in1=xs[:, 8:16]), reads=[r2], writes=[r2])
            kk.op("dve", lambda e: e.tensor_max(out=mx[:], in0=mx[:], in1=xs[:, 16:24]), reads=[r2], writes=[r2])
            kk.op("dve", lambda e: e.tensor_max(out=mx[:], in0=mx[:], in1=xs[:, 24:32]), reads=[r2], writes=[r2])
            ex = ph.sb("lbex", [128, 32], F32)
            for l in range(4):
                kk.op("dve", lambda e, l=l: e.tensor_sub(out=ex[:, l * 8:(l + 1) * 8], in0=xs[:, l * 8:(l + 1) * 8], in1=mx[:]),
                      reads=[r2], writes=[r2])
            kk.op("act", lambda e: e.activation(out=ex[:], in_=ex[:], func=AF.Exp), reads=[r2], writes=[r2])
            sm = ph.sb("lbsm", [128, 8], F32)
            kk.op("dve", lambda e: e.tensor_add(out=sm[:], in0=ex[:, 0:8], in1=ex[:, 8:16]), reads=[r2], writes=[r2])
            kk.op("dve", lambda e: e.tensor_add(out=sm[:], in0=sm[:], in1=ex[:, 16:24]), reads=[r2], writes=[r2])
            kk.op("dve", lambda e: e.tensor_add(out=sm[:], in0=sm[:], in1=ex[:, 24:32]), reads=[r2], writes=[r2])
            kk.op("dve", lambda e: e.reciprocal(out=sm[:], in_=sm[:]), reads=[r2], writes=[r2])
            for l in range(4):
                kk.op("dve", lambda e, l=l: e.tensor_mul(out=ex[:, l * 8:(l + 1) * 8], in0=ex[:, l * 8:(l + 1) * 8], in1=sm[:]),
                      reads=[r2], writes=[r2])
            lr = self.LBR = Res("lb")
            kk.op("dve", lambda e: e.memset(self.lbt[:, 0:8], 0.0), reads=[r2], writes=[lr])
            kk.op("dve", lambda e: e.tensor_copy(out=self.lbt[:, 8:16], in_=ex[:, 8:16]), reads=[r2, lr], writes=[lr])
            kk.op("dve", lambda e: e.tensor_add(out=self.lbt[:, 16:24], in0=self.lbt[:, 8:16], in1=ex[:, 16:24]), reads=[r2, lr], writes=[lr])
            kk.op("dve", lambda e: e.tensor_add(out=self.lbt[:, 24:32], in0=self.lbt[:, 16:24], in1=ex[:, 24:32]), reads=[r2, lr], writes=[lr])
            kk.op("dve", lambda e: e.tensor_scalar(out=self.omlt[:], in0=self.lbt[:], scalar1=-1.0, scalar2=1.0, op0=ALU.mult, op1=ALU.add),
                  reads=[lr], writes=[lr])
            kk.op("dve", lambda e: e.tensor_scalar(out=self.nomlt[:], in0=self.lbt[:], scalar1=1.0, scalar2=-1.0, op0=ALU.mult, op1=ALU.add),
                  reads=[lr], writes=[lr])

    def load_gain_bc(self, ph, name, row_ap, n):
        t = ph.sb(name, [128, n], F32)
        r = ph.r(name)
        self.kk.dma("sp", t[:], row_ap.partition_broadcast(128), writes=[r])
        return t, r

    def rstd_from_ss(self, ss_ap, rstd_ap, n, res):
        kk = self.kk
        kk.op("act", lambda e: e.activation(out=rstd_ap, in_=ss_ap, func=AF.Ln, bias=self.epsb[:, 0:1], scale=1.0 / n),
              reads=[res, self.CR3], writes=[res])
        kk.op("act", lambda e: e.activation(out=rstd_ap, in_=rstd_ap, func=AF.Exp, scale=-0.5), reads=[res], writes=[res])

    def norm_T(self, ph, tag, srcs, gain, gain_r, dstT, dst_rs, col0=0):
        kk = self.kk
        n = len(srcs)
        ss = ph.sb(tag + "ss", [128, n], F32)
        ssr = ph.r(tag + "ss")
        junk = ph.sb(tag + "junk", [128, D], BF16)
        jr = ph.r(tag + "junk")
        hn = [ph.sb(tag + "hn%d" % j, [128, D], BF16) for j in range(2)]
        hnr = ph.rs(2, tag + "hn")
        for i, (ap, r) in enumerate(srcs):
            kk.op("act", lambda e, ap=ap, i=i: e.activation(out=junk[:], in_=ap, func=AF.Square, accum_out=ss[:, i:i + 1]),
                  reads=[r], writes=[jr, ssr])
        self.rstd_from_ss(ss[:], ss[:], D, ssr)
        for i, (ap, r) in enumerate(srcs):
            h, hr = hn[i % 2], hnr[i % 2]
            kk.op("dve", lambda e, ap=ap, i=i, h=h: e.scalar_tensor_tensor(out=h[:], in0=ap, scalar=ss[:, i:i + 1], in1=gain[:],
                                                                         op0=ALU.mult, op1=ALU.mult),
                  reads=[r, ssr, gain_r], writes=[hr])
            b = kk.bank()
            pv = self.pbb(b)
            for c in range(NCH):
                kk.op("pe", lambda e, c=c, h=h, pv=pv: e.transpose(out=pv[:, c * 128:(c + 1) * 128], in_=h[:, c * 128:(c + 1) * 128],
                                                                  identity=self.identb[:]),
                      reads=[hr, self.CR2], writes=[self.PB[b]])
            kk.op("act", lambda e, i=i, pv=pv: e.copy(out=dstT[:, :, col0 + i * 128: col0 + (i + 1) * 128],
                                                      in_=pv.rearrange("p (c t) -> p c t", c=NCH)),
                  reads=[self.PB[b]], writes=[dst_rs[i]])

    def resid_add(self, i, b, hf):
        kk = self.kk
        xs = self.X[:, i, hf * 512:(hf + 1) * 512]
        kk.op("dve", lambda e: e.tensor_tensor(out=xs, in0=xs, in1=self.P[:, b, :], op=ALU.add),
              reads=[self.PB[b], self.XR[i]], writes=[self.XR[i]])

    def dump(self):
        if not self.dbg:
            return
        k = self.ndbg
        self.ndbg += 1
        for i in range(self.NT):
            self.kk.dma("sp", self.dbg_d[k, i * 128:(i + 1) * 128, :], self.X[:, i, :], reads=[self.XR[i]], owner=self.XR[i])

    def run_seq(self, s):
        kk, NT = self.kk, self.NT
        if s == 0:
            self.epsb = self.es.enter_context(self.nc.sbuf_tensor("epsb", [128, 1], F32))
            self.CR3 = Res("eps")
            kk.op("dve", lambda e: e.memset(self.epsb[:], EPS), writes=[self.CR3])
        for i in range(NT):
            kk.dma("sp", self.X[:, i, :], self.x_d[s, i * 128:(i + 1) * 128, :], writes=[self.XR[i]])
        for l in self.layers:
            if "mix" in self.parts:
                if l % 2 == 0:
                    self.even_mixer(s, l)
                else:
                    self.odd_mixer(s, l)
                self.dump()
            if "xa" in self.parts:
                self.xattn(s, l)
                self.dump()
            if "ffn" in self.parts:
                self.ffn(s, l)
                self.dump()
        for i in range(NT):
            kk.dma("sp", self.out_d[s, i * 128:(i + 1) * 128, :], self.X[:, i, :], reads=[self.XR[i]], owner=self.XR[i])

    def ffn(self, s, l):
        kk, S, NT, NG = self.kk, self.S, self.NT, self.NG
        with Phase(kk) as ph:
            gain, gr = self.load_gain_bc(ph, "fg", self.norm_ffn[l], D)
            hT = ph.sb("fhT", [128, NCH, S], BF16)
            hTr = ph.rs(NT, "fhT")
            self.norm_T(ph, "fn", [(self.X[:, i, :], self.XR[i]) for i in range(NT)], gain, gr, hT, hTr)
            cst = ph.sb("cst", [128, 3, 128], F32)
            cstr = ph.r("cst")
            cw = self.ffn_conv_w[l].rearrange("t (c p) -> (t c) p", p=128)
            cb = self.ffn_conv_b[l].rearrange("(c p) -> c p", p=128)
            kk.dma("sp", cst[:, 0, :], cw[0:128, :], writes=[cstr])
            kk.dma("sp", cst[0:4, 1, :], cw[128:132, :], writes=[cstr])
            kk.dma("sp", cst[0:44, 2, :], cb, writes=[cstr])
            cwb = ph.sb("cwb", [128, 176], F32)
            cwr = ph.r("cwb")
            b = kk.bank()
            kk.op("pe", lambda e: e.transpose(out=self.P[:, b, 0:128], in_=cst[:, 0, :], identity=self.identf[:]),
                  reads=[cstr, self.CR], writes=[self.PB[b]])
            kk.op("pe", lambda e: e.transpose(out=self.P[:, b, 128:132], in_=cst[0:4, 1, :], identity=self.identf[0:4, 0:4]),
                  reads=[cstr, self.CR], writes=[self.PB[b]])
            kk.op("pe", lambda e: e.transpose(out=self.P[:, b, 132:176], in_=cst[0:44, 2, :], identity=self.identf[0:44, 0:44]),
                  reads=[cstr, self.CR], writes=[self.PB[b]])
            kk.op("act", lambda e: e.copy(out=cwb[:], in_=self.P[:, b, 0:176]), reads=[self.PB[b]], writes=[cwr])

            NQ = 4
            qchunks = [list(range(0, 6)), list(range(6, 11)), list(range(11, 17)), list(range(17, 22))]
            yT = ph.sb("yT", [128, 6, S], BF16)
            yTr = [ph.rs(NG, "yT%d_" % j) for j in range(6)]
            NWB = 3
            wup = [ph.sb("wup%d" % j, [128, NCH, 256], BF16) for j in range(NWB)]
            wupr = ph.rs(NWB, "wup")
            wdn = [ph.sb("wdn%d" % j, [128, 6, D], BF16) for j in range(2)]
            wdnr = ph.rs(2, "wdn")
            NU = 3
            U = [ph.sb("U%d" % j, [128, 2, 514], F32) for j in range(NU)]
            Ur = ph.rs(NU, "U")
            TG = [ph.sb("TG%d" % j, [128, 2, 512], F32) for j in range(2)]
            TGr = ph.rs(2, "TG")
            SG = [ph.sb("SG%d" % j, [128, 512], F32) for j in range(2)]
            SGr = ph.rs(2, "SG")
            wup_d = self.ffn_w_up[l]
            wdn_d = self.ffn_w_down[l]
            ui = 0
            wi = 0
            ti = 0
            for qi, chunks in enumerate(qchunks):
                nq = len(chunks)
                wd, wdr = wdn[qi % 2], wdnr[qi % 2]
                kk.dma("pool", wd[:, 0:nq, :], wblk(wdn_d, chunks[0], nq, 0, D), writes=[wdr])
                for ci, c in enumerate(chunks):
                    w, wr = wup[wi % NWB], wupr[wi % NWB]
                    wi += 1
                    kk.dma("pool", w[:, :, 0:128], wblk(wup_d, 0, NCH, c * 128, 128), writes=[wr])
                    kk.dma("pool", w[:, :, 128:256], wblk(wup_d, 0, NCH, DFF + c * 128, 128), writes=[wr])
                    prevU = None
                    for st in range(NG):
                        bg = kk.bank()
                        bu = kk.bank()
                        for (bb, co) in ((bg, 0), (bu, 128)):
                            for kc in range(NCH):
                                kk.op("pe", lambda e, bb=bb, co=co, kc=kc, w=w, st=st: e.matmul(
                                    self.P[:, bb, :], lhsT=w[:, kc, co:co + 128], rhs=hT[:, kc, st * 512:(st + 1) * 512],
                                    start=(kc == 0), stop=(kc == NCH - 1)),
                                    reads=[wr] + hTr[st * 4:(st + 1) * 4], writes=[self.PB[bb]])
                        u, ur = U[ui % NU], Ur[ui % NU]
                        ui += 1
                        kk.op("act", lambda e, u=u, bg=bg: e.copy(out=u[:, 0, 2:514], in_=self.P[:, bg, :]),
                              reads=[self.PB[bg]], writes=[ur])
                        kk.op("act", lambda e, u=u, bu=bu: e.copy(out=u[:, 1, 2:514], in_=self.P[:, bu, :]),
                              reads=[self.PB[bu]], writes=[ur])
                        if prevU is not None:
                            pu, pur = prevU
                            kk.op("act", lambda e, u=u, pu=pu: e.copy(out=u[:, :, 0:2], in_=pu[:, :, 512:514]),
                                  reads=[pur, ur], writes=[ur])
                        else:
                            kk.op("dve", lambda e, u=u: e.memset(u[:, :, 0:2], 0.0), reads=[ur], writes=[ur])
                        prevU = (u, ur)
                        tg, tgr = TG[ti % 2], TGr[ti % 2]
                        sg, sgr = SG[ti % 2], SGr[ti % 2]
                        ti += 1
                        for gi, fc in ((0, c), (1, NFC + c)):
                            w2 = cwb[:, 2 * 44 + fc:2 * 44 + fc + 1]
                            w1 = cwb[:, 1 * 44 + fc:1 * 44 + fc + 1]
                            w0 = cwb[:, 0 * 44 + fc:0 * 44 + fc + 1]
                            bb_ = cwb[:, 3 * 44 + fc:3 * 44 + fc + 1]
                            bsrc = bg if gi == 0 else bu
                            kk.op("act", lambda e, tg=tg, gi=gi, w2=w2, bb_=bb_, bsrc=bsrc: e.activation(
                                out=tg[:, gi, :], in_=self.P[:, bsrc, :], func=AF.Identity, bias=bb_, scale=w2),
                                reads=[self.PB[bsrc], cwr], writes=[tgr])
                            kk.op("dve", lambda e, u=u, tg=tg, gi=gi, w1=w1: e.scalar_tensor_tensor(
                                out=tg[:, gi, :], in0=u[:, gi, 1:513], scalar=w1, in1=tg[:, gi, :], op0=ALU.mult, op1=ALU.add),
                                reads=[ur, cwr, tgr], writes=[tgr])
                            kk.op("dve", lambda e, u=u, tg=tg, gi=gi, w0=w0: e.scalar_tensor_tensor(
                                out=tg[:, gi, :], in0=u[:, gi, 0:512], scalar=w0, in1=tg[:, gi, :], op0=ALU.mult, op1=ALU.add),
                                reads=[ur, cwr, tgr], writes=[tgr])
                        kk.op("act", lambda e, sg=sg, tg=tg: e.activation(out=sg[:], in_=tg[:, 0, :], func=AF.Silu),
                              reads=[tgr], writes=[sgr])
                        kk.op("dve", lambda e, sg=sg, tg=tg, ci=ci, st=st: e.tensor_tensor(
                            out=yT[:, ci, st * 512:(st + 1) * 512], in0=sg[:], in1=tg[:, 1, :], op=ALU.mult),
                            reads=[sgr, tgr], writes=[yTr[ci][st]])
                for i in range(NT):
                    for hf in range(2):
                        b = kk.bank()
                        for ci in range(nq):
                            kk.op("pe", lambda e, b=b, ci=ci, i=i, hf=hf, wd=wd: e.matmul(
                                self.P[:, b, :], lhsT=yT[:, ci, i * 128:(i + 1) * 128], rhs=wd[:, ci, hf * 512:(hf + 1) * 512],
                                start=(ci == 0), stop=(ci == nq - 1)),
                                reads=[wdr, yTr[ci][i // 4]], writes=[self.PB[b]])
                        self.resid_add(i, b, hf)

    def headnorm(self, ph, tag, bank0, nh, hd, gain, gain_r, out, out_r, extra_reads=()):
        kk = self.kk
        nb = (nh * hd) // 512
        src = self.pb(bank0, nb)
        pbr = [self.PB[bank0 + j] for j in range(nb)]
        ss = ph.sb(tag + "ss", [128, nh], F32)
        ssr = ph.r(tag + "ss")
        junk = ph.sb(tag + "jk", [128, hd], BF16)
        jr = ph.r(tag + "jk")
        for h in range(nh):
            kk.op("act", lambda e, h=h: e.activation(out=junk[:], in_=src[:, h * hd:(h + 1) * hd], func=AF.Square,
                                                     accum_out=ss[:, h:h + 1]),
                  reads=pbr, writes=[jr, ssr])
        self.rstd_from_ss(ss[:], ss[:], hd, ssr)
        for h in range(nh):
            kk.op("dve", lambda e, h=h: e.scalar_tensor_tensor(out=out[:, h * hd:(h + 1) * hd], in0=src[:, h * hd:(h + 1) * hd],
                                                               scalar=ss[:, h:h + 1], in1=gain[:], op0=ALU.mult, op1=ALU.mult),
                  reads=pbr + [ssr, gain_r], writes=[out_r])

    def xattn(self, s, l):
        kk, S, NT, NG = self.kk, self.S, self.NT, self.NG
        with Phase(kk) as ph0:
            kT = ph0.sb("xkT", [128, NCH, N_MEM], BF16)
            kTr = ph0.r("xkT")
            Vt = ph0.sb("xV", [128, 2, D], BF16)
            Vr = ph0.r("xV")
            with Phase(kk) as ph:
                gm, gmr = self.load_gain_bc(ph, "gm", self.norm_mem[l], D)
                gk, gkr = self.load_gain_bc(ph, "gk", self.xa_k_norm[l], 256)
                mem = ph.sb("mem", [128, 2, D], F32)
                memr = ph.rs(2, "mem")
                for mt in range(2):
                    kk.dma("sp", mem[:, mt, :], self.mem_d[s, mt * 128:(mt + 1) * 128, :], writes=[memr[mt]])
                memT = ph.sb("memT", [128, NCH, N_MEM], BF16)
                memTr = ph.rs(2, "memT")
                self.norm_T(ph, "mn", [(mem[:, mt, :], memr[mt]) for mt in range(2)], gm, gmr, memT, memTr)
                wkv = [ph.sb("wkv%d" % j, [128, NCH, 512], BF16) for j in range(4)]
                wkvr = ph.rs(4, "wkv")
                for j in range(4):
                    kk.dma("pool", wkv[j][:], wblk(self.xa_w_kv[l], 0, NCH, j * 512, 512), writes=[wkvr[j]])
                kn = ph.sb("kn", [128, D], BF16)
                knr = ph.r("kn")
                for mt in range(2):
                    b0 = kk.bank(2)
                    for hf in range(2):
                        for kc in range(NCH):
                            kk.op("pe", lambda e, hf=hf, kc=kc, mt=mt, b0=b0: e.matmul(
                                self.P[:, b0 + hf, :], lhsT=memT[:, kc, mt * 128:(mt + 1) * 128], rhs=wkv[hf][:, kc, :],
                                start=(kc == 0), stop=(kc == NCH - 1)),
                                reads=[memTr[mt], wkvr[hf]], writes=[self.PB[b0 + hf]])
                    self.headnorm(ph, "kh%d" % mt, b0, 4, 256, gk, gkr, kn, knr)
                    b = kk.bank()
                    pv = self.pbb(b)
                    for c in range(NCH):
                        kk.op("pe", lambda e, c=c, pv=pv: e.transpose(out=pv[:, c * 128:(c + 1) * 128], in_=kn[:, c * 128:(c + 1) * 128],
                                                                      identity=self.identb[:]),
                              reads=[knr, self.CR2], writes=[self.PB[b]])
                    kk.op("act", lambda e, mt=mt, pv=pv: e.copy(out=kT[:, :, mt * 128:(mt + 1) * 128],
                                                              in_=pv.rearrange("p (c t) -> p c t", c=NCH)),
                          reads=[self.PB[b]], writes=[kTr])
                    b1 = kk.bank(2)
                    for hf in range(2):
                        for kc in range(NCH):
                            kk.op("pe", lambda e, hf=hf, kc=kc, mt=mt, b1=b1: e.matmul(
                                self.P[:, b1 + hf, :], lhsT=memT[:, kc, mt * 128:(mt + 1) * 128], rhs=wkv[2 + hf][:, kc, :],
                                start=(kc == 0), stop=(kc == NCH - 1)),
                                reads=[memTr[mt], wkvr[2 + hf]], writes=[self.PB[b1 + hf]])
                    kk.op("act", lambda e, mt=mt, b1=b1: e.copy(out=Vt[:, mt, :], in_=self.pb(b1, 2)),
                          reads=[self.PB[b1], self.PB[b1 + 1]], writes=[Vr])
            with Phase(kk) as ph:
                gc, gcr = self.load_gain_bc(ph, "gc", self.norm_cross[l], D)
                gq, gqr = self.load_gain_bc(ph, "gq", self.xa_q_norm[l], 256)
                wq = ph.sb("wq", [128, NCH, D], BF16)
                wqr = ph.r("wq")
                wo = ph.sb("wo", [128, NCH, D], BF16)
                wor = ph.r("wo")
                for hf in range(2):
                    kk.dma("pool", wq[:, :, hf * 512:(hf + 1) * 512], wblk(self.xa_w_q[l], 0, NCH, hf * 512, 512), writes=[wqr])
                for hf in range(2):
                    kk.dma("pool", wo[:, :, hf * 512:(hf + 1) * 512], wblk(self.xa_w_o[l], 0, NCH, hf * 512, 512), writes=[wor])
                hTg = [ph.sb("xh%d" % j, [128, NCH, 512], BF16) for j in range(2)]
                hTgr = [ph.rs(4, "xh%d_" % j) for j in range(2)]
                qTg = [ph.sb("xq%d" % j, [128, NCH, 512], BF16) for j in range(2)]
                qTgr = [ph.rs(4, "xq%d_" % j) for j in range(2)]
                oTg = [ph.sb("xo%d" % j, [128, NCH, 512], BF16) for j in range(2)]
                oTgr = [ph.rs(NCH, "xo%d_" % j) for j in range(2)]
                qn = [ph.sb("xqn%d" % j, [128, D], BF16) for j in range(2)]
                qnr = ph.rs(2, "xqn")
                PT = [ph.sb("xPT%d" % j, [128, 2, 512], BF16) for j in range(2)]
                PTr = ph.rs(2, "xPT")
                rden = [ph.sb("xrd%d" % j, [128, 512], F32) for j in range(2)]
                rdr = ph.rs(2, "xrd")
                pi = 0
                for g in range(NG):
                    hT, hTr = hTg[g % 2], hTgr[g % 2]
                    qT, qTr = qTg[g % 2], qTgr[g % 2]
                    oT, oTr = oTg[g % 2], oTgr[g % 2]
                    self.norm_T(ph, "xn%d_" % g, [(self.X[:, g * 4 + j, :], self.XR[g * 4 + j]) for j in range(4)], gc, gcr, hT, hTr)
                    for j in range(4):
                        b0 = kk.bank(2)
                        for hf in range(2):
                            for kc in range(NCH):
                                kk.op("pe", lambda e, hf=hf, kc=kc, j=j, b0=b0, hT=hT: e.matmul(
                                    self.P[:, b0 + hf, :], lhsT=hT[:, kc, j * 128:(j + 1) * 128], rhs=wq[:, kc, hf * 512:(hf + 1) * 512],
                                    start=(kc == 0), stop=(kc == NCH - 1)),
                                    reads=[hTr[j], wqr], writes=[self.PB[b0 + hf]])
                        q_, q_r = qn[j % 2], qnr[j % 2]
                        self.headnorm(ph, "qh%d_%d" % (g, j), b0, 4, 256, gq, gqr, q_, q_r)
                        b = kk.bank()
                        pv = self.pbb(b)
                        for c in range(NCH):
                            kk.op("pe", lambda e, c=c, pv=pv, q_=q_: e.transpose(out=pv[:, c * 128:(c + 1) * 128],
                                                                              in_=q_[:, c * 128:(c + 1) * 128], identity=self.identb[:]),
                                  reads=[q_r, self.CR2], writes=[self.PB[b]])
                        kk.op("act", lambda e, j=j, pv=pv, qT=qT: e.copy(out=qT[:, :, j * 128:(j + 1) * 128],
                                                                      in_=pv.rearrange("p (c t) -> p c t", c=NCH)),
                              reads=[self.PB[b]], writes=[qTr[j]])
                    for h in range(4):
                        pt, ptr = PT[pi % 2], PTr[pi % 2]
                        rd, rdr_ = rden[pi % 2], rdr[pi % 2]
                        pi += 1
                        b0 = kk.bank(2)
                        for mt in range(2):
                            for hh in range(2):
                                kk.op("pe", lambda e, mt=mt, hh=hh, h=h, b0=b0, qT=qT: e.matmul(
                                    self.P[:, b0 + mt, :], lhsT=kT[:, h * 2 + hh, mt * 128:(mt + 1) * 128], rhs=qT[:, h * 2 + hh, :],
                                    start=(hh == 0), stop=(hh == 1)),
                                    reads=[kTr] + qTr, writes=[self.PB[b0 + mt]])
                        kk.op("act", lambda e, b0=b0, pt=pt: e.activation(out=pt[:].rearrange("p a b -> p (a b)"), in_=self.pb(b0, 2),
                                                                        func=AF.Exp, scale=1.0 / 16.0),
                              reads=[self.PB[b0], self.PB[b0 + 1]], writes=[ptr])
                        bd = kk.bank()
                        for mt in range(2):
                            kk.op("pe", lambda e, mt=mt, bd=bd, pt=pt: e.matmul(self.P[:, bd, :], lhsT=self.onesb[:], rhs=pt[:, mt, :],
                                                                             start=(mt == 0), stop=(mt == 1)),
                                  reads=[ptr, self.CR2], writes=[self.PB[bd]])
                        kk.op("dve", lambda e, bd=bd, rd=rd: e.reciprocal(out=rd[:], in_=self.P[:, bd, :]),
                              reads=[self.PB[bd]], writes=[rdr_])
                        for hh in range(2):
                            bo = kk.bank()
                            for mt in range(2):
                                kk.op("pe", lambda e, mt=mt, bo=bo, pt=pt, h=h, hh=hh: e.matmul(
                                    self.P[:, bo, :], lhsT=Vt[:, mt, h * 256 + hh * 128:h * 256 + (hh + 1) * 128], rhs=pt[:, mt, :],
                                    start=(mt == 0), stop=(mt == 1)),
                                    reads=[ptr, Vr], writes=[self.PB[bo]])
                            kk.op("dve", lambda e, bo=bo, rd=rd, oT=oT, h=h, hh=hh: e.tensor_tensor(
                                out=oT[:, h * 2 + hh, :], in0=self.P[:, bo, :], in1=rd[:], op=ALU.mult),
                                reads=[self.PB[bo], rdr_], writes=[oTr[h * 2 + hh]])
                    for j in range(4):
                        i = g * 4 + j
                        for hf in range(2):
                            b = kk.bank()
                            for c in range(NCH):
                                kk.op("pe", lambda e, c=c, b=b, j=j, hf=hf, oT=oT: e.matmul(
                                    self.P[:, b, :], lhsT=oT[:, c, j * 128:(j + 1) * 128], rhs=wo[:, c, hf * 512:(hf + 1) * 512],
                                    start=(c == 0), stop=(c == NCH - 1)),
                                    reads=[oTr[c], wor], writes=[self.PB[b]])
                            self.resid_add(i, b, hf)

    def odd_mixer(self, s, l):
        kk, S, NT, NG = self.kk, self.S, self.NT, self.NG
        o = l // 2
        NCK = NT * 4
        with Phase(kk) as ph:
            gain, gr = self.load_gain_bc(ph, "og", self.norm_mix[l], D)
            go, gor = self.load_gain_bc(ph, "ogo", self.hgrn_o_norm[o], 128)
            hT = ph.sb("ohT", [128, NCH, S], BF16)
            hTr = ph.rs(NT, "ohT")
            self.norm_T(ph, "on", [(self.X[:, i, :], self.XR[i]) for i in range(NT)], gain, gr, hT, hTr)
            rm = ph.sb("orm", [128, 512], BF16)
            rmr = ph.r("orm")
            kk.op("dve", lambda e: e.memset(rm[:], 1.0), writes=[rmr])
            kk.op("dve", lambda e: e.memset(rm[:, 0:512:32], 0.0), writes=[rmr])
            win = [ph.sb("owin%d" % j, [128, NCH, 512], BF16) for j in range(2)]
            winr = ph.rs(2, "owin")
            wout = [ph.sb("owout%d" % j, [128, D], BF16) for j in range(2)]
            woutr = ph.rs(2, "owout")
            qt = ph.sb("oqt", [128, S], BF16)
            qtr = ph.rs(NG, "oqt")
            kt = ph.sb("okt", [128, S], BF16)
            ktr = ph.rs(NG, "okt")
            khT = ph.sb("okhT", [128, NT, 128], BF16)
            khTr = ph.rs(NT, "okhT")
            vtok = ph.sb("ovt", [128, NT, 128], BF16)
            vtr = ph.rs(NT, "ovt")
            sgt = ph.sb("osg", [128, NT, 128], BF16)
            sgr = ph.rs(NT, "osg")
            adec = ph.sb("oadec", [128, NCK], F32)
            adr = ph.rs(NG, "oadec")
            Sbf = ph.sb("oSbf", [128, NCK, 128], BF16)
            Sbfr = ph.rs(NCK, "oSbf")
            Sst = [ph.sb("oSst%d" % j, [128, 128], F32) for j in range(2)]
            Sstr = ph.rs(2, "oSst")
            T1 = [[ph.sb("ot%d_%d" % (a, j), [128, 512], F32) for j in range(2)] for a in range(6)]
            T1r = [ph.rs(2, "ot%d_" % a) for a in range(6)]
            kh = [ph.sb("okh%d" % j, [128, 512], BF16) for j in range(2)]
            khr = ph.rs(2, "okh")
            vm = [ph.sb("ovm%d" % j, [128, 4, 128], BF16) for j in range(2)]
            vmr = ph.rs(2, "ovm")
            qm = [ph.sb("oqm%d" % j, [128, 4, 128], BF16) for j in range(2)]
            qmr = ph.rs(2, "oqm")
            for j in range(2):
                kk.op("dve", lambda e, j=j: e.memset(qm[j][:], 0.0), writes=[qmr[j]])
            scm = [ph.sb("oscm%d" % j, [128, 128], BF16) for j in range(2)]
            scmr = ph.rs(2, "oscm")
            yf = [ph.sb("oyf%d" % j, [128, 128], F32) for j in range(2)]
            yfr = ph.rs(2, "oyf")
            yb = [ph.sb("oyb%d" % j, [128, 128], BF16) for j in range(2)]
            ybr = ph.rs(2, "oyb")
            yT = [ph.sb("oyT%d" % j, [128, 128], BF16) for j in range(2)]
            yTr = ph.rs(2, "oyT")
            oss = ph.sb("oss", [128, 2], F32)
            ossr = ph.rs(2, "oss")
            ojk = ph.sb("ojk", [128, 128], BF16)
            ojr = ph.r("ojk")
            w_in = self.hgrn_w_in[o]
            t1i = 0
            import os
            STG = int(os.environ.get("ODD_STAGE", "9"))
            for hd in range(int(os.environ.get("ODD_HEADS", "8"))):
                if STG < 1:
                    break
                w, wr = win[hd % 2], winr[hd % 2]
                for j in range(4):
                    kk.dma("pool", w[:, :, j * 128:(j + 1) * 128], wblk(w_in, 0, NCH, j * 1024 + hd * 128, 128), writes=[wr])
                wo_, wor_ = wout[hd % 2], woutr[hd % 2]
                kk.dma("pool", wo_[:], self.hgrn_w_out[o][hd * 128:(hd + 1) * 128, :], writes=[wor_])
                col = l * 8 + hd
                lb = self.lbt[:, col:col + 1]
                oml = self.omlt[:, col:col + 1]
                noml = self.nomlt[:, col:col + 1]
                for g in range(NG):
                    tsl = slice(g * 512, (g + 1) * 512)
                    bq = kk.bank()
                    bf = kk.bank()
                    for (bb, co) in ((bq, 0), (bf, 128)):
                        for kc in range(NCH):
                            kk.op("pe", lambda e, bb=bb, co=co, kc=kc, w=w, tsl=tsl: e.matmul(
                                self.P[:, bb, :], lhsT=w[:, kc, co:co + 128], rhs=hT[:, kc, tsl],
                                start=(kc == 0), stop=(kc == NCH - 1)),
                                reads=[wr] + hTr[g * 4:(g + 1) * 4], writes=[self.PB[bb]])
                    p = t1i % 2
                    t1i += 1
                    sig, lf, kf, bb_, eb, enb = [T1[a][p] for a in range(6)]
                    sigr, lfr, kfr, bbr, ebr, enbr = [T1r[a][p] for a in range(6)]
                    kk.op("act", lambda e, sig=sig, bf=bf: e.activation(out=sig[:], in_=self.P[:, bf, :], func=AF.Sigmoid),
                          reads=[self.PB[bf]], writes=[sigr])
                    kk.op("act", lambda e, sig=sig, lf=lf: e.activation(out=lf[:], in_=sig[:], func=AF.Ln, bias=lb, scale=oml),
                          reads=[sigr, self.LBR], writes=[lfr])
                    kk.op("dve", lambda e, sig=sig, kf=kf: e.tensor_scalar(out=kf[:], in0=sig[:], scalar1=noml, scalar2=oml,
                                                                         op0=ALU.mult, op1=ALU.add),
                          reads=[sigr, self.LBR], writes=[kfr])
                    kk.op("dve", lambda e, lf=lf, bb_=bb_: e.tensor_tensor_scan(out=bb_[:], data0=rm[:], data1=lf[:], initial=0.0,
                                                                               op0=ALU.mult, op1=ALU.add),
                          reads=[lfr, rmr], writes=[bbr])
                    kk.op("act", lambda e, bb_=bb_, eb=eb: e.activation(out=eb[:], in_=bb_[:], func=AF.Exp), reads=[bbr], writes=[ebr])
                    kk.op("act", lambda e, bb_=bb_, enb=enb: e.activation(out=enb[:], in_=bb_[:], func=AF.Exp, scale=-1.0),
                          reads=[bbr], writes=[enbr])
                    kk.op("dve", lambda e, eb=eb, bq=bq, tsl=tsl: e.tensor_tensor(out=qt[:, tsl], in0=self.P[:, bq, :], in1=eb[:], op=ALU.mult),
                          reads=[self.PB[bq], ebr], writes=[qtr[g]])
                    kk.op("dve", lambda e, kf=kf, enb=enb, tsl=tsl: e.tensor_tensor(out=kt[:, tsl], in0=kf[:], in1=enb[:], op=ALU.mult),
                          reads=[kfr, enbr], writes=[ktr[g]])
                    kh_, khr_ = kh[p], khr[p]
                    for cc in range(16):
                        kk.op("dve", lambda e, eb=eb, kh_=kh_, cc=cc, g=g: e.tensor_scalar(
                            out=kh_[:, cc * 32:(cc + 1) * 32], in0=kt[:, g * 512 + cc * 32:g * 512 + (cc + 1) * 32],
                            scalar1=eb[:, cc * 32 + 31:cc * 32 + 32], scalar2=None, op0=ALU.mult),
                            reads=[ktr[g], ebr], writes=[khr_])
                    kk.op("act", lambda e, eb=eb, g=g: e.copy(out=adec[:, g * 16:(g + 1) * 16], in_=eb[:, 31:512:32]),
                          reads=[ebr], writes=[adr[g]])
                    b = kk.bank()
                    pv = self.pbb(b)
                    for j in range(4):
                        kk.op("pe", lambda e, j=j, pv=pv, kh_=kh_: e.transpose(out=pv[:, j * 128:(j + 1) * 128],
                                                                            in_=kh_[:, j * 128:(j + 1) * 128], identity=self.identb[:]),
                              reads=[khr_, self.CR2], writes=[self.PB[b]])
                    kk.op("act", lambda e, pv=pv, g=g: e.copy(out=khT[:, g * 4:(g + 1) * 4, :],
                                                              in_=pv[:, 0:512].rearrange("p (c t) -> p c t", c=4)),
                          reads=[self.PB[b]], writes=khTr[g * 4:(g + 1) * 4])
                if STG < 2:
                    continue
                for i in range(NT):
                    b = kk.bank()
                    for kc in range(NCH):
                        kk.op("pe", lambda e, kc=kc, b=b, i=i, w=w: e.matmul(
                            self.P[:, b, 0:256], lhsT=hT[:, kc, i * 128:(i + 1) * 128], rhs=w[:, kc, 256:512],
                            start=(kc == 0), stop=(kc == NCH - 1)),
                            reads=[wr, hTr[i]], writes=[self.PB[b]])
                    kk.op("act", lambda e, b=b, i=i: e.copy(out=vtok[:, i, :], in_=self.P[:, b, 0:128]),
                          reads=[self.PB[b]], writes=[vtr[i]])
                    kk.op("act", lambda e, b=b, i=i: e.activation(out=sgt[:, i, :], in_=self.P[:, b, 128:256], func=AF.Silu),
                          reads=[self.PB[b]], writes=[sgr[i]])
                if STG < 3:
                    continue
                kk.op("dve", lambda e: e.memset(Sst[0][:], 0.0), writes=[Sstr[0]])
                kk.op("dve", lambda e: e.memset(Sbf[:, 0, :], 0.0), writes=[Sbfr[0]])
                for cj in range(NCK - 1):
                    i, j = divmod(cj, 4)
                    if j == 0:
                        vm_, vm_r = vm[i % 2], vmr[i % 2]
                        for jj in range(4):
                            kk.op("dve", lambda e, i=i, vm_=vm_, jj=jj: e.tensor_scalar(
                                out=vm_[:, jj, :], in0=vtok[:, i, :], scalar1=self.rmask4[:, jj * 128:jj * 128 + 1], scalar2=None, op0=ALU.mult),
                                reads=[vtr[i], self.CR2], writes=[vm_r])
                    bd = kk.bank()
                    kk.op("pe", lambda e, bd=bd, i=i, j=j, vm_=vm_: e.matmul(
                        self.P[:, bd, 0:128], lhsT=khT[:, i, :], rhs=vm_[:, j, :], start=True, stop=True),
                        reads=[khTr[i], vm_r], writes=[self.PB[bd]])
                    kk.op("dve", lambda e, bd=bd, j=j, cj=cj: e.scalar_tensor_tensor(
                        out=Sst[(cj + 1) % 2][:], in0=Sst[cj % 2][:], scalar=adec[:, cj:cj + 1], in1=self.P[:, bd, 0:128],
                        op0=ALU.mult, op1=ALU.add),
                        reads=[Sstr[cj % 2], adr[cj // 16], self.PB[bd]], writes=[Sstr[(cj + 1) % 2]])
                    kk.op("act", lambda e, cj=cj: e.copy(out=Sbf[:, cj + 1, :], in_=Sst[(cj + 1) % 2][:]), reads=[Sstr[(cj + 1) % 2]], writes=[Sbfr[cj + 1]])
                if STG < 4:
                    continue
                bobox = {}

                def odd_X(i):
                    p = i % 2
                    bs = kk.bank()
                    kk.op("pe", lambda e, bs=bs, i=i: e.matmul(self.P[:, bs, 0:128], lhsT=kt[:, i * 128:(i + 1) * 128],
                                                              rhs=qt[:, i * 128:(i + 1) * 128], start=True, stop=True),
                          reads=[ktr[i // 4], qtr[i // 4]], writes=[self.PB[bs]])
                    kk.op("dve", lambda e, bs=bs, p=p: e.tensor_tensor(out=scm[p][:], in0=self.P[:, bs, 0:128], in1=self.hmask[:], op=ALU.mult),
                          reads=[self.PB[bs], self.CR], writes=[scmr[p]])
                    bo = kk.bank()
                    kk.op("pe", lambda e, bo=bo, i=i, p=p: e.matmul(self.P[:, bo, 0:128], lhsT=scm[p][:], rhs=vtok[:, i, :],
                                                                 start=True, stop=False),
                          reads=[scmr[p], vtr[i]], writes=[self.PB[bo]])
                    qm_, qm_r = qm[p], qmr[p]
                    for jj in range(4):
                        kk.op("dve", lambda e, i=i, qm_=qm_, jj=jj: e.tensor_copy(
                            out=qm_[:, jj, jj * 32:(jj + 1) * 32], in_=qt[:, i * 128 + jj * 32:i * 128 + (jj + 1) * 32]),
                            reads=[qtr[i // 4]], writes=[qm_r])
                    for j in range(4):
                        kk.op("pe", lambda e, bo=bo, i=i, j=j, qm_=qm_: e.matmul(
                            self.P[:, bo, 0:128], lhsT=qm_[:, j, :], rhs=Sbf[:, 4 * i + j, :], start=False, stop=(j == 3)),
                            reads=[qm_r, Sbfr[4 * i + j]], writes=[self.PB[bo]])
                    bobox[i] = bo
                    kk.reserved.add(bo)

                def odd_Y(i, wo_=wo_, wor_=wor_):
                    p = i % 2
                    bo = bobox.pop(i)
                    kk.op("act", lambda e, bo=bo, p=p: e.activation(out=ojk[:], in_=self.P[:, bo, 0:128], func=AF.Square,
                                                                    accum_out=oss[:, p:p + 1]),
                          reads=[self.PB[bo]], writes=[ojr, ossr[p]])
                    self.rstd_from_ss(oss[:, p:p + 1], oss[:, p:p + 1], 128, ossr[p])
                    kk.op("dve", lambda e, bo=bo, p=p: e.scalar_tensor_tensor(out=yf[p][:], in0=self.P[:, bo, 0:128], scalar=oss[:, p:p + 1],
                                                                             in1=go[:], op0=ALU.mult, op1=ALU.mult),
                          reads=[self.PB[bo], ossr[p], gor], writes=[yfr[p]])
                    kk.reserved.discard(bo)
                    kk.op("dve", lambda e, p=p, i=i: e.tensor_tensor(out=yb[p][:], in0=yf[p][:], in1=sgt[:, i, :], op=ALU.mult),
                          reads=[yfr[p], sgr[i]], writes=[ybr[p]])
                    bt = kk.bank()
                    pv = self.pbb(bt)
                    kk.op("pe", lambda e, pv=pv, p=p: e.transpose(out=pv[:, 0:128], in_=yb[p][:], identity=self.identb[:]),
                          reads=[ybr[p], self.CR2], writes=[self.PB[bt]])
                    kk.op("act", lambda e, pv=pv, p=p: e.copy(out=yT[p][:], in_=pv[:, 0:128]), reads=[self.PB[bt]], writes=[yTr[p]])
                    for hf in range(2):
                        b = kk.bank()
                        kk.op("pe", lambda e, b=b, p=p, hf=hf, wo_=wo_: e.matmul(self.P[:, b, :], lhsT=yT[p][:], rhs=wo_[:, hf * 512:(hf + 1) * 512],
                                                                              start=True, stop=True),
                              reads=[yTr[p], wor_], writes=[self.PB[b]])
                        self.resid_add(i, b, hf)

                odd_X(0)
                for i in range(NT):
                    if i + 1 < NT:
                        odd_X(i + 1)
                    odd_Y(i)

    def rope_tables(self, ph, s):
        kk, NT = self.kk, self.NT
        n = NT * 32
        posi = ph.sb("posi", [128, NT], I32)
        pr = ph.r("pos")
        with self.nc.allow_non_contiguous_dma(reason="positions token-major gather"):
            kk.dma("sp", posi[:], self.pos_d[s].rearrange("(n p) -> p n", p=128), writes=[pr])
        posf = ph.sb("posf", [128, NT], F32)
        kk.op("dve", lambda e: e.tensor_copy(out=posf[:], in_=posi[:]), reads=[pr], writes=[pr])
        ang = ph.sb("ang", [128, NT, 32], F32)
        for t in range(NT):
            kk.op("dve", lambda e, t=t: e.tensor_scalar(out=ang[:, t, :], in0=self.invf[:], scalar1=posf[:, t:t + 1], scalar2=None,
                                                       op0=ALU.mult),
                  reads=[pr, self.CR], writes=[pr])
        a2 = ang[:].rearrange("p a b -> p (a b)")
        kf = ph.sb("ropek", [128, n], F32)
        r_ = ph.sb("roper", [128, n], F32)
        rc = ph.sb("roperc", [128, n], F32)
        m_ = ph.sb("ropem", [128, n], F32)
        kk.op("dve", lambda e: e.tensor_scalar(out=kf[:], in0=a2, scalar1=1.0 / TWO_PI, scalar2=MAGIC, op0=ALU.mult, op1=ALU.add),
              reads=[pr], writes=[pr])
        kk.op("dve", lambda e: e.tensor_scalar(out=kf[:], in0=kf[:], scalar1=-MAGIC, scalar2=None, op0=ALU.add), reads=[pr], writes=[pr])
        kk.op("dve", lambda e: e.scalar_tensor_tensor(out=r_[:], in0=kf[:], scalar=-CW1, in1=a2, op0=ALU.mult, op1=ALU.add),
              reads=[pr], writes=[pr])
        kk.op("dve", lambda e: e.scalar_tensor_tensor(out=r_[:], in0=kf[:], scalar=-CW2, in1=r_[:], op0=ALU.mult, op1=ALU.add),
              reads=[pr], writes=[pr])
        kk.op("dve", lambda e: e.tensor_scalar(out=rc[:], in0=r_[:], scalar1=0.5 * np.pi, scalar2=None, op0=ALU.add), reads=[pr], writes=[pr])
        kk.op("dve", lambda e: e.tensor_scalar(out=m_[:], in0=rc[:], scalar1=float(np.pi), scalar2=None, op0=ALU.is_gt), reads=[pr], writes=[pr])
        kk.op("dve", lambda e: e.scalar_tensor_tensor(out=rc[:], in0=m_[:], scalar=-TWO_PI, in1=rc[:], op0=ALU.mult, op1=ALU.add),
              reads=[pr], writes=[pr])
        for t_ in (r_, rc):
            kk.op("dve", lambda e, t_=t_: e.tensor_scalar(out=t_[:], in0=t_[:], scalar1=-PI_LO, scalar2=PI_LO, op0=ALU.max, op1=ALU.min),
                  reads=[pr], writes=[pr])
        cos3 = ph.sb("cos3", [128, NT, 3, 32], F32)
        sin3 = ph.sb("sin3", [128, NT, 3, 32], F32)
        tr = ph.r("ropetab")
        kk.op("act", lambda e: e.activation(out=sin3[:, :, 0, :], in_=r_[:].rearrange("p (a b) -> p a b", b=32), func=AF.Sin),
              reads=[pr], writes=[tr])
        kk.op("act", lambda e: e.activation(out=cos3[:, :, 0, :], in_=rc[:].rearrange("p (a b) -> p a b", b=32), func=AF.Sin),
              reads=[pr], writes=[tr])
        for j in (1, 2):
            kk.op("dve", lambda e, j=j: e.tensor_copy(out=sin3[:, :, j, :], in_=sin3[:, :, 0, :]), reads=[tr], writes=[tr])
            kk.op("dve", lambda e, j=j: e.tensor_copy(out=cos3[:, :, j, :], in_=cos3[:, :, 0, :]), reads=[tr], writes=[tr])
        return cos3, sin3, tr

    def outproj_partial(self, i, aT_ap, aT_r, wo_, wor_):
        kk = self.kk
        for hf in range(2):
            b = kk.bank()
            kk.op("pe", lambda e, b=b, hf=hf: e.matmul(self.P[:, b, :], lhsT=aT_ap, rhs=wo_[:, hf * 512:(hf + 1) * 512], start=True, stop=True),
                  reads=[aT_r, wor_], writes=[self.PB[b]])
            self.resid_add(i, b, hf)

    def even_mixer(self, s, l):
        kk, S, NT, NG = self.kk, self.S, self.NT, self.NG
        ei = l // 2
        w_in = self.ab_w_in[ei]
        w_out = self.ab_w_out[ei]
        with Phase(kk) as ph:
            hT = ph.sb("ehT", [128, NCH, S], BF16)
            hTr = ph.rs(NT, "ehT")
            with Phase(kk) as pn:
                gain, gr = self.load_gain_bc(pn, "eg", self.norm_mix[l], D)
                self.norm_T(pn, "en", [(self.X[:, i, :], self.XR[i]) for i in range(NT)], gain, gr, hT, hTr)
            wout = [ph.sb("ewout%d" % j, [128, D], BF16) for j in range(2)]
            woutr = ph.rs(2, "ewout")
            Oc = [ph.sb("eOc%d" % j, [128, 128], BF16) for j in range(2)]
            Ocr = ph.rs(2, "eOc")
            aT = [ph.sb("eaT%d" % j, [128, 128], BF16) for j in range(2)]
            aTr = ph.rs(2, "eaT")
            oi = 0
            with Phase(kk) as pa:
                cos3, sin3, tabr = self.rope_tables(pa, s)
                g3 = pa.sb("g3", [128, 3, 64], F32)
                g3r = pa.r("g3")
                kk.dma("sp", g3[:, 0, :], self.swa_q_norm[ei].partition_broadcast(128), writes=[g3r])
                kk.dma("sp", g3[:, 1, :], self.swa_q_norm[ei].partition_broadcast(128), writes=[g3r])
                kk.dma("sp", g3[:, 2, :], self.swa_k_norm[ei].partition_broadcast(128), writes=[g3r])
                esk = pa.sb("esk", [128, 8], F32)
                eskr = pa.r("esk")
                kk.dma("sp", esk[:], self.swa_sinks[ei].partition_broadcast(128), writes=[eskr])
                kk.op("act", lambda e: e.activation(out=esk[:], in_=esk[:], func=AF.Exp), reads=[eskr], writes=[eskr])
                win = [pa.sb("ewa%d" % j, [128, NCH, 256], BF16) for j in range(2)]
                winr = pa.rs(2, "ewa")
                raw = [pa.sb("eraw%d" % j, [128, 256], F32) for j in range(2)]
                rawr = pa.rs(2, "eraw")
                ss3 = [pa.sb("ess%d" % j, [128, 3], F32) for j in range(2)]
                ss3r = pa.rs(2, "ess")
                jk = pa.sb("ejk", [128, 64], BF16)
                jkr = pa.r("ejk")
                xn = [pa.sb("exn%d" % j, [128, 3, 64], F32) for j in range(2)]
                xnr = pa.rs(2, "exn")
                tt = [pa.sb("ett%d" % j, [128, 4, 3, 32], F32) for j in range(2)]
                ttr = pa.rs(2, "ett")
                rp = [pa.sb("erp%d" % j, [128, 3, 64], F32) for j in range(2)]
                rpr = pa.rs(2, "erp")
                stage = [pa.sb("est%d" % j, [128, 2, 128], BF16) for j in range(2)]
                stager = pa.rs(2, "est")
                vst = [pa.sb("evs%d" % j, [128, 65], BF16) for j in range(3)]
                vstr = pa.rs(3, "evs")
                for j in range(3):
                    kk.op("dve", lambda e, j=j: e.memset(vst[j][:, 64:65], 1.0), writes=[vstr[j]])
                qkT = [pa.sb("eqk%d" % j, [128, 2, 128], BF16) for j in range(3)]
                qkTr = pa.rs(3, "eqk")
                PT = [pa.sb("ePT%d" % j, [128, 512], BF16) for j in range(2)]
                PTr = pa.rs(2, "ePT")
                den = [pa.sb("eden%d" % j, [128, 2], F32) for j in range(2)]
                denr = pa.rs(2, "eden")
                for c in range(4):
                    g = c // 2
                    w, wr = win[c % 2], winr[c % 2]
                    kk.dma("pool", w[:, :, 0:128], wblk(w_in, 0, NCH, c * 128, 128), writes=[wr])
                    kk.dma("pool", w[:, :, 128:192], wblk(w_in, 0, NCH, 512 + g * 64, 64), writes=[wr])
                    kk.dma("pool", w[:, :, 192:256], wblk(w_in, 0, NCH, 640 + g * 64, 64), writes=[wr])
                    wo_, wor_ = wout[oi % 2], woutr[oi % 2]
                    kk.dma("pool", wo_[:], w_out[c * 128:(c + 1) * 128, :], writes=[wor_])
                    def swa_A(i, c=c, g=g, w=w, wr=wr):
                        p2, p3 = i % 2, i % 3
                        b = kk.bank()
                        for kc in range(NCH):
                            kk.op("pe", lambda e, kc=kc, b=b, i=i, w=w: e.matmul(
                                self.P[:, b, 0:256], lhsT=hT[:, kc, i * 128:(i + 1) * 128], rhs=w[:, kc, :],
                                start=(kc == 0), stop=(kc == NCH - 1)),
                                reads=[wr, hTr[i]], writes=[self.PB[b]])
                        rw, rwr = raw[p2], rawr[p2]
                        kk.op("act", lambda e, b=b, rw=rw: e.copy(out=rw[:], in_=self.P[:, b, 0:256]), reads=[self.PB[b]], writes=[rwr])
                        s3, s3r = ss3[p2], ss3r[p2]
                        for h in range(3):
                            kk.op("act", lambda e, h=h, rw=rw, s3=s3: e.activation(out=jk[:], in_=rw[:, h * 64:(h + 1) * 64], func=AF.Square,
                                                                              accum_out=s3[:, h:h + 1]),
                                  reads=[rwr], writes=[jkr, s3r])
                        self.rstd_from_ss(s3[:], s3[:], 64, s3r)
                        x_, x_r = xn[p2], xnr[p2]
                        for h in range(3):
                            kk.op("dve", lambda e, h=h, rw=rw, s3=s3, x_=x_: e.scalar_tensor_tensor(
                                out=x_[:, h, :], in0=rw[:, h * 64:(h + 1) * 64], scalar=s3[:, h:h + 1], in1=g3[:, h, :],
                                op0=ALU.mult, op1=ALU.mult),
                                reads=[rwr, s3r, g3r], writes=[x_r])
                        t_, t_r = tt[p2], ttr[p2]
                        r2, r2r = rp[p2], rpr[p2]
                        x1, x2 = x_[:, :, 0:32], x_[:, :, 32:64]
                        cs_, sn_ = cos3[:, i, :, :], sin3[:, i, :, :]
                        for k_, (a_, b_) in enumerate(((x1, cs_), (x2, sn_), (x2, cs_), (x1, sn_))):
                            kk.op("pool", lambda e, k_=k_, a_=a_, b_=b_, t_=t_: e.tensor_tensor(out=t_[:, k_, :, :], in0=a_, in1=b_, op=ALU.mult),
                                  reads=[x_r, tabr], writes=[t_r])
                        kk.op("pool", lambda e, t_=t_, r2=r2: e.tensor_tensor(out=r2[:, :, 0:32], in0=t_[:, 0, :, :], in1=t_[:, 1, :, :], op=ALU.subtract),
                              reads=[t_r], writes=[r2r])
                        kk.op("pool", lambda e, t_=t_, r2=r2: e.tensor_tensor(out=r2[:, :, 32:64], in0=t_[:, 2, :, :], in1=t_[:, 3, :, :], op=ALU.add),
                              reads=[t_r], writes=[r2r])
                        st_, st_r = stage[p2], stager[p2]
                        kk.op("act", lambda e, st_=st_, r2=r2: e.copy(out=st_[:, 0, :].rearrange("p (a b) -> p a b", a=2), in_=r2[:, 0:2, :]),
                              reads=[r2r], writes=[st_r])
                        kk.op("act", lambda e, st_=st_, r2=r2: e.copy(out=st_[:, 1, 0:64], in_=r2[:, 2, :]), reads=[r2r], writes=[st_r])
                        kk.op("act", lambda e, st_=st_, r2=r2: e.copy(out=st_[:, 1, 64:128], in_=r2[:, 2, :]), reads=[r2r], writes=[st_r])
                        vs, vsr = vst[p3], vstr[p3]
                        kk.op("dve", lambda e, vs=vs, rw=rw: e.tensor_copy(out=vs[:, 0:64], in_=rw[:, 192:256]), reads=[rwr], writes=[vsr])
                        bt = kk.bank()
                        pv = self.pbb(bt)
                        for j in range(2):
                            kk.op("pe", lambda e, j=j, pv=pv, st_=st_: e.transpose(out=pv[:, j * 128:(j + 1) * 128], in_=st_[:, j, :],
                                                                                identity=self.identb[:]),
                                  reads=[st_r, self.CR2], writes=[self.PB[bt]])
                        qk, qkr = qkT[p3], qkTr[p3]
                        kk.op("act", lambda e, pv=pv, qk=qk: e.copy(out=qk[:].rearrange("p a b -> p (a b)"), in_=pv[:, 0:256]),
                              reads=[self.PB[bt]], writes=[qkr])
                    def swa_B(i, c=c, wo_=wo_, wor_=wor_):
                        p2, p3 = i % 2, i % 3
                        qk, qkr = qkT[p3], qkTr[p3]
                        oi = oibox[0]
                        blks = [(1, i)] if i == 0 else [(0, i - 1), (1, i)]
                        bs = kk.bank(2)
                        lo = 128 if i == 0 else 0
                        for (blk, ti) in blks:
                            for hh in range(2):
                                kq, kqr = qkT[ti % 3], qkTr[ti % 3]
                                kk.op("pe", lambda e, blk=blk, hh=hh, kq=kq, qk=qk, bs=bs: e.matmul(
                                    self.P[:, bs + hh, blk * 128:(blk + 1) * 128],
                                    lhsT=kq[hh * 64:(hh + 1) * 64, 1, :], rhs=qk[hh * 64:(hh + 1) * 64, 0, :], start=True, stop=True),
                                    reads=[kqr, qkr], writes=[self.PB[bs + hh]])
                        pt, ptr = PT[p2], PTr[p2]
                        pt3 = pt[:].rearrange("p (a b) -> p a b", a=2)
                        mk3 = self.swamask[:].rearrange("p (a b) -> p a b", a=2)
                        kk.op("act", lambda e, pt3=pt3, bs=bs, lo=lo: e.activation(out=pt3[:, :, lo:256], in_=self.P[:, bs:bs + 2, lo:256], func=AF.Exp, scale=0.125),
                              reads=[self.PB[bs], self.PB[bs + 1]], writes=[ptr])
                        kk.op("dve", lambda e, pt3=pt3, mk3=mk3, lo=lo: e.tensor_tensor(out=pt3[:, :, lo:256], in0=pt3[:, :, lo:256], in1=mk3[:, :, lo:256], op=ALU.mult),
                              reads=[ptr, self.CR2], writes=[ptr])
                        bo = kk.bank()
                        for hh in range(2):
                            for bi, (blk, ti) in enumerate(blks):
                                kk.op("pe", lambda e, hh=hh, blk=blk, ti=ti, bi=bi, bo=bo, pt=pt: e.matmul(
                                    self.P[:, bo, hh * 65:(hh + 1) * 65], lhsT=pt[:, (hh * 2 + blk) * 128:(hh * 2 + blk + 1) * 128],
                                    rhs=vst[ti % 3][:, :], start=(bi == 0), stop=(bi == len(blks) - 1)),
                                    reads=[ptr, vstr[ti % 3]], writes=[self.PB[bo]])
                        dn, dnr = den[p2], denr[p2]
                        kk.op("dve", lambda e, dn=dn, bo=bo, c=c: e.tensor_tensor(out=dn[:], in0=self.P[:, bo, 64:130:65], in1=esk[:, 2 * c:2 * c + 2], op=ALU.add),
                              reads=[self.PB[bo], eskr], writes=[dnr])
                        kk.op("dve", lambda e, dn=dn: e.reciprocal(out=dn[:], in_=dn[:]), reads=[dnr], writes=[dnr])
                        oc, ocr = Oc[oi % 2], Ocr[oi % 2]
                        for hh in range(2):
                            kk.op("dve", lambda e, hh=hh, dn=dn, bo=bo, oc=oc: e.tensor_scalar(
                                out=oc[:, hh * 64:(hh + 1) * 64], in0=self.P[:, bo, hh * 65:hh * 65 + 64], scalar1=dn[:, hh:hh + 1], scalar2=None,
                                op0=ALU.mult),
                                reads=[self.PB[bo], dnr], writes=[ocr])
                        bt2 = kk.bank()
                        pv2 = self.pbb(bt2)
                        kk.op("pe", lambda e, pv2=pv2, oc=oc: e.transpose(out=pv2[:, 0:128], in_=oc[:], identity=self.identb[:]),
                              reads=[ocr, self.CR2], writes=[self.PB[bt2]])
                        at, atr = aT[oi % 2], aTr[oi % 2]
                        kk.op("act", lambda e, pv2=pv2, at=at: e.copy(out=at[:], in_=pv2[:, 0:128]), reads=[self.PB[bt2]], writes=[atr])
                        self.outproj_partial(i, at[:], atr, wo_, wor_)
                        oibox[0] = oi + 1

                    oibox = [oi]
                    swa_A(0)
                    for i in range(NT):
                        if i + 1 < NT:
                            swa_A(i + 1)
                        swa_B(i)
                    oi = oibox[0]
            with Phase(kk) as pb_:
                win = [pb_.sb("ewb%d" % j, [128, NCH, 384], BF16) for j in range(2)]
                winr = pb_.rs(2, "ewb")
                qbT = [pb_.sb("eqb%d" % j, [128, S], BF16) for j in range(2)]
                qbTr = [pb_.rs(NG, "eqb%d_" % j) for j in range(2)]
                kbT = [pb_.sb("ekb%d" % j, [128, S], BF16) for j in range(2)]
                kbTr = [pb_.rs(NG, "ekb%d_" % j) for j in range(2)]
                vb = [pb_.sb("evb%d" % j, [128, NT, 128], BF16) for j in range(2)]
                vbr = [pb_.rs(NT, "evb%d_" % j) for j in range(2)]
                ones = pb_.sb("eones", [128, S], BF16)
                onesr = pb_.r("eones")
                kk.op("dve", lambda e: e.memset(ones[:], 1.0), writes=[onesr])
                E = [pb_.sb("eE%d" % j, [128, S], F32) for j in range(2)]
                Er = pb_.rs(2, "eE")
                SPb = [pb_.sb("eSP%d" % j, [128, S], F32) for j in range(2)]
                SPr = pb_.rs(2, "eSP")
                CS = pb_.sb("eCS", [128, S + 1], F32)
                CSr = pb_.r("eCS")
                kk.op("dve", lambda e: e.memset(CS[:, 0:1], 0.0), writes=[CSr])
                ntot = pb_.sb("ent", [128, 1], F32)
                ntr = pb_.r("ent")
                Ab = [pb_.sb("eA%d" % j, [128, S], BF16) for j in range(2)]
                Abr = pb_.rs(2, "eA")
                AT = [pb_.sb("eAT%d" % j, [128, NT, 128], BF16) for j in range(1)]
                ATr = pb_.rs(1, "eAT")
                for c in range(4):
                    w, wr = win[c % 2], winr[c % 2]
                    kk.dma("pool", w[:, :, 0:128], wblk(w_in, 0, NCH, 768 + c * 128, 128), writes=[wr])
                    kk.dma("pool", w[:, :, 128:256], wblk(w_in, 0, NCH, 1280 + c * 128, 128), writes=[wr])
                    kk.dma("pool", w[:, :, 256:384], wblk(w_in, 0, NCH, 1792 + c * 128, 128), writes=[wr])
                    wo_, wor_ = wout[oi % 2], woutr[oi % 2]
                    kk.dma("pool", wo_[:], w_out[512 + c * 128:512 + (c + 1) * 128, :], writes=[wor_])
                    q_, q_r = qbT[c % 2], qbTr[c % 2]
                    k_, k_r = kbT[c % 2], kbTr[c % 2]
                    v_, v_r = vb[c % 2], vbr[c % 2]
                    for g in range(NG):
                        tsl = slice(g * 512, (g + 1) * 512)
                        for (dst, dstr, co) in ((q_, q_r, 0), (k_, k_r, 128)):
                            bb = kk.bank()
                            for kc in range(NCH):
                                kk.op("pe", lambda e, bb=bb, co=co, kc=kc, w=w, tsl=tsl: e.matmul(
                                    self.P[:, bb, :], lhsT=w[:, kc, co:co + 128], rhs=hT[:, kc, tsl],
                                    start=(kc == 0), stop=(kc == NCH - 1)),
                                    reads=[wr] + hTr[g * 4:(g + 1) * 4], writes=[self.PB[bb]])
                            kk.op("act", lambda e, bb=bb, dst=dst, tsl=tsl: e.copy(out=dst[:, tsl], in_=self.P[:, bb, :]),
                                  reads=[self.PB[bb]], writes=[dstr[g]])
                    for i in range(NT):
                        if i % 4 == 0:
                            bv = kk.bank()
                        for kc in range(NCH):
                            kk.op("pe", lambda e, kc=kc, bv=bv, i=i, w=w: e.matmul(
                                self.P[:, bv, (i % 4) * 128:(i % 4 + 1) * 128], lhsT=hT[:, kc, i * 128:(i + 1) * 128], rhs=w[:, kc, 256:384],
                                start=(kc == 0), stop=(kc == NCH - 1)),
                                reads=[wr, hTr[i]], writes=[self.PB[bv]])
                        kk.op("dve", lambda e, bv=bv, i=i, v_=v_: e.tensor_copy(out=v_[:, i, :], in_=self.P[:, bv, (i % 4) * 128:(i % 4 + 1) * 128]),
                              reads=[self.PB[bv]], writes=[v_r[i]])
                    its = [(n, hh) for n in range(NT) for hh in range(2)]
                    state = {}

                    def stA(k, c=c, q_=q_, q_r=q_r, k_=k_, k_r=k_r):
                        n, hh = its[k]
                        L = (n + 1) * 128
                        nb = 1 if L <= 512 else (2 if L <= 1024 else 4)
                        ps_ = slice(hh * 64, (hh + 1) * 64)
                        bz = kk.bank(nb)
                        Z = self.pb(bz, nb)
                        zr = [self.PB[bz + j] for j in range(nb)]
                        nj = (L + 511) // 512
                        for j in range(nj):
                            c0, c1 = j * 512, min(L, (j + 1) * 512)
                            kk.op("pe", lambda e, c0=c0, c1=c1: e.matmul(
                                Z[:, c0:c1], lhsT=q_[ps_, n * 128:(n + 1) * 128], rhs=k_[ps_, c0:c1], start=True, stop=True),
                                reads=[q_r[n // 4], k_r[j]], writes=[self.PB[bz + j]])
                        gi = state.setdefault("gi", 0)
                        state["gi"] = gi + 1
                        e_, e_r = E[gi % 2], Er[gi % 2]
                        sp_, sp_r = SPb[gi % 2], SPr[gi % 2]
                        ab, abr = Ab[gi % 2], Abr[gi % 2]
                        state[k] = (L, e_, e_r, sp_, sp_r, ab, abr)
                        kk.op("act", lambda e: e.activation(out=e_[:, 0:L], in_=Z[:, 0:L], func=AF.Exp, scale=0.125),
                              reads=zr[0:nj], writes=[e_r])
                        kk.op("dve", lambda e: e.tensor_tensor(out=e_[:, L - 128:L], in0=e_[:, L - 128:L], in1=self.sbmask[:], op=ALU.mult),
                              reads=[e_r, self.CR], writes=[e_r])
                        kk.op("act", lambda e: e.activation(out=sp_[:, 0:L], in_=e_[:, 0:L], func=AF.Ln, bias=1.0, scale=1.0),
                              reads=[e_r], writes=[sp_r])

                    def stB(k):
                        L, e_, e_r, sp_, sp_r, ab, abr = state[k]
                        kk.op("dve", lambda e: e.tensor_tensor_scan(out=CS[:, 1:L + 1], data0=ones[:, 0:L], data1=sp_[:, 0:L], initial=0.0,
                                                                     op0=ALU.mult, op1=ALU.add),
                              reads=[sp_r, onesr], writes=[CSr])
                        kk.op("dve", lambda e: e.tensor_scalar(out=ntot[:], in0=CS[:, L:L + 1], scalar1=-1.0, scalar2=None, op0=ALU.mult),
                              reads=[CSr], writes=[ntr])
                        kk.op("act", lambda e: e.activation(out=sp_[:, 0:L], in_=CS[:, 0:L], func=AF.Exp, bias=ntot[:, 0:1], scale=1.0),
                              reads=[CSr, ntr], writes=[sp_r])
                        kk.op("pool", lambda e: e.tensor_tensor(out=ab[:, 0:L], in0=e_[:, 0:L], in1=sp_[:, 0:L], op=ALU.mult),
                              reads=[e_r, sp_r], writes=[abr])

                    def stC(k, c=c, v_=v_, v_r=v_r, wo_=wo_, wor_=wor_):
                        n, hh = its[k]
                        L, e_, e_r, sp_, sp_r, ab, abr = state.pop(k)
                        if hh == 0:
                            state["bo"] = kk.bank()
                            kk.reserved.add(state["bo"])
                        bo = state["bo"]
                        at_, at_r = AT[0], ATr[0]
                        for k0 in range(0, n + 1, 8):
                            k1 = min(n + 1, k0 + 8)
                            bt = kk.bank()
                            while bt == bo:
                                bt = kk.bank()
                            pv = self.pbb(bt)
                            for kb in range(k0, k1):
                                kk.op("pe", lambda e, kb=kb, k0=k0, pv=pv: e.transpose(
                                    out=pv[:, (kb - k0) * 128:(kb - k0 + 1) * 128], in_=ab[:, kb * 128:(kb + 1) * 128], identity=self.identb[:]),
                                    reads=[abr, self.CR2], writes=[self.PB[bt]])
                            kk.op("act", lambda e, k0=k0, k1=k1, pv=pv: e.copy(
                                out=at_[:, k0:k1, :], in_=pv[:, 0:(k1 - k0) * 128].rearrange("p (a b) -> p a b", b=128)),
                                reads=[self.PB[bt]], writes=[at_r])
                        for kb in range(n + 1):
                            kk.op("pe", lambda e, kb=kb: e.matmul(
                                self.P[:, bo, hh * 64:(hh + 1) * 64], lhsT=at_[:, kb, :], rhs=v_[:, kb, hh * 64:(hh + 1) * 64],
                                start=(kb == 0), stop=(kb == n)),
                                reads=[at_r, v_r[kb]], writes=[self.PB[bo]])
                        if hh == 1:
                            oi = state["oi"]
                            oc, ocr = Oc[oi % 2], Ocr[oi % 2]
                            kk.op("dve", lambda e: e.tensor_copy(out=oc[:], in_=self.P[:, bo, 0:128]), reads=[self.PB[bo]], writes=[ocr])
                            kk.reserved.discard(bo)
                            bt2 = kk.bank()
                            pv2 = self.pbb(bt2)
                            kk.op("pe", lambda e: e.transpose(out=pv2[:, 0:128], in_=oc[:], identity=self.identb[:]),
                                  reads=[ocr, self.CR2], writes=[self.PB[bt2]])
                            at, atr = aT[oi % 2], aTr[oi % 2]
                            kk.op("act", lambda e: e.copy(out=at[:], in_=pv2[:, 0:128]), reads=[self.PB[bt2]], writes=[atr])
                            self.outproj_partial(n, at[:], atr, wo_, wor_)
                            state["oi"] = oi + 1

                    state["oi"] = oi
                    NI = len(its)
                    for t in range(NI + 2):
                        if 0 <= t - 2 < NI:
                            stC(t - 2)
                        if 0 <= t - 1 < NI:
                            stB(t - 1)
                        if t < NI:
                            stA(t)
                    oi = state["oi"]


def host_consts():
    p = np.arange(128)[:, None]
    i = np.arange(128)[None, :]
    ident = np.eye(128, dtype=np.float32)
    mprev = (i < p).astype(np.float32)
    mcur = (i >= p).astype(np.float32)
    swamask = np.stack([mprev, mcur, mprev, mcur], axis=1).reshape(128, 512).astype(np.float32)
    sbmask = (p > i).astype(np.float32)
    hmask = ((i >= p) & ((i // 32) == (p // 32))).astype(np.float32)
    invf = (10000.0 ** (-np.arange(0, 64, 2, dtype=np.float32) / np.float32(64))).astype(np.float32)
    invf = np.broadcast_to(invf[None, :], (128, 32)).copy()
    j4 = np.arange(4)[None, :, None]
    t4 = np.arange(128)[None, None, :]
    p4 = np.arange(128)[:, None, None]
    cmask4 = np.broadcast_to((t4 // 32) == j4, (128, 4, 128)).astype(np.float32).reshape(128, 512)
    rmask4 = np.broadcast_to((p4 // 32) == j4, (128, 4, 128)).astype(np.float32).reshape(128, 512)
    return {"c_ident": ident, "c_swamask": swamask, "c_sbmask": sbmask, "c_hmask": hmask, "c_invf": invf,
            "c_cmask4": np.ascontiguousarray(cmask4), "c_rmask4": np.ascontiguousarray(rmask4)}


_PROG = {}


def kernel(**inputs):
    n = 8
    key = "full"
    if key not in _PROG:
        _PROG[key] = Prog()
    prog = _PROG[key]
    consts = host_consts()
    per = 32 // n
    in_maps = []
    for c in range(n):
        m = {}
        for k, v in inputs.items():
            v = np.asarray(v)
            if k in ("x", "mem", "positions"):
                m[k] = np.ascontiguousarray(v[c * per:(c + 1) * per])
            else:
                m[k] = np.ascontiguousarray(v)
        m.update(consts)
        in_maps.append(m)
    res = run_bass_kernel_spmd(prog.nc, in_maps, core_ids=list(range(n)))
    return np.concatenate([r["out"] for r in res.results], axis=0).astype(np.float32)
```

```python
import contextlib
import numpy as np
import concourse.bass as bass
import concourse.mybir as mybir
from concourse.bass_utils import run_bass_kernel_spmd

F32 = mybir.dt.float32
BF16 = mybir.dt.bfloat16
I32 = mybir.dt.int32
AF = mybir.ActivationFunctionType
ALU = mybir.AluOpType
AX = mybir.AxisListType

D = 1024
NCH = 8
DFF = 2816
NFC = 22
EPS = 1e-6
N_MEM = 256
TWO_PI = 6.283185307179586
CW1 = 6.28125
CW2 = TWO_PI - 6.28125
MAGIC = 12582912.0
PI_LO = 3.1415925


class Res:
    __slots__ = ("name", "w", "rd", "dsem", "excl")

    def __init__(self, name="", excl=False):
        self.name = name
        self.w = None
        self.rd = {}
        self.dsem = None
        self.excl = excl


class DSem:
    __slots__ = ("h", "cnt", "key")

    def __init__(self, h, key):
        self.h = h
        self.cnt = 0
        self.key = key


class K:
    def __init__(self, nc, es):
        self.nc = nc
        self.es = es
        self.eng = {"pe": nc.tensor, "act": nc.scalar, "dve": nc.vector, "pool": nc.gpsimd, "sp": nc.sync}
        self.sem = {}
        self.semobj = {}
        for k in ("pe", "act", "dve", "pool"):
            h = es.enter_context(nc.semaphore("s_" + k))
            self.sem[k] = h
            self.semobj[k] = h
        self.cnt = {k: 0 for k in self.eng}
        self.seen = {k: {} for k in self.eng}
        self.free_dsems = {"sp": [], "pool": []}
        self.ndsem = 0
        self.bank_rr = 0
        self.reserved = set()
        self.n_ins = 0

    def _dsem(self, q):
        fl = self.free_dsems[q]
        if fl:
            return fl.pop()
        key = "d%d" % self.ndsem
        self.ndsem += 1
        h = self.es.enter_context(self.nc.semaphore(key))
        ds = DSem(h, key)
        self.semobj[key] = h
        return ds

    def release(self, q, res_list):
        for r in res_list:
            if r.dsem is not None and r.dsem[0] == q:
                self.free_dsems[q].append(r.dsem[1])
                r.dsem = None

    def _waits(self, e, reads, writes):
        raw = {}
        oth = {}
        for r in reads:
            if r.w is not None:
                k, v = r.w
                if raw.get(k, 0) < v:
                    raw[k] = v
            if r.excl:
                for k, v in r.rd.items():
                    if k != e and oth.get(k, 0) < v:
                        oth[k] = v
        for w in writes:
            if w.w is not None:
                k, v = w.w
                if oth.get(k, 0) < v:
                    oth[k] = v
            for k, v in w.rd.items():
                if oth.get(k, 0) < v:
                    oth[k] = v
        deps = dict(raw)
        for k, v in oth.items():
            if k == e and e == "pe":
                continue
            if deps.get(k, 0) < v:
                deps[k] = v
        if e == "pe":
            deps.pop("pe", None)
        eng = self.eng[e]
        seen = self.seen[e]
        for k, v in deps.items():
            if seen.get(k, 0) >= v:
                continue
            eng.wait_ge(self.semobj[k], v)
            seen[k] = v
            self.n_ins += 1

    def op(self, e, fn, reads=(), writes=()):
        self._waits(e, reads, writes)
        ins = fn(self.eng[e])
        self.cnt[e] += 1
        c = self.cnt[e]
        ins.then_inc(self.sem[e], 1)
        self.n_ins += 1
        for r in reads:
            if r.rd.get(e, 0) < c:
                r.rd[e] = c
        for w in writes:
            w.w = (e, c)
            w.rd = {}
        return ins

    def dma(self, q, out, in_, reads=(), writes=(), owner=None):
        self._waits(q, reads, writes)
        if owner is None:
            owner = writes[0] if writes else reads[0]
        if owner.dsem is None or owner.dsem[0] != q:
            assert owner.dsem is None
            owner.dsem = (q, self._dsem(q))
        ds = owner.dsem[1]
        ins = self.eng[q].dma_start(out=out, in_=in_)
        ins.then_inc(ds.h, 16)
        ds.cnt += 16
        self.n_ins += 1
        tok = (ds.key, ds.cnt)
        for r in reads:
            if r.rd.get(ds.key, 0) < ds.cnt:
                r.rd[ds.key] = ds.cnt
        for w in writes:
            w.w = tok
            w.rd = {}
        return ins

    def barrier(self):
        ce = ("pe", "act", "dve", "pool")
        for a in ce + ("sp",):
            for b in ce:
                if a == b:
                    continue
                v = self.cnt[b]
                if v and self.seen[a].get(b, 0) < v:
                    self.eng[a].wait_ge(self.sem[b], v)
                    self.seen[a][b] = v
                    self.n_ins += 1

    def wait_all(self, e, res_list):
        self._waits(e, [], res_list)

    def bank(self, k=1):
        for _ in range(16):
            b = self.bank_rr
            if b % k:
                b += k - (b % k)
            if b + k > 8:
                b = 0
            self.bank_rr = (b + k) % 8
            if not any((b + j) in self.reserved for j in range(k)):
                return b
        raise RuntimeError("no free PSUM bank")


class Phase:
    uid = 0

    def __init__(self, kk):
        self.kk = kk
        self.es = contextlib.ExitStack()
        self.res = []

    def __enter__(self):
        self.es.__enter__()
        return self

    def sb(self, name, shape, dtype):
        Phase.uid += 1
        return self.es.enter_context(self.kk.nc.sbuf_tensor("%s_u%d" % (name, Phase.uid), list(shape), dtype))

    def r(self, name=""):
        x = Res(name)
        self.res.append(x)
        return x

    def rs(self, n, name=""):
        return [self.r(name + str(i)) for i in range(n)]

    def __exit__(self, *a):
        kk = self.kk
        kk.barrier()
        for q in ("sp", "pool"):
            kk.release(q, self.res)
        return self.es.__exit__(*a)


def wblk(w2d, kc0, nkc, c0, ncols):
    return w2d.rearrange("(kc p) n -> p kc n", p=128)[:, kc0:kc0 + nkc, c0:c0 + ncols]


class Prog:
    def __init__(self, S=2048, NSEQ=4, layers=(0, 1, 2, 3), dbg=False, parts=("mix", "xa", "ffn")):
        self.S = S
        self.NT = S // 128
        self.NG = S // 512
        self.NSEQ = NSEQ
        self.layers = layers
        self.dbg = dbg
        self.parts = parts
        self.nc = bass.Bass("TRN2", target_bir_lowering=False)
        self.build()

    def din(self, name, shape, dt=F32):
        return self.nc.dram_tensor(name, list(shape), dt, kind="ExternalInput").ap()

    def build(self):
        nc, S, NT, NSEQ = self.nc, self.S, self.NT, self.NSEQ
        self.x_d = self.din("x", [NSEQ, S, D])
        self.mem_d = self.din("mem", [NSEQ, N_MEM, D])
        self.pos_d = self.din("positions", [NSEQ, S], I32)
        self.norm_mix = self.din("norm_mix", [4, D])
        self.norm_cross = self.din("norm_cross", [4, D])
        self.norm_mem = self.din("norm_mem", [4, D])
        self.norm_ffn = self.din("norm_ffn", [4, D])
        self.ab_w_in = self.din("ab_w_in", [2, D, 2304])
        self.ab_w_out = self.din("ab_w_out", [2, D, D])
        self.swa_q_norm = self.din("swa_q_norm", [2, 64])
        self.swa_k_norm = self.din("swa_k_norm", [2, 64])
        self.swa_sinks = self.din("swa_sinks", [2, 8])
        self.hgrn_w_in = self.din("hgrn_w_in", [2, D, 4096])
        self.hgrn_w_out = self.din("hgrn_w_out", [2, D, D])
        self.hgrn_o_norm = self.din("hgrn_o_norm", [2, 128])
        self.hgrn_lb = self.din("hgrn_lb", [4, D])
        self.xa_w_q = self.din("xa_w_q", [4, D, D])
        self.xa_w_kv = self.din("xa_w_kv", [4, D, 2 * D])
        self.xa_w_o = self.din("xa_w_o", [4, D, D])
        self.xa_q_norm = self.din("xa_q_norm", [4, 256])
        self.xa_k_norm = self.din("xa_k_norm", [4, 256])
        self.ffn_w_up = self.din("ffn_w_up", [4, D, 2 * DFF])
        self.ffn_conv_w = self.din("ffn_conv_w", [4, 3, 2 * DFF])
        self.ffn_conv_b = self.din("ffn_conv_b", [4, 2 * DFF])
        self.ffn_w_down = self.din("ffn_w_down", [4, DFF, D])
        self.c_ident = self.din("c_ident", [128, 128])
        self.c_swamask = self.din("c_swamask", [128, 512])
        self.c_sbmask = self.din("c_sbmask", [128, 128])
        self.c_hmask = self.din("c_hmask", [128, 128])
        self.c_invf = self.din("c_invf", [128, 32])
        self.c_cmask4 = self.din("c_cmask4", [128, 512])
        self.c_rmask4 = self.din("c_rmask4", [128, 512])
        self.out_d = nc.dram_tensor("out", [NSEQ, S, D], F32, kind="ExternalOutput").ap()
        if self.dbg:
            self.dbg_d = nc.dram_tensor("dbg", [16, S, D], F32, kind="ExternalOutput").ap()
            self.ndbg = 0

        with contextlib.ExitStack() as es:
            self.es = es
            kk = self.kk = K(nc, es)
            sb = lambda name, shape, dt: es.enter_context(nc.sbuf_tensor(name, list(shape), dt))
            self.X = sb("X", [128, NT, D], F32)
            self.XR = [Res("X%d" % i) for i in range(NT)]
            self.P = es.enter_context(nc.psum_tensor("P", [128, 8, 512], F32))
            self.PB = [Res("B%d" % i, excl=True) for i in range(8)]
            self.identf = sb("identf", [128, 128], F32)
            self.identb = sb("identb", [128, 128], BF16)
            self.onesb = sb("onesb", [128, 128], BF16)
            self.swamask = sb("swamask", [128, 512], BF16)
            self.sbmask = sb("sbmask", [128, 128], F32)
            self.hmask = sb("hmask", [128, 128], F32)
            self.invf = sb("invf", [128, 32], F32)
            self.lbt = sb("lbt", [128, 32], F32)
            self.omlt = sb("omlt", [128, 32], F32)
            self.nomlt = sb("nomlt", [128, 32], F32)
            self.CR = Res("consts")
            cr = [self.CR]
            kk.dma("sp", self.identf[:], self.c_ident, writes=cr)
            kk.dma("sp", self.sbmask[:], self.c_sbmask, writes=cr)
            kk.dma("sp", self.hmask[:], self.c_hmask, writes=cr)
            kk.dma("sp", self.invf[:], self.c_invf, writes=cr)
            self.CR2 = Res("consts2")
            kk.dma("pool", self.identb[:], self.c_ident, writes=[self.CR2])
            kk.dma("pool", self.swamask[:], self.c_swamask, writes=[self.CR2])
            self.cmask4 = sb("cmask4", [128, 512], BF16)
            self.rmask4 = sb("rmask4", [128, 512], BF16)
            kk.dma("pool", self.cmask4[:], self.c_cmask4, writes=[self.CR2])
            kk.dma("pool", self.rmask4[:], self.c_rmask4, writes=[self.CR2])
            kk.op("dve", lambda e: e.memset(self.onesb[:], 1.0), writes=[self.CR2])
            self.setup_lb()
            for s in range(NSEQ):
                self.run_seq(s)
            kk.wait_all("sp", self.XR)
            kk.barrier()

    def pb(self, b, k=1):
        if k == 1:
            return self.P[:, b, :]
        return self.P[:, b:b + k, :].rearrange("p b f -> p (b f)")

    def pbb(self, b):
        return self.P[:, b, :].bitcast(BF16)

    def setup_lb(self):
        kk = self.kk
        with Phase(kk) as ph:
            raw = ph.sb("lbraw", [32, 128], F32)
            rr = ph.r("lbraw")
            kk.dma("sp", raw[:], self.hgrn_lb.rearrange("l (c p) -> (l c) p", p=128), writes=[rr])
            b = kk.bank()
            kk.op("pe", lambda e: e.transpose(out=self.P[:, b, 0:32], in_=raw[:], identity=self.identf[0:32, 0:32]),
                  reads=[rr, self.CR], writes=[self.PB[b]])
            xs = ph.sb("lbx", [128, 32], F32)
            r2 = ph.r("lbx")
            kk.op("act", lambda e: e.copy(out=xs[:], in_=self.P[:, b, 0:32]), reads=[self.PB[b]], writes=[r2])
            mx = ph.sb("lbmx", [128, 8], F32)
            kk.op("dve", lambda e: e.tensor_max(out=mx[:], in0=xs[:, 0:8], in1=xs[:, 8:16]), reads=[r2], writes=[r2])
            kk.op("dve", lambda e: e.tensor_max(out=mx[:], in0=mx[:], in1=xs[:, 16:24]), reads=[r2], writes=[r2])
            kk.op("dve", lambda e: e.tensor_max(out=mx[:], in0=mx[:], in1=xs[:, 24:32]), reads=[r2], writes=[r2])
            ex = ph.sb("lbex", [128, 32], F32)
            for l in range(4):
                kk.op("dve", lambda e, l=l: e.tensor_sub(out=ex[:, l * 8:(l + 1) * 8], in0=xs[:, l * 8:(l + 1) * 8], in1=mx[:]),
                      reads=[r2], writes=[r2])
            kk.op("act", lambda e: e.activation(out=ex[:], in_=ex[:], func=AF.Exp), reads=[r2], writes=[r2])
            sm = ph.sb("lbsm", [128, 8], F32)
            kk.op("dve", lambda e: e.tensor_add(out=sm[:], in0=ex[:, 0:8], in1=ex[:, 8:16]), reads=[r2], writes=[r2])
            kk.op("dve", lambda e: e.tensor_add(out=sm[:], in0=sm[:], in1=ex[:, 16:24]), reads=[r2], writes=[r2])
            kk.op("dve", lambda e: e.tensor_add(out=sm[:], in0=sm[:], in1=ex[:, 24:32]), reads=[r2], writes=[r2])
            kk.op("dve", lambda e: e.reciprocal(out=sm[:], in_=sm[:]), reads=[r2], writes=[r2])
            for l in range(4):
                kk.op("dve", lambda e, l=l: e.tensor_mul(out=ex[:, l * 8:(l + 1) * 8], in0=ex[:, l * 8:(l + 1) * 8], in1=sm[:]),
                      reads=[r2], writes=[r2])
            lr = self.LBR = Res("lb")
            kk.op("dve", lambda e: e.memset(self.lbt[:, 0:8], 0.0), reads=[r2], writes=[lr])
            kk.op("dve", lambda e: e.tensor_copy(out=self.lbt[:, 8:16], in_=ex[:, 8:16]), reads=[r2, lr], writes=[lr])
            kk.op("dve", lambda e: e.tensor_add(out=self.lbt[:, 16:24], in0=self.lbt[:, 8:16], in1=ex[:, 16:24]), reads=[r2, lr], writes=[lr])
            kk.op("dve", lambda e: e.tensor_add(out=self.lbt[:, 24:32], in0=self.lbt[:, 16:24], in1=ex[:, 24:32]), reads=[r2, lr], writes=[lr])
            kk.op("dve", lambda e: e.tensor_scalar(out=self.omlt[:], in0=self.lbt[:], scalar1=-1.0, scalar2=1.0, op0=ALU.mult, op1=ALU.add),
                  reads=[lr], writes=[lr])
            kk.op("dve", lambda e: e.tensor_scalar(out=self.nomlt[:], in0=self.lbt[:], scalar1=1.0, scalar2=-1.0, op0=ALU.mult, op1=ALU.add),
                  reads=[lr], writes=[lr])

    def load_gain_bc(self, ph, name, row_ap, n):
        t = ph.sb(name, [128, n], F32)
        r = ph.r(name)
        self.kk.dma("sp", t[:], row_ap.partition_broadcast(128), writes=[r])
        return t, r

    def rstd_from_ss(self, ss_ap, rstd_ap, n, res):
        kk = self.kk
        kk.op("act", lambda e: e.activation(out=rstd_ap, in_=ss_ap, func=AF.Ln, bias=self.epsb[:, 0:1], scale=1.0 / n),
              reads=[res, self.CR3], writes=[res])
        kk.op("act", lambda e: e.activation(out=rstd_ap, in_=rstd_ap, func=AF.Exp, scale=-0.5), reads=[res], writes=[res])

    def norm_T(self, ph, tag, srcs, gain, gain_r, dstT, dst_rs, col0=0):
        kk = self.kk
        n = len(srcs)
        ss = ph.sb(tag + "ss", [128, n], F32)
        ssr = ph.r(tag + "ss")
        junk = ph.sb(tag + "junk", [128, D], BF16)
        jr = ph.r(tag + "junk")
        hn = [ph.sb(tag + "hn%d" % j, [128, D], BF16) for j in range(2)]
        hnr = ph.rs(2, tag + "hn")
        for i, (ap, r) in enumerate(srcs):
            kk.op("act", lambda e, ap=ap, i=i: e.activation(out=junk[:], in_=ap, func=AF.Square, accum_out=ss[:, i:i + 1]),
                  reads=[r], writes=[jr, ssr])
        self.rstd_from_ss(ss[:], ss[:], D, ssr)
        for i, (ap, r) in enumerate(srcs):
            h, hr = hn[i % 2], hnr[i % 2]
            kk.op("dve", lambda e, ap=ap, i=i, h=h: e.scalar_tensor_tensor(out=h[:], in0=ap, scalar=ss[:, i:i + 1], in1=gain[:],
                                                                         op0=ALU.mult, op1=ALU.mult),
                  reads=[r, ssr, gain_r], writes=[hr])
            b = kk.bank()
            pv = self.pbb(b)
            for c in range(NCH):
                kk.op("pe", lambda e, c=c, h=h, pv=pv: e.transpose(out=pv[:, c * 128:(c + 1) * 128], in_=h[:, c * 128:(c + 1) * 128],
                                                                  identity=self.identb[:]),
                      reads=[hr, self.CR2], writes=[self.PB[b]])
            kk.op("act", lambda e, i=i, pv=pv: e.copy(out=dstT[:, :, col0 + i * 128: col0 + (i + 1) * 128],
                                                      in_=pv.rearrange("p (c t) -> p c t", c=NCH)),
                  reads=[self.PB[b]], writes=[dst_rs[i]])

    def resid_add(self, i, b, hf):
        kk = self.kk
        xs = self.X[:, i, hf * 512:(hf + 1) * 512]
        kk.op("dve", lambda e: e.tensor_tensor(out=xs, in0=xs, in1=self.P[:, b, :], op=ALU.add),
              reads=[self.PB[b], self.XR[i]], writes=[self.XR[i]])

    def dump(self):
        if not self.dbg:
            return
        k = self.ndbg
        self.ndbg += 1
        for i in range(self.NT):
            self.kk.dma("sp", self.dbg_d[k, i * 128:(i + 1) * 128, :], self.X[:, i, :], reads=[self.XR[i]], owner=self.XR[i])

    def run_seq(self, s):
        kk, NT = self.kk, self.NT
        if s == 0:
            self.epsb = self.es.enter_context(self.nc.sbuf_tensor("epsb", [128, 1], F32))
            self.CR3 = Res("eps")
            kk.op("dve", lambda e: e.memset(self.epsb[:], EPS), writes=[self.CR3])
        for i in range(NT):
            kk.dma("sp", self.X[:, i, :], self.x_d[s, i * 128:(i + 1) * 128, :], writes=[self.XR[i]])
        for l in self.layers:
            if "mix" in self.parts:
                if l % 2 == 0:
                    self.even_mixer(s, l)
                else:
                    self.odd_mixer(s, l)
                self.dump()
            if "xa" in self.parts:
                self.xattn(s, l)
                self.dump()
            if "ffn" in self.parts:
                self.ffn(s, l)
                self.dump()
        for i in range(NT):
            kk.dma("sp", self.out_d[s, i * 128:(i + 1) * 128, :], self.X[:, i, :], reads=[self.XR[i]], owner=self.XR[i])

    def ffn(self, s, l):
        kk, S, NT, NG = self.kk, self.S, self.NT, self.NG
        with Phase(kk) as ph:
            gain, gr = self.load_gain_bc(ph, "fg", self.norm_ffn[l], D)
            hT = ph.sb("fhT", [128, NCH, S], BF16)
            hTr = ph.rs(NT, "fhT")
            self.norm_T(ph, "fn", [(self.X[:, i, :], self.XR[i]) for i in range(NT)], gain, gr, hT, hTr)
            cst = ph.sb("cst", [128, 3, 128], F32)
            cstr = ph.r("cst")
            cw = self.ffn_conv_w[l].rearrange("t (c p) -> (t c) p", p=128)
            cb = self.ffn_conv_b[l].rearrange("(c p) -> c p", p=128)
            kk.dma("sp", cst[:, 0, :], cw[0:128, :], writes=[cstr])
            kk.dma("sp", cst[0:4, 1, :], cw[128:132, :], writes=[cstr])
            kk.dma("sp", cst[0:44, 2, :], cb, writes=[cstr])
            cwb = ph.sb("cwb", [128, 176], F32)
            cwr = ph.r("cwb")
            b = kk.bank()
            kk.op("pe", lambda e: e.transpose(out=self.P[:, b, 0:128], in_=cst[:, 0, :], identity=self.identf[:]),
                  reads=[cstr, self.CR], writes=[self.PB[b]])
            kk.op("pe", lambda e: e.transpose(out=self.P[:, b, 128:132], in_=cst[0:4, 1, :], identity=self.identf[0:4, 0:4]),
                  reads=[cstr, self.CR], writes=[self.PB[b]])
            kk.op("pe", lambda e: e.transpose(out=self.P[:, b, 132:176], in_=cst[0:44, 2, :], identity=self.identf[0:44, 0:44]),
                  reads=[cstr, self.CR], writes=[self.PB[b]])
            kk.op("act", lambda e: e.copy(out=cwb[:], in_=self.P[:, b, 0:176]), reads=[self.PB[b]], writes=[cwr])

            NQ = 4
            qchunks = [list(range(0, 6)), list(range(6, 11)), list(range(11, 17)), list(range(17, 22))]
            yT = ph.sb("yT", [128, 6, S], BF16)
            yTr = [ph.rs(NG, "yT%d_" % j) for j in range(6)]
            NWB = 3
            wup = [ph.sb("wup%d" % j, [128, NCH, 256], BF16) for j in range(NWB)]
            wupr = ph.rs(NWB, "wup")
            wdn = [ph.sb("wdn%d" % j, [128, 6, D], BF16) for j in range(2)]
            wdnr = ph.rs(2, "wdn")
            NU = 3
            U = [ph.sb("U%d" % j, [128, 2, 514], F32) for j in range(NU)]
            Ur = ph.rs(NU, "U")
            TG = [ph.sb("TG%d" % j, [128, 2, 512], F32) for j in range(3)]
            TGr = ph.rs(3, "TG")
            SG = [ph.sb("SG%d" % j, [128, 512], F32) for j in range(2)]
            SGr = ph.rs(2, "SG")
            wup_d = self.ffn_w_up[l]
            wdn_d = self.ffn_w_down[l]
            ui = 0
            wi = 0
            ti = 0
            pend = []
            for qi, chunks in enumerate(qchunks):
                nq = len(chunks)
                wd, wdr = wdn[qi % 2], wdnr[qi % 2]
                kk.dma("pool", wd[:, 0:nq, :], wblk(wdn_d, chunks[0], nq, 0, D), writes=[wdr])
                for ci, c in enumerate(chunks):
                    w, wr = wup[wi % NWB], wupr[wi % NWB]
                    wi += 1
                    kk.dma("pool", w[:, :, 0:128], wblk(wup_d, 0, NCH, c * 128, 128), writes=[wr])
                    kk.dma("pool", w[:, :, 128:256], wblk(wup_d, 0, NCH, DFF + c * 128, 128), writes=[wr])
                    prevU = None
                    for st in range(NG):
                        bg = kk.bank()
                        bu = kk.bank()
                        for (bb, co) in ((bg, 0), (bu, 128)):
                            for kc in range(NCH):
                                kk.op("pe", lambda e, bb=bb, co=co, kc=kc, w=w, st=st: e.matmul(
                                    self.P[:, bb, :], lhsT=w[:, kc, co:co + 128], rhs=hT[:, kc, st * 512:(st + 1) * 512],
                                    start=(kc == 0), stop=(kc == NCH - 1)),
                                    reads=[wr] + hTr[st * 4:(st + 1) * 4], writes=[self.PB[bb]])
                        u, ur = U[ui % NU], Ur[ui % NU]
                        ui += 1
                        kk.op("act", lambda e, u=u, bg=bg: e.copy(out=u[:, 0, 2:514], in_=self.P[:, bg, :]),
                              reads=[self.PB[bg]], writes=[ur])
                        kk.op("act", lambda e, u=u, bu=bu: e.copy(out=u[:, 1, 2:514], in_=self.P[:, bu, :]),
                              reads=[self.PB[bu]], writes=[ur])
                        if prevU is not None:
                            pu, pur = prevU
                            kk.op("act", lambda e, u=u, pu=pu: e.copy(out=u[:, :, 0:2], in_=pu[:, :, 512:514]),
                                  reads=[pur, ur], writes=[ur])
                        else:
                            kk.op("dve", lambda e, u=u: e.memset(u[:, :, 0:2], 0.0), reads=[ur], writes=[ur])
                        prevU = (u, ur)
                        tg, tgr = TG[ti % 3], TGr[ti % 3]
                        sg, sgr = SG[ti % 2], SGr[ti % 2]
                        ti += 1
                        for gi, fc in ((0, c), (1, NFC + c)):
                            w2 = cwb[:, 2 * 44 + fc:2 * 44 + fc + 1]
                            w1 = cwb[:, 1 * 44 + fc:1 * 44 + fc + 1]
                            w0 = cwb[:, 0 * 44 + fc:0 * 44 + fc + 1]
                            bb_ = cwb[:, 3 * 44 + fc:3 * 44 + fc + 1]
                            bsrc = bg if gi == 0 else bu
                            kk.op("act", lambda e, tg=tg, gi=gi, w2=w2, bb_=bb_, bsrc=bsrc: e.activation(
                                out=tg[:, gi, :], in_=self.P[:, bsrc, :], func=AF.Identity, bias=bb_, scale=w2),
                                reads=[self.PB[bsrc], cwr], writes=[tgr])
                            kk.op("dve", lambda e, u=u, tg=tg, gi=gi, w1=w1: e.scalar_tensor_tensor(
                                out=tg[:, gi, :], in0=u[:, gi, 1:513], scalar=w1, in1=tg[:, gi, :], op0=ALU.mult, op1=ALU.add),
                                reads=[ur, cwr, tgr], writes=[tgr])
                            kk.op("dve", lambda e, u=u, tg=tg, gi=gi, w0=w0: e.scalar_tensor_tensor(
                                out=tg[:, gi, :], in0=u[:, gi, 0:512], scalar=w0, in1=tg[:, gi, :], op0=ALU.mult, op1=ALU.add),
                                reads=[ur, cwr, tgr], writes=[tgr])
                        def fin(sg=sg, tg=tg, ci=ci, st=st, sgr=sgr, tgr=tgr):
                            kk.op("act", lambda e: e.activation(out=sg[:], in_=tg[:, 0, :], func=AF.Silu),
                                  reads=[tgr], writes=[sgr])
                            kk.op("dve", lambda e: e.tensor_tensor(
                                out=yT[:, ci, st * 512:(st + 1) * 512], in0=sg[:], in1=tg[:, 1, :], op=ALU.mult),
                                reads=[sgr, tgr], writes=[yTr[ci][st]])
                        while pend:
                            pend.pop(0)()
                        pend.append(fin)
                while pend:
                    pend.pop(0)()
                for i in range(NT):
                    for hf in range(2):
                        b = kk.bank()
                        for ci in range(nq):
                            kk.op("pe", lambda e, b=b, ci=ci, i=i, hf=hf, wd=wd: e.matmul(
                                self.P[:, b, :], lhsT=yT[:, ci, i * 128:(i + 1) * 128], rhs=wd[:, ci, hf * 512:(hf + 1) * 512],
                                start=(ci == 0), stop=(ci == nq - 1)),
                                reads=[wdr, yTr[ci][i // 4]], writes=[self.PB[b]])
                        self.resid_add(i, b, hf)

    def headnorm(self, ph, tag, bank0, nh, hd, gain, gain_r, out, out_r, extra_reads=()):
        kk = self.kk
        nb = (nh * hd) // 512
        src = self.pb(bank0, nb)
        pbr = [self.PB[bank0 + j] for j in range(nb)]
        ss = ph.sb(tag + "ss", [128, nh], F32)
        ssr = ph.r(tag + "ss")
        junk = ph.sb(tag + "jk", [128, hd], BF16)
        jr = ph.r(tag + "jk")
        for h in range(nh):
            kk.op("act", lambda e, h=h: e.activation(out=junk[:], in_=src[:, h * hd:(h + 1) * hd], func=AF.Square,
                                                     accum_out=ss[:, h:h + 1]),
                  reads=pbr, writes=[jr, ssr])
        self.rstd_from_ss(ss[:], ss[:], hd, ssr)
        for h in range(nh):
            kk.op("dve", lambda e, h=h: e.scalar_tensor_tensor(out=out[:, h * hd:(h + 1) * hd], in0=src[:, h * hd:(h + 1) * hd],
                                                               scalar=ss[:, h:h + 1], in1=gain[:], op0=ALU.mult, op1=ALU.mult),
                  reads=pbr + [ssr, gain_r], writes=[out_r])

    def xattn(self, s, l):
        kk, S, NT, NG = self.kk, self.S, self.NT, self.NG
        with Phase(kk) as ph0:
            kT = ph0.sb("xkT", [128, NCH, N_MEM], BF16)
            kTr = ph0.r("xkT")
            Vt = ph0.sb("xV", [128, 2, D], BF16)
            Vr = ph0.r("xV")
            with Phase(kk) as ph:
                gm, gmr = self.load_gain_bc(ph, "gm", self.norm_mem[l], D)
                gk, gkr = self.load_gain_bc(ph, "gk", self.xa_k_norm[l], 256)
                mem = ph.sb("mem", [128, 2, D], F32)
                memr = ph.rs(2, "mem")
                for mt in range(2):
                    kk.dma("sp", mem[:, mt, :], self.mem_d[s, mt * 128:(mt + 1) * 128, :], writes=[memr[mt]])
                memT = ph.sb("memT", [128, NCH, N_MEM], BF16)
                memTr = ph.rs(2, "memT")
                self.norm_T(ph, "mn", [(mem[:, mt, :], memr[mt]) for mt in range(2)], gm, gmr, memT, memTr)
                wkv = [ph.sb("wkv%d" % j, [128, NCH, 512], BF16) for j in range(4)]
                wkvr = ph.rs(4, "wkv")
                for j in range(4):
                    kk.dma("pool", wkv[j][:], wblk(self.xa_w_kv[l], 0, NCH, j * 512, 512), writes=[wkvr[j]])
                kn = ph.sb("kn", [128, D], BF16)
                knr = ph.r("kn")
                for mt in range(2):
                    b0 = kk.bank(2)
                    for hf in range(2):
                        for kc in range(NCH):
                            kk.op("pe", lambda e, hf=hf, kc=kc, mt=mt, b0=b0: e.matmul(
                                self.P[:, b0 + hf, :], lhsT=memT[:, kc, mt * 128:(mt + 1) * 128], rhs=wkv[hf][:, kc, :],
                                start=(kc == 0), stop=(kc == NCH - 1)),
                                reads=[memTr[mt], wkvr[hf]], writes=[self.PB[b0 + hf]])
                    self.headnorm(ph, "kh%d" % mt, b0, 4, 256, gk, gkr, kn, knr)
                    b = kk.bank()
                    pv = self.pbb(b)
                    for c in range(NCH):
                        kk.op("pe", lambda e, c=c, pv=pv: e.transpose(out=pv[:, c * 128:(c + 1) * 128], in_=kn[:, c * 128:(c + 1) * 128],
                                                                      identity=self.identb[:]),
                              reads=[knr, self.CR2], writes=[self.PB[b]])
                    kk.op("act", lambda e, mt=mt, pv=pv: e.copy(out=kT[:, :, mt * 128:(mt + 1) * 128],
                                                              in_=pv.rearrange("p (c t) -> p c t", c=NCH)),
                          reads=[self.PB[b]], writes=[kTr])
                    b1 = kk.bank(2)
                    for hf in range(2):
                        for kc in range(NCH):
                            kk.op("pe", lambda e, hf=hf, kc=kc, mt=mt, b1=b1: e.matmul(
                                self.P[:, b1 + hf, :], lhsT=memT[:, kc, mt * 128:(mt + 1) * 128], rhs=wkv[2 + hf][:, kc, :],
                                start=(kc == 0), stop=(kc == NCH - 1)),
                                reads=[memTr[mt], wkvr[2 + hf]], writes=[self.PB[b1 + hf]])
                    kk.op("act", lambda e, mt=mt, b1=b1: e.copy(out=Vt[:, mt, :], in_=self.pb(b1, 2)),
                          reads=[self.PB[b1], self.PB[b1 + 1]], writes=[Vr])
            with Phase(kk) as ph:
                gc, gcr = self.load_gain_bc(ph, "gc", self.norm_cross[l], D)
                gq, gqr = self.load_gain_bc(ph, "gq", self.xa_q_norm[l], 256)
                wq = ph.sb("wq", [128, NCH, D], BF16)
                wqr = ph.r("wq")
                wo = ph.sb("wo", [128, NCH, D], BF16)
                wor = ph.r("wo")
                for hf in range(2):
                    kk.dma("pool", wq[:, :, hf * 512:(hf + 1) * 512], wblk(self.xa_w_q[l], 0, NCH, hf * 512, 512), writes=[wqr])
                for hf in range(2):
                    kk.dma("pool", wo[:, :, hf * 512:(hf + 1) * 512], wblk(self.xa_w_o[l], 0, NCH, hf * 512, 512), writes=[wor])
                hTg = [ph.sb("xh%d" % j, [128, NCH, 512], BF16) for j in range(2)]
                hTgr = [ph.rs(4, "xh%d_" % j) for j in range(2)]
                qTg = [ph.sb("xq%d" % j, [128, NCH, 512], BF16) for j in range(2)]
                qTgr = [ph.rs(4, "xq%d_" % j) for j in range(2)]
                oTg = [ph.sb("xo%d" % j, [128, NCH, 512], BF16) for j in range(2)]
                oTgr = [ph.rs(NCH, "xo%d_" % j) for j in range(2)]
                qn = [ph.sb("xqn%d" % j, [128, D], BF16) for j in range(2)]
                qnr = ph.rs(2, "xqn")
                PT = [ph.sb("xPT%d" % j, [128, 2, 512], BF16) for j in range(2)]
                PTr = ph.rs(2, "xPT")
                rden = [ph.sb("xrd%d" % j, [128, 512], F32) for j in range(2)]
                rdr = ph.rs(2, "xrd")
                pi = 0
                for g in range(NG):
                    hT, hTr = hTg[g % 2], hTgr[g % 2]
                    qT, qTr = qTg[g % 2], qTgr[g % 2]
                    oT, oTr = oTg[g % 2], oTgr[g % 2]
                    self.norm_T(ph, "xn%d_" % g, [(self.X[:, g * 4 + j, :], self.XR[g * 4 + j]) for j in range(4)], gc, gcr, hT, hTr)
                    for j in range(4):
                        b0 = kk.bank(2)
                        for hf in range(2):
                            for kc in range(NCH):
                                kk.op("pe", lambda e, hf=hf, kc=kc, j=j, b0=b0, hT=hT: e.matmul(
                                    self.P[:, b0 + hf, :], lhsT=hT[:, kc, j * 128:(j + 1) * 128], rhs=wq[:, kc, hf * 512:(hf + 1) * 512],
                                    start=(kc == 0), stop=(kc == NCH - 1)),
                                    reads=[hTr[j], wqr], writes=[self.PB[b0 + hf]])
                        q_, q_r = qn[j % 2], qnr[j % 2]
                        self.headnorm(ph, "qh%d_%d" % (g, j), b0, 4, 256, gq, gqr, q_, q_r)
                        b = kk.bank()
                        pv = self.pbb(b)
                        for c in range(NCH):
                            kk.op("pe", lambda e, c=c, pv=pv, q_=q_: e.transpose(out=pv[:, c * 128:(c + 1) * 128],
                                                                              in_=q_[:, c * 128:(c + 1) * 128], identity=self.identb[:]),
                                  reads=[q_r, self.CR2], writes=[self.PB[b]])
                        kk.op("act", lambda e, j=j, pv=pv, qT=qT: e.copy(out=qT[:, :, j * 128:(j + 1) * 128],
                                                                      in_=pv.rearrange("p (c t) -> p c t", c=NCH)),
                              reads=[self.PB[b]], writes=[qTr[j]])
                    for h in range(4):
                        pt, ptr = PT[pi % 2], PTr[pi % 2]
                        rd, rdr_ = rden[pi % 2], rdr[pi % 2]
                        pi += 1
                        b0 = kk.bank(2)
                        for mt in range(2):
                            for hh in range(2):
                                kk.op("pe", lambda e, mt=mt, hh=hh, h=h, b0=b0, qT=qT: e.matmul(
                                    self.P[:, b0 + mt, :], lhsT=kT[:, h * 2 + hh, mt * 128:(mt + 1) * 128], rhs=qT[:, h * 2 + hh, :],
                                    start=(hh == 0), stop=(hh == 1)),
                                    reads=[kTr] + qTr, writes=[self.PB[b0 + mt]])
                        kk.op("act", lambda e, b0=b0, pt=pt: e.activation(out=pt[:].rearrange("p a b -> p (a b)"), in_=self.pb(b0, 2),
                                                                        func=AF.Exp, scale=1.0 / 16.0),
                              reads=[self.PB[b0], self.PB[b0 + 1]], writes=[ptr])
                        bd = kk.bank()
                        for mt in range(2):
                            kk.op("pe", lambda e, mt=mt, bd=bd, pt=pt: e.matmul(self.P[:, bd, :], lhsT=self.onesb[:], rhs=pt[:, mt, :],
                                                                             start=(mt == 0), stop=(mt == 1)),
                                  reads=[ptr, self.CR2], writes=[self.PB[bd]])
                        kk.op("dve", lambda e, bd=bd, rd=rd: e.reciprocal(out=rd[:], in_=self.P[:, bd, :]),
                              reads=[self.PB[bd]], writes=[rdr_])
                        for hh in range(2):
                            bo = kk.bank()
                            for mt in range(2):
                                kk.op("pe", lambda e, mt=mt, bo=bo, pt=pt, h=h, hh=hh: e.matmul(
                                    self.P[:, bo, :], lhsT=Vt[:, mt, h * 256 + hh * 128:h * 256 + (hh + 1) * 128], rhs=pt[:, mt, :],
                                    start=(mt == 0), stop=(mt == 1)),
                                    reads=[ptr, Vr], writes=[self.PB[bo]])
                            kk.op("dve", lambda e, bo=bo, rd=rd, oT=oT, h=h, hh=hh: e.tensor_tensor(
                                out=oT[:, h * 2 + hh, :], in0=self.P[:, bo, :], in1=rd[:], op=ALU.mult),
                                reads=[self.PB[bo], rdr_], writes=[oTr[h * 2 + hh]])
                    for j in range(4):
                        i = g * 4 + j
                        for hf in range(2):
                            b = kk.bank()
                            for c in range(NCH):
                                kk.op("pe", lambda e, c=c, b=b, j=j, hf=hf, oT=oT: e.matmul(
                                    self.P[:, b, :], lhsT=oT[:, c, j * 128:(j + 1) * 128], rhs=wo[:, c, hf * 512:(hf + 1) * 512],
                                    start=(c == 0), stop=(c == NCH - 1)),
                                    reads=[oTr[c], wor], writes=[self.PB[b]])
                            self.resid_add(i, b, hf)

    def odd_mixer(self, s, l):
        kk, S, NT, NG = self.kk, self.S, self.NT, self.NG
        o = l // 2
        NCK = NT * 4
        with Phase(kk) as ph:
            gain, gr = self.load_gain_bc(ph, "og", self.norm_mix[l], D)
            go, gor = self.load_gain_bc(ph, "ogo", self.hgrn_o_norm[o], 128)
            hT = ph.sb("ohT", [128, NCH, S], BF16)
            hTr = ph.rs(NT, "ohT")
            self.norm_T(ph, "on", [(self.X[:, i, :], self.XR[i]) for i in range(NT)], gain, gr, hT, hTr)
            rm = ph.sb("orm", [128, 512], BF16)
            rmr = ph.r("orm")
            kk.op("dve", lambda e: e.memset(rm[:], 1.0), writes=[rmr])
            kk.op("dve", lambda e: e.memset(rm[:, 0:512:32], 0.0), writes=[rmr])
            win = [ph.sb("owin%d" % j, [128, NCH, 512], BF16) for j in range(2)]
            winr = ph.rs(2, "owin")
            wout = [ph.sb("owout%d" % j, [128, D], BF16) for j in range(2)]
            woutr = ph.rs(2, "owout")
            qt = ph.sb("oqt", [128, S], BF16)
            qtr = ph.rs(NG, "oqt")
            kt = ph.sb("okt", [128, S], BF16)
            ktr = ph.rs(NG, "okt")
            khT = ph.sb("okhT", [128, NT, 128], BF16)
            khTr = ph.rs(NT, "okhT")
            vtok = ph.sb("ovt", [128, NT, 128], BF16)
            vtr = ph.rs(NT, "ovt")
            sgt = ph.sb("osg", [128, NT, 128], BF16)
            sgr = ph.rs(NT, "osg")
            adec = ph.sb("oadec", [128, NCK], F32)
            adr = ph.rs(NG, "oadec")
            Sbf = ph.sb("oSbf", [128, NCK, 128], BF16)
            Sbfr = ph.rs(NCK, "oSbf")
            Sst = [ph.sb("oSst%d" % j, [128, 128], F32) for j in range(2)]
            Sstr = ph.rs(2, "oSst")
            T1 = [[ph.sb("ot%d_%d" % (a, j), [128, 512], F32) for j in range(2)] for a in range(6)]
            T1r = [ph.rs(2, "ot%d_" % a) for a in range(6)]
            kh = [ph.sb("okh%d" % j, [128, 512], BF16) for j in range(2)]
            khr = ph.rs(2, "okh")
            vm = [ph.sb("ovm%d" % j, [128, 4, 128], BF16) for j in range(2)]
            vmr = ph.rs(2, "ovm")
            qm = [ph.sb("oqm%d" % j, [128, 4, 128], BF16) for j in range(2)]
            qmr = ph.rs(2, "oqm")
            for j in range(2):
                kk.op("dve", lambda e, j=j: e.memset(qm[j][:], 0.0), writes=[qmr[j]])
            scm = [ph.sb("oscm%d" % j, [128, 128], BF16) for j in range(2)]
            scmr = ph.rs(2, "oscm")
            yf = [ph.sb("oyf%d" % j, [128, 128], F32) for j in range(2)]
            yfr = ph.rs(2, "oyf")
            yb = [ph.sb("oyb%d" % j, [128, 128], BF16) for j in range(2)]
            ybr = ph.rs(2, "oyb")
            yT = [ph.sb("oyT%d" % j, [128, 128], BF16) for j in range(2)]
            yTr = ph.rs(2, "oyT")
            oss = ph.sb("oss", [128, 2], F32)
            ossr = ph.rs(2, "oss")
            ojk = ph.sb("ojk", [128, 128], BF16)
            ojr = ph.r("ojk")
            w_in = self.hgrn_w_in[o]
            t1i = 0
            import os
            STG = int(os.environ.get("ODD_STAGE", "9"))
            for hd in range(int(os.environ.get("ODD_HEADS", "8"))):
                if STG < 1:
                    break
                w, wr = win[hd % 2], winr[hd % 2]
                for j in range(4):
                    kk.dma("pool", w[:, :, j * 128:(j + 1) * 128], wblk(w_in, 0, NCH, j * 1024 + hd * 128, 128), writes=[wr])
                wo_, wor_ = wout[hd % 2], woutr[hd % 2]
                kk.dma("pool", wo_[:], self.hgrn_w_out[o][hd * 128:(hd + 1) * 128, :], writes=[wor_])
                col = l * 8 + hd
                lb = self.lbt[:, col:col + 1]
                oml = self.omlt[:, col:col + 1]
                noml = self.nomlt[:, col:col + 1]
                for g in range(NG):
                    tsl = slice(g * 512, (g + 1) * 512)
                    bq = kk.bank()
                    bf = kk.bank()
                    for (bb, co) in ((bq, 0), (bf, 128)):
                        for kc in range(NCH):
                            kk.op("pe", lambda e, bb=bb, co=co, kc=kc, w=w, tsl=tsl: e.matmul(
                                self.P[:, bb, :], lhsT=w[:, kc, co:co + 128], rhs=hT[:, kc, tsl],
                                start=(kc == 0), stop=(kc == NCH - 1)),
                                reads=[wr] + hTr[g * 4:(g + 1) * 4], writes=[self.PB[bb]])
                    p = t1i % 2
                    t1i += 1
                    sig, lf, kf, bb_, eb, enb = [T1[a][p] for a in range(6)]
                    sigr, lfr, kfr, bbr, ebr, enbr = [T1r[a][p] for a in range(6)]
                    kk.op("act", lambda e, sig=sig, bf=bf: e.activation(out=sig[:], in_=self.P[:, bf, :], func=AF.Sigmoid),
                          reads=[self.PB[bf]], writes=[sigr])
                    kk.op("act", lambda e, sig=sig, lf=lf: e.activation(out=lf[:], in_=sig[:], func=AF.Ln, bias=lb, scale=oml),
                          reads=[sigr, self.LBR], writes=[lfr])
                    kk.op("dve", lambda e, sig=sig, kf=kf: e.tensor_scalar(out=kf[:], in0=sig[:], scalar1=noml, scalar2=oml,
                                                                         op0=ALU.mult, op1=ALU.add),
                          reads=[sigr, self.LBR], writes=[kfr])
                    kk.op("dve", lambda e, lf=lf, bb_=bb_: e.tensor_tensor_scan(out=bb_[:], data0=rm[:], data1=lf[:], initial=0.0,
                                                                               op0=ALU.mult, op1=ALU.add),
                          reads=[lfr, rmr], writes=[bbr])
                    kk.op("act", lambda e, bb_=bb_, eb=eb: e.activation(out=eb[:], in_=bb_[:], func=AF.Exp), reads=[bbr], writes=[ebr])
                    kk.op("act", lambda e, bb_=bb_, enb=enb: e.activation(out=enb[:], in_=bb_[:], func=AF.Exp, scale=-1.0),
                          reads=[bbr], writes=[enbr])
                    kk.op("dve", lambda e, eb=eb, bq=bq, tsl=tsl: e.tensor_tensor(out=qt[:, tsl], in0=self.P[:, bq, :], in1=eb[:], op=ALU.mult),
                          reads=[self.PB[bq], ebr], writes=[qtr[g]])
                    kk.op("dve", lambda e, kf=kf, enb=enb, tsl=tsl: e.tensor_tensor(out=kt[:, tsl], in0=kf[:], in1=enb[:], op=ALU.mult),
                          reads=[kfr, enbr], writes=[ktr[g]])
                    kh_, khr_ = kh[p], khr[p]
                    for cc in range(16):
                        kk.op("dve", lambda e, eb=eb, kh_=kh_, cc=cc, g=g: e.tensor_scalar(
                            out=kh_[:, cc * 32:(cc + 1) * 32], in0=kt[:, g * 512 + cc * 32:g * 512 + (cc + 1) * 32],
                            scalar1=eb[:, cc * 32 + 31:cc * 32 + 32], scalar2=None, op0=ALU.mult),
                            reads=[ktr[g], ebr], writes=[khr_])
                    kk.op("act", lambda e, eb=eb, g=g: e.copy(out=adec[:, g * 16:(g + 1) * 16], in_=eb[:, 31:512:32]),
                          reads=[ebr], writes=[adr[g]])
                    b = kk.bank()
                    pv = self.pbb(b)
                    for j in range(4):
                        kk.op("pe", lambda e, j=j, pv=pv, kh_=kh_: e.transpose(out=pv[:, j * 128:(j + 1) * 128],
                                                                            in_=kh_[:, j * 128:(j + 1) * 128], identity=self.identb[:]),
                              reads=[khr_, self.CR2], writes=[self.PB[b]])
                    kk.op("act", lambda e, pv=pv, g=g: e.copy(out=khT[:, g * 4:(g + 1) * 4, :],
                                                              in_=pv[:, 0:512].rearrange("p (c t) -> p c t", c=4)),
                          reads=[self.PB[b]], writes=khTr[g * 4:(g + 1) * 4])
                if STG < 2:
                    continue
                for i in range(NT):
                    b = kk.bank()
                    for kc in range(NCH):
                        kk.op("pe", lambda e, kc=kc, b=b, i=i, w=w: e.matmul(
                            self.P[:, b, 0:256], lhsT=hT[:, kc, i * 128:(i + 1) * 128], rhs=w[:, kc, 256:512],
                            start=(kc == 0), stop=(kc == NCH - 1)),
                            reads=[wr, hTr[i]], writes=[self.PB[b]])
                    kk.op("act", lambda e, b=b, i=i: e.copy(out=vtok[:, i, :], in_=self.P[:, b, 0:128]),
                          reads=[self.PB[b]], writes=[vtr[i]])
                    kk.op("act", lambda e, b=b, i=i: e.activation(out=sgt[:, i, :], in_=self.P[:, b, 128:256], func=AF.Silu),
                          reads=[self.PB[b]], writes=[sgr[i]])
                if STG < 3:
                    continue
                kk.op("dve", lambda e: e.memset(Sst[0][:], 0.0), writes=[Sstr[0]])
                kk.op("dve", lambda e: e.memset(Sbf[:, 0, :], 0.0), writes=[Sbfr[0]])
                for cj in range(NCK - 1):
                    i, j = divmod(cj, 4)
                    if j == 0:
                        vm_, vm_r = vm[i % 2], vmr[i % 2]
                        for jj in range(4):
                            kk.op("dve", lambda e, i=i, vm_=vm_, jj=jj: e.tensor_scalar(
                                out=vm_[:, jj, :], in0=vtok[:, i, :], scalar1=self.rmask4[:, jj * 128:jj * 128 + 1], scalar2=None, op0=ALU.mult),
                                reads=[vtr[i], self.CR2], writes=[vm_r])
                    bd = kk.bank()
                    kk.op("pe", lambda e, bd=bd, i=i, j=j, vm_=vm_: e.matmul(
                        self.P[:, bd, 0:128], lhsT=khT[:, i, :], rhs=vm_[:, j, :], start=True, stop=True),
                        reads=[khTr[i], vm_r], writes=[self.PB[bd]])
                    kk.op("dve", lambda e, bd=bd, j=j, cj=cj: e.scalar_tensor_tensor(
                        out=Sst[(cj + 1) % 2][:], in0=Sst[cj % 2][:], scalar=adec[:, cj:cj + 1], in1=self.P[:, bd, 0:128],
                        op0=ALU.mult, op1=ALU.add),
                        reads=[Sstr[cj % 2], adr[cj // 16], self.PB[bd]], writes=[Sstr[(cj + 1) % 2]])
                    kk.op("act", lambda e, cj=cj: e.copy(out=Sbf[:, cj + 1, :], in_=Sst[(cj + 1) % 2][:]), reads=[Sstr[(cj + 1) % 2]], writes=[Sbfr[cj + 1]])
                if STG < 4:
                    continue
                bobox = {}

                def odd_X(i):
                    p = i % 2
                    bs = kk.bank()
                    kk.op("pe", lambda e, bs=bs, i=i: e.matmul(self.P[:, bs, 0:128], lhsT=kt[:, i * 128:(i + 1) * 128],
                                                              rhs=qt[:, i * 128:(i + 1) * 128], start=True, stop=True),
                          reads=[ktr[i // 4], qtr[i // 4]], writes=[self.PB[bs]])
                    kk.op("dve", lambda e, bs=bs, p=p: e.tensor_tensor(out=scm[p][:], in0=self.P[:, bs, 0:128], in1=self.hmask[:], op=ALU.mult),
                          reads=[self.PB[bs], self.CR], writes=[scmr[p]])
                    bo = kk.bank()
                    kk.op("pe", lambda e, bo=bo, i=i, p=p: e.matmul(self.P[:, bo, 0:128], lhsT=scm[p][:], rhs=vtok[:, i, :],
                                                                 start=True, stop=False),
                          reads=[scmr[p], vtr[i]], writes=[self.PB[bo]])
                    qm_, qm_r = qm[p], qmr[p]
                    for jj in range(4):
                        kk.op("dve", lambda e, i=i, qm_=qm_, jj=jj: e.tensor_copy(
                            out=qm_[:, jj, jj * 32:(jj + 1) * 32], in_=qt[:, i * 128 + jj * 32:i * 128 + (jj + 1) * 32]),
                            reads=[qtr[i // 4]], writes=[qm_r])
                    for j in range(4):
                        kk.op("pe", lambda e, bo=bo, i=i, j=j, qm_=qm_: e.matmul(
                            self.P[:, bo, 0:128], lhsT=qm_[:, j, :], rhs=Sbf[:, 4 * i + j, :], start=False, stop=(j == 3)),
                            reads=[qm_r, Sbfr[4 * i + j]], writes=[self.PB[bo]])
                    bobox[i] = bo
                    kk.reserved.add(bo)

                def odd_Y(i, wo_=wo_, wor_=wor_):
                    p = i % 2
                    bo = bobox.pop(i)
                    kk.op("act", lambda e, bo=bo, p=p: e.activation(out=ojk[:], in_=self.P[:, bo, 0:128], func=AF.Square,
                                                                    accum_out=oss[:, p:p + 1]),
                          reads=[self.PB[bo]], writes=[ojr, ossr[p]])
                    self.rstd_from_ss(oss[:, p:p + 1], oss[:, p:p + 1], 128, ossr[p])
                    kk.op("dve", lambda e, bo=bo, p=p: e.scalar_tensor_tensor(out=yf[p][:], in0=self.P[:, bo, 0:128], scalar=oss[:, p:p + 1],
                                                                             in1=go[:], op0=ALU.mult, op1=ALU.mult),
                          reads=[self.PB[bo], ossr[p], gor], writes=[yfr[p]])
                    kk.reserved.discard(bo)
                    kk.op("dve", lambda e, p=p, i=i: e.tensor_tensor(out=yb[p][:], in0=yf[p][:], in1=sgt[:, i, :], op=ALU.mult),
                          reads=[yfr[p], sgr[i]], writes=[ybr[p]])
                    bt = kk.bank()
                    pv = self.pbb(bt)
                    kk.op("pe", lambda e, pv=pv, p=p: e.transpose(out=pv[:, 0:128], in_=yb[p][:], identity=self.identb[:]),
                          reads=[ybr[p], self.CR2], writes=[self.PB[bt]])
                    kk.op("act", lambda e, pv=pv, p=p: e.copy(out=yT[p][:], in_=pv[:, 0:128]), reads=[self.PB[bt]], writes=[yTr[p]])
                    for hf in range(2):
                        b = kk.bank()
                        kk.op("pe", lambda e, b=b, p=p, hf=hf, wo_=wo_: e.matmul(self.P[:, b, :], lhsT=yT[p][:], rhs=wo_[:, hf * 512:(hf + 1) * 512],
                                                                              start=True, stop=True),
                              reads=[yTr[p], wor_], writes=[self.PB[b]])
                        self.resid_add(i, b, hf)

                odd_X(0)
                for i in range(NT):
                    if i + 1 < NT:
                        odd_X(i + 1)
                    odd_Y(i)

    def rope_tables(self, ph, s):
        kk, NT = self.kk, self.NT
        n = NT * 32
        posi = ph.sb("posi", [128, NT], I32)
        pr = ph.r("pos")
        with self.nc.allow_non_contiguous_dma(reason="positions token-major gather"):
            kk.dma("sp", posi[:], self.pos_d[s].rearrange("(n p) -> p n", p=128), writes=[pr])
        posf = ph.sb("posf", [128, NT], F32)
        kk.op("dve", lambda e: e.tensor_copy(out=posf[:], in_=posi[:]), reads=[pr], writes=[pr])
        ang = ph.sb("ang", [128, NT, 32], F32)
        for t in range(NT):
            kk.op("dve", lambda e, t=t: e.tensor_scalar(out=ang[:, t, :], in0=self.invf[:], scalar1=posf[:, t:t + 1], scalar2=None,
                                                       op0=ALU.mult),
                  reads=[pr, self.CR], writes=[pr])
        a2 = ang[:].rearrange("p a b -> p (a b)")
        kf = ph.sb("ropek", [128, n], F32)
        r_ = ph.sb("roper", [128, n], F32)
        rc = ph.sb("roperc", [128, n], F32)
        m_ = ph.sb("ropem", [128, n], F32)
        kk.op("dve", lambda e: e.tensor_scalar(out=kf[:], in0=a2, scalar1=1.0 / TWO_PI, scalar2=MAGIC, op0=ALU.mult, op1=ALU.add),
              reads=[pr], writes=[pr])
        kk.op("dve", lambda e: e.tensor_scalar(out=kf[:], in0=kf[:], scalar1=-MAGIC, scalar2=None, op0=ALU.add), reads=[pr], writes=[pr])
        kk.op("dve", lambda e: e.scalar_tensor_tensor(out=r_[:], in0=kf[:], scalar=-CW1, in1=a2, op0=ALU.mult, op1=ALU.add),
              reads=[pr], writes=[pr])
        kk.op("dve", lambda e: e.scalar_tensor_tensor(out=r_[:], in0=kf[:], scalar=-CW2, in1=r_[:], op0=ALU.mult, op1=ALU.add),
              reads=[pr], writes=[pr])
        kk.op("dve", lambda e: e.tensor_scalar(out=rc[:], in0=r_[:], scalar1=0.5 * np.pi, scalar2=None, op0=ALU.add), reads=[pr], writes=[pr])
        kk.op("dve", lambda e: e.tensor_scalar(out=m_[:], in0=rc[:], scalar1=float(np.pi), scalar2=None, op0=ALU.is_gt), reads=[pr], writes=[pr])
        kk.op("dve", lambda e: e.scalar_tensor_tensor(out=rc[:], in0=m_[:], scalar=-TWO_PI, in1=rc[:], op0=ALU.mult, op1=ALU.add),
              reads=[pr], writes=[pr])
        for t_ in (r_, rc):
            kk.op("dve", lambda e, t_=t_: e.tensor_scalar(out=t_[:], in0=t_[:], scalar1=-PI_LO, scalar2=PI_LO, op0=ALU.max, op1=ALU.min),
                  reads=[pr], writes=[pr])
        cos3 = ph.sb("cos3", [128, NT, 3, 32], F32)
        sin3 = ph.sb("sin3", [128, NT, 3, 32], F32)
        tr = ph.r("ropetab")
        kk.op("act", lambda e: e.activation(out=sin3[:, :, 0, :], in_=r_[:].rearrange("p (a b) -> p a b", b=32), func=AF.Sin),
              reads=[pr], writes=[tr])
        kk.op("act", lambda e: e.activation(out=cos3[:, :, 0, :], in_=rc[:].rearrange("p (a b) -> p a b", b=32), func=AF.Sin),
              reads=[pr], writes=[tr])
        for j in (1, 2):
            kk.op("dve", lambda e, j=j: e.tensor_copy(out=sin3[:, :, j, :], in_=sin3[:, :, 0, :]), reads=[tr], writes=[tr])
            kk.op("dve", lambda e, j=j: e.tensor_copy(out=cos3[:, :, j, :], in_=cos3[:, :, 0, :]), reads=[tr], writes=[tr])
        return cos3, sin3, tr

    def outproj_partial(self, i, aT_ap, aT_r, wo_, wor_):
        kk = self.kk
        for hf in range(2):
            b = kk.bank()
            kk.op("pe", lambda e, b=b, hf=hf: e.matmul(self.P[:, b, :], lhsT=aT_ap, rhs=wo_[:, hf * 512:(hf + 1) * 512], start=True, stop=True),
                  reads=[aT_r, wor_], writes=[self.PB[b]])
            self.resid_add(i, b, hf)

    def even_mixer(self, s, l):
        kk, S, NT, NG = self.kk, self.S, self.NT, self.NG
        ei = l // 2
        w_in = self.ab_w_in[ei]
        w_out = self.ab_w_out[ei]
        with Phase(kk) as ph:
            hT = ph.sb("ehT", [128, NCH, S], BF16)
            hTr = ph.rs(NT, "ehT")
            with Phase(kk) as pn:
                gain, gr = self.load_gain_bc(pn, "eg", self.norm_mix[l], D)
                self.norm_T(pn, "en", [(self.X[:, i, :], self.XR[i]) for i in range(NT)], gain, gr, hT, hTr)
            wout = [ph.sb("ewout%d" % j, [128, D], BF16) for j in range(2)]
            woutr = ph.rs(2, "ewout")
            Oc = [ph.sb("eOc%d" % j, [128, 128], BF16) for j in range(2)]
            Ocr = ph.rs(2, "eOc")
            aT = [ph.sb("eaT%d" % j, [128, 128], BF16) for j in range(2)]
            aTr = ph.rs(2, "eaT")
            oi = 0
            with Phase(kk) as pa:
                cos3, sin3, tabr = self.rope_tables(pa, s)
                g3 = pa.sb("g3", [128, 3, 64], F32)
                g3r = pa.r("g3")
                kk.dma("sp", g3[:, 0, :], self.swa_q_norm[ei].partition_broadcast(128), writes=[g3r])
                kk.dma("sp", g3[:, 1, :], self.swa_q_norm[ei].partition_broadcast(128), writes=[g3r])
                kk.dma("sp", g3[:, 2, :], self.swa_k_norm[ei].partition_broadcast(128), writes=[g3r])
                esk = pa.sb("esk", [128, 8], F32)
                eskr = pa.r("esk")
                kk.dma("sp", esk[:], self.swa_sinks[ei].partition_broadcast(128), writes=[eskr])
                kk.op("act", lambda e: e.activation(out=esk[:], in_=esk[:], func=AF.Exp), reads=[eskr], writes=[eskr])
                win = [pa.sb("ewa%d" % j, [128, NCH, 256], BF16) for j in range(2)]
                winr = pa.rs(2, "ewa")
                raw = [pa.sb("eraw%d" % j, [128, 256], F32) for j in range(2)]
                rawr = pa.rs(2, "eraw")
                ss3 = [pa.sb("ess%d" % j, [128, 3], F32) for j in range(2)]
                ss3r = pa.rs(2, "ess")
                jk = pa.sb("ejk", [128, 64], BF16)
                jkr = pa.r("ejk")
                xn = [pa.sb("exn%d" % j, [128, 3, 64], F32) for j in range(2)]
                xnr = pa.rs(2, "exn")
                tt = [pa.sb("ett%d" % j, [128, 4, 3, 32], F32) for j in range(2)]
                ttr = pa.rs(2, "ett")
                rp = [pa.sb("erp%d" % j, [128, 3, 64], F32) for j in range(2)]
                rpr = pa.rs(2, "erp")
                stage = [pa.sb("est%d" % j, [128, 2, 128], BF16) for j in range(2)]
                stager = pa.rs(2, "est")
                vst = [pa.sb("evs%d" % j, [128, 65], BF16) for j in range(3)]
                vstr = pa.rs(3, "evs")
                for j in range(3):
                    kk.op("dve", lambda e, j=j: e.memset(vst[j][:, 64:65], 1.0), writes=[vstr[j]])
                qkT = [pa.sb("eqk%d" % j, [128, 2, 128], BF16) for j in range(3)]
                qkTr = pa.rs(3, "eqk")
                PT = [pa.sb("ePT%d" % j, [128, 512], BF16) for j in range(2)]
                PTr = pa.rs(2, "ePT")
                den = [pa.sb("eden%d" % j, [128, 2], F32) for j in range(2)]
                denr = pa.rs(2, "eden")
                for c in range(4):
                    g = c // 2
                    w, wr = win[c % 2], winr[c % 2]
                    kk.dma("pool", w[:, :, 0:128], wblk(w_in, 0, NCH, c * 128, 128), writes=[wr])
                    kk.dma("pool", w[:, :, 128:192], wblk(w_in, 0, NCH, 512 + g * 64, 64), writes=[wr])
                    kk.dma("pool", w[:, :, 192:256], wblk(w_in, 0, NCH, 640 + g * 64, 64), writes=[wr])
                    wo_, wor_ = wout[oi % 2], woutr[oi % 2]
                    kk.dma("pool", wo_[:], w_out[c * 128:(c + 1) * 128, :], writes=[wor_])
                    def swa_A(i, c=c, g=g, w=w, wr=wr):
                        p2, p3 = i % 2, i % 3
                        b = kk.bank()
                        for kc in range(NCH):
                            kk.op("pe", lambda e, kc=kc, b=b, i=i, w=w: e.matmul(
                                self.P[:, b, 0:256], lhsT=hT[:, kc, i * 128:(i + 1) * 128], rhs=w[:, kc, :],
                                start=(kc == 0), stop=(kc == NCH - 1)),
                                reads=[wr, hTr[i]], writes=[self.PB[b]])
                        rw, rwr = raw[p2], rawr[p2]
                        kk.op("act", lambda e, b=b, rw=rw: e.copy(out=rw[:], in_=self.P[:, b, 0:256]), reads=[self.PB[b]], writes=[rwr])
                        s3, s3r = ss3[p2], ss3r[p2]
                        for h in range(3):
                            kk.op("act", lambda e, h=h, rw=rw, s3=s3: e.activation(out=jk[:], in_=rw[:, h * 64:(h + 1) * 64], func=AF.Square,
                                                                              accum_out=s3[:, h:h + 1]),
                                  reads=[rwr], writes=[jkr, s3r])
                        self.rstd_from_ss(s3[:], s3[:], 64, s3r)
                        x_, x_r = xn[p2], xnr[p2]
                        for h in range(3):
                            kk.op("dve", lambda e, h=h, rw=rw, s3=s3, x_=x_: e.scalar_tensor_tensor(
                                out=x_[:, h, :], in0=rw[:, h * 64:(h + 1) * 64], scalar=s3[:, h:h + 1], in1=g3[:, h, :],
                                op0=ALU.mult, op1=ALU.mult),
                                reads=[rwr, s3r, g3r], writes=[x_r])
                        t_, t_r = tt[p2], ttr[p2]
                        r2, r2r = rp[p2], rpr[p2]
                        x1, x2 = x_[:, :, 0:32], x_[:, :, 32:64]
                        cs_, sn_ = cos3[:, i, :, :], sin3[:, i, :, :]
                        for k_, (a_, b_) in enumerate(((x1, cs_), (x2, sn_), (x2, cs_), (x1, sn_))):
                            kk.op("pool", lambda e, k_=k_, a_=a_, b_=b_, t_=t_: e.tensor_tensor(out=t_[:, k_, :, :], in0=a_, in1=b_, op=ALU.mult),
                                  reads=[x_r, tabr], writes=[t_r])
                        kk.op("pool", lambda e, t_=t_, r2=r2: e.tensor_tensor(out=r2[:, :, 0:32], in0=t_[:, 0, :, :], in1=t_[:, 1, :, :], op=ALU.subtract),
                              reads=[t_r], writes=[r2r])
                        kk.op("pool", lambda e, t_=t_, r2=r2: e.tensor_tensor(out=r2[:, :, 32:64], in0=t_[:, 2, :, :], in1=t_[:, 3, :, :], op=ALU.add),
                              reads=[t_r], writes=[r2r])
                        st_, st_r = stage[p2], stager[p2]
                        kk.op("act", lambda e, st_=st_, r2=r2: e.copy(out=st_[:, 0, :].rearrange("p (a b) -> p a b", a=2), in_=r2[:, 0:2, :]),
                              reads=[r2r], writes=[st_r])
                        kk.op("act", lambda e, st_=st_, r2=r2: e.copy(out=st_[:, 1, 0:64], in_=r2[:, 2, :]), reads=[r2r], writes=[st_r])
                        kk.op("act", lambda e, st_=st_, r2=r2: e.copy(out=st_[:, 1, 64:128], in_=r2[:, 2, :]), reads=[r2r], writes=[st_r])
                        vs, vsr = vst[p3], vstr[p3]
                        kk.op("dve", lambda e, vs=vs, rw=rw: e.tensor_copy(out=vs[:, 0:64], in_=rw[:, 192:256]), reads=[rwr], writes=[vsr])
                        bt = kk.bank()
                        pv = self.pbb(bt)
                        for j in range(2):
                            kk.op("pe", lambda e, j=j, pv=pv, st_=st_: e.transpose(out=pv[:, j * 128:(j + 1) * 128], in_=st_[:, j, :],
                                                                                identity=self.identb[:]),
                                  reads=[st_r, self.CR2], writes=[self.PB[bt]])
                        qk, qkr = qkT[p3], qkTr[p3]
                        kk.op("act", lambda e, pv=pv, qk=qk: e.copy(out=qk[:].rearrange("p a b -> p (a b)"), in_=pv[:, 0:256]),
                              reads=[self.PB[bt]], writes=[qkr])
                    def swa_B(i, c=c, wo_=wo_, wor_=wor_):
                        p2, p3 = i % 2, i % 3
                        qk, qkr = qkT[p3], qkTr[p3]
                        oi = oibox[0]
                        blks = [(1, i)] if i == 0 else [(0, i - 1), (1, i)]
                        bs = kk.bank(2)
                        lo = 128 if i == 0 else 0
                        for (blk, ti) in blks:
                            for hh in range(2):
                                kq, kqr = qkT[ti % 3], qkTr[ti % 3]
                                kk.op("pe", lambda e, blk=blk, hh=hh, kq=kq, qk=qk, bs=bs: e.matmul(
                                    self.P[:, bs + hh, blk * 128:(blk + 1) * 128],
                                    lhsT=kq[hh * 64:(hh + 1) * 64, 1, :], rhs=qk[hh * 64:(hh + 1) * 64, 0, :], start=True, stop=True),
                                    reads=[kqr, qkr], writes=[self.PB[bs + hh]])
                        pt, ptr = PT[p2], PTr[p2]
                        pt3 = pt[:].rearrange("p (a b) -> p a b", a=2)
                        mk3 = self.swamask[:].rearrange("p (a b) -> p a b", a=2)
                        kk.op("act", lambda e, pt3=pt3, bs=bs, lo=lo: e.activation(out=pt3[:, :, lo:256], in_=self.P[:, bs:bs + 2, lo:256], func=AF.Exp, scale=0.125),
                              reads=[self.PB[bs], self.PB[bs + 1]], writes=[ptr])
                        kk.op("dve", lambda e, pt3=pt3, mk3=mk3, lo=lo: e.tensor_tensor(out=pt3[:, :, lo:256], in0=pt3[:, :, lo:256], in1=mk3[:, :, lo:256], op=ALU.mult),
                              reads=[ptr, self.CR2], writes=[ptr])
                        bo = kk.bank()
                        for hh in range(2):
                            for bi, (blk, ti) in enumerate(blks):
                                kk.op("pe", lambda e, hh=hh, blk=blk, ti=ti, bi=bi, bo=bo, pt=pt: e.matmul(
                                    self.P[:, bo, hh * 65:(hh + 1) * 65], lhsT=pt[:, (hh * 2 + blk) * 128:(hh * 2 + blk + 1) * 128],
                                    rhs=vst[ti % 3][:, :], start=(bi == 0), stop=(bi == len(blks) - 1)),
                                    reads=[ptr, vstr[ti % 3]], writes=[self.PB[bo]])
                        dn, dnr = den[p2], denr[p2]
                        kk.op("dve", lambda e, dn=dn, bo=bo, c=c: e.tensor_tensor(out=dn[:], in0=self.P[:, bo, 64:130:65], in1=esk[:, 2 * c:2 * c + 2], op=ALU.add),
                              reads=[self.PB[bo], eskr], writes=[dnr])
                        kk.op("dve", lambda e, dn=dn: e.reciprocal(out=dn[:], in_=dn[:]), reads=[dnr], writes=[dnr])
                        oc, ocr = Oc[oi % 2], Ocr[oi % 2]
                        for hh in range(2):
                            kk.op("dve", lambda e, hh=hh, dn=dn, bo=bo, oc=oc: e.tensor_scalar(
                                out=oc[:, hh * 64:(hh + 1) * 64], in0=self.P[:, bo, hh * 65:hh * 65 + 64], scalar1=dn[:, hh:hh + 1], scalar2=None,
                                op0=ALU.mult),
                                reads=[self.PB[bo], dnr], writes=[ocr])
                        bt2 = kk.bank()
                        pv2 = self.pbb(bt2)
                        kk.op("pe", lambda e, pv2=pv2, oc=oc: e.transpose(out=pv2[:, 0:128], in_=oc[:], identity=self.identb[:]),
                              reads=[ocr, self.CR2], writes=[self.PB[bt2]])
                        at, atr = aT[oi % 2], aTr[oi % 2]
                        kk.op("act", lambda e, pv2=pv2, at=at: e.copy(out=at[:], in_=pv2[:, 0:128]), reads=[self.PB[bt2]], writes=[atr])
                        self.outproj_partial(i, at[:], atr, wo_, wor_)
                        oibox[0] = oi + 1

                    oibox = [oi]
                    swa_A(0)
                    for i in range(NT):
                        if i + 1 < NT:
                            swa_A(i + 1)
                        swa_B(i)
                    oi = oibox[0]
            with Phase(kk) as pb_:
                win = [pb_.sb("ewb%d" % j, [128, NCH, 384], BF16) for j in range(2)]
                winr = pb_.rs(2, "ewb")
                qbT = [pb_.sb("eqb%d" % j, [128, S], BF16) for j in range(2)]
                qbTr = [pb_.rs(NG, "eqb%d_" % j) for j in range(2)]
                kbT = [pb_.sb("ekb%d" % j, [128, S], BF16) for j in range(2)]
                kbTr = [pb_.rs(NG, "ekb%d_" % j) for j in range(2)]
                vb = [pb_.sb("evb%d" % j, [128, NT, 128], BF16) for j in range(2)]
                vbr = [pb_.rs(NT, "evb%d_" % j) for j in range(2)]
                ones = pb_.sb("eones", [128, S], BF16)
                onesr = pb_.r("eones")
                kk.op("dve", lambda e: e.memset(ones[:], 1.0), writes=[onesr])
                E = [pb_.sb("eE%d" % j, [128, S], F32) for j in range(2)]
                Er = pb_.rs(2, "eE")
                SPb = [pb_.sb("eSP%d" % j, [128, S], F32) for j in range(2)]
                SPr = pb_.rs(2, "eSP")
                CS = pb_.sb("eCS", [128, S + 1], F32)
                CSr = pb_.r("eCS")
                kk.op("dve", lambda e: e.memset(CS[:, 0:1], 0.0), writes=[CSr])
                ntot = pb_.sb("ent", [128, 1], F32)
                ntr = pb_.r("ent")
                Ab = [pb_.sb("eA%d" % j, [128, S], BF16) for j in range(2)]
                Abr = pb_.rs(2, "eA")
                AT = [pb_.sb("eAT%d" % j, [128, NT, 128], BF16) for j in range(1)]
                ATr = pb_.rs(1, "eAT")
                for c in range(4):
                    w, wr = win[c % 2], winr[c % 2]
                    kk.dma("pool", w[:, :, 0:128], wblk(w_in, 0, NCH, 768 + c * 128, 128), writes=[wr])
                    kk.dma("pool", w[:, :, 128:256], wblk(w_in, 0, NCH, 1280 + c * 128, 128), writes=[wr])
                    kk.dma("pool", w[:, :, 256:384], wblk(w_in, 0, NCH, 1792 + c * 128, 128), writes=[wr])
                    wo_, wor_ = wout[oi % 2], woutr[oi % 2]
                    kk.dma("pool", wo_[:], w_out[512 + c * 128:512 + (c + 1) * 128, :], writes=[wor_])
                    q_, q_r = qbT[c % 2], qbTr[c % 2]
                    k_, k_r = kbT[c % 2], kbTr[c % 2]
                    v_, v_r = vb[c % 2], vbr[c % 2]
                    for g in range(NG):
                        tsl = slice(g * 512, (g + 1) * 512)
                        for (dst, dstr, co) in ((q_, q_r, 0), (k_, k_r, 128)):
                            bb = kk.bank()
                            for kc in range(NCH):
                                kk.op("pe", lambda e, bb=bb, co=co, kc=kc, w=w, tsl=tsl: e.matmul(
                                    self.P[:, bb, :], lhsT=w[:, kc, co:co + 128], rhs=hT[:, kc, tsl],
                                    start=(kc == 0), stop=(kc == NCH - 1)),
                                    reads=[wr] + hTr[g * 4:(g + 1) * 4], writes=[self.PB[bb]])
                            kk.op("act", lambda e, bb=bb, dst=dst, tsl=tsl: e.copy(out=dst[:, tsl], in_=self.P[:, bb, :]),
                                  reads=[self.PB[bb]], writes=[dstr[g]])
                    for i in range(NT):
                        if i % 4 == 0:
                            bv = kk.bank()
                        for kc in range(NCH):
                            kk.op("pe", lambda e, kc=kc, bv=bv, i=i, w=w: e.matmul(
                                self.P[:, bv, (i % 4) * 128:(i % 4 + 1) * 128], lhsT=hT[:, kc, i * 128:(i + 1) * 128], rhs=w[:, kc, 256:384],
                                start=(kc == 0), stop=(kc == NCH - 1)),
                                reads=[wr, hTr[i]], writes=[self.PB[bv]])
                        kk.op("dve", lambda e, bv=bv, i=i, v_=v_: e.tensor_copy(out=v_[:, i, :], in_=self.P[:, bv, (i % 4) * 128:(i % 4 + 1) * 128]),
                              reads=[self.PB[bv]], writes=[v_r[i]])
                    its = [(n, hh) for n in range(NT) for hh in range(2)]
                    state = {}

                    def stA(k, c=c, q_=q_, q_r=q_r, k_=k_, k_r=k_r):
                        n, hh = its[k]
                        L = (n + 1) * 128
                        nb = 1 if L <= 512 else (2 if L <= 1024 else 4)
                        ps_ = slice(hh * 64, (hh + 1) * 64)
                        bz = kk.bank(nb)
                        Z = self.pb(bz, nb)
                        zr = [self.PB[bz + j] for j in range(nb)]
                        nj = (L + 511) // 512
                        for j in range(nj):
                            c0, c1 = j * 512, min(L, (j + 1) * 512)
                            kk.op("pe", lambda e, c0=c0, c1=c1: e.matmul(
                                Z[:, c0:c1], lhsT=q_[ps_, n * 128:(n + 1) * 128], rhs=k_[ps_, c0:c1], start=True, stop=True),
                                reads=[q_r[n // 4], k_r[j]], writes=[self.PB[bz + j]])
                        gi = state.setdefault("gi", 0)
                        state["gi"] = gi + 1
                        e_, e_r = E[gi % 2], Er[gi % 2]
                        sp_, sp_r = SPb[gi % 2], SPr[gi % 2]
                        ab, abr = Ab[gi % 2], Abr[gi % 2]
                        state[k] = (L, e_, e_r, sp_, sp_r, ab, abr)
                        kk.op("act", lambda e: e.activation(out=e_[:, 0:L], in_=Z[:, 0:L], func=AF.Exp, scale=0.125),
                              reads=zr[0:nj], writes=[e_r])
                        kk.op("dve", lambda e: e.tensor_tensor(out=e_[:, L - 128:L], in0=e_[:, L - 128:L], in1=self.sbmask[:], op=ALU.mult),
                              reads=[e_r, self.CR], writes=[e_r])
                        kk.op("act", lambda e: e.activation(out=sp_[:, 0:L], in_=e_[:, 0:L], func=AF.Ln, bias=1.0, scale=1.0),
                              reads=[e_r], writes=[sp_r])

                    def stB(k):
                        L, e_, e_r, sp_, sp_r, ab, abr = state[k]
                        kk.op("dve", lambda e: e.tensor_tensor_scan(out=CS[:, 1:L + 1], data0=ones[:, 0:L], data1=sp_[:, 0:L], initial=0.0,
                                                                     op0=ALU.mult, op1=ALU.add),
                              reads=[sp_r, onesr], writes=[CSr])
                        kk.op("dve", lambda e: e.tensor_scalar(out=ntot[:], in0=CS[:, L:L + 1], scalar1=-1.0, scalar2=None, op0=ALU.mult),
                              reads=[CSr], writes=[ntr])
                        kk.op("act", lambda e: e.activation(out=sp_[:, 0:L], in_=CS[:, 0:L], func=AF.Exp, bias=ntot[:, 0:1], scale=1.0),
                              reads=[CSr, ntr], writes=[sp_r])
                        kk.op("pool", lambda e: e.tensor_tensor(out=ab[:, 0:L], in0=e_[:, 0:L], in1=sp_[:, 0:L], op=ALU.mult),
                              reads=[e_r, sp_r], writes=[abr])

                    def stC(k, c=c, v_=v_, v_r=v_r, wo_=wo_, wor_=wor_):
                        n, hh = its[k]
                        L, e_, e_r, sp_, sp_r, ab, abr = state.pop(k)
                        if hh == 0:
                            state["bo"] = kk.bank()
                            kk.reserved.add(state["bo"])
                        bo = state["bo"]
                        at_, at_r = AT[0], ATr[0]
                        for k0 in range(0, n + 1, 8):
                            k1 = min(n + 1, k0 + 8)
                            bt = kk.bank()
                            while bt == bo:
                                bt = kk.bank()
                            pv = self.pbb(bt)
                            for kb in range(k0, k1):
                                kk.op("pe", lambda e, kb=kb, k0=k0, pv=pv: e.transpose(
                                    out=pv[:, (kb - k0) * 128:(kb - k0 + 1) * 128], in_=ab[:, kb * 128:(kb + 1) * 128], identity=self.identb[:]),
                                    reads=[abr, self.CR2], writes=[self.PB[bt]])
                            kk.op("act", lambda e, k0=k0, k1=k1, pv=pv: e.copy(
                                out=at_[:, k0:k1, :], in_=pv[:, 0:(k1 - k0) * 128].rearrange("p (a b) -> p a b", b=128)),
                                reads=[self.PB[bt]], writes=[at_r])
                        for kb in range(n + 1):
                            kk.op("pe", lambda e, kb=kb: e.matmul(
                                self.P[:, bo, hh * 64:(hh + 1) * 64], lhsT=at_[:, kb, :], rhs=v_[:, kb, hh * 64:(hh + 1) * 64],
                                start=(kb == 0), stop=(kb == n)),
                                reads=[at_r, v_r[kb]], writes=[self.PB[bo]])
                        if hh == 1:
                            oi = state["oi"]
                            oc, ocr = Oc[oi % 2], Ocr[oi % 2]
                            kk.op("dve", lambda e: e.tensor_copy(out=oc[:], in_=self.P[:, bo, 0:128]), reads=[self.PB[bo]], writes=[ocr])
                            kk.reserved.discard(bo)
                            bt2 = kk.bank()
                            pv2 = self.pbb(bt2)
                            kk.op("pe", lambda e: e.transpose(out=pv2[:, 0:128], in_=oc[:], identity=self.identb[:]),
                                  reads=[ocr, self.CR2], writes=[self.PB[bt2]])
                            at, atr = aT[oi % 2], aTr[oi % 2]
                            kk.op("act", lambda e: e.copy(out=at[:], in_=pv2[:, 0:128]), reads=[self.PB[bt2]], writes=[atr])
                            self.outproj_partial(n, at[:], atr, wo_, wor_)
                            state["oi"] = oi + 1

                    state["oi"] = oi
                    NI = len(its)
                    for t in range(NI + 2):
                        if 0 <= t - 2 < NI:
                            stC(t - 2)
                        if 0 <= t - 1 < NI:
                            stB(t - 1)
                        if t < NI:
                            stA(t)
                    oi = state["oi"]


def host_consts():
    p = np.arange(128)[:, None]
    i = np.arange(128)[None, :]
    ident = np.eye(128, dtype=np.float32)
    mprev = (i < p).astype(np.float32)
    mcur = (i >= p).astype(np.float32)
    swamask = np.stack([mprev, mcur, mprev, mcur], axis=1).reshape(128, 512).astype(np.float32)
    sbmask = (p > i).astype(np.float32)
    hmask = ((i >= p) & ((i // 32) == (p // 32))).astype(np.float32)
    invf = (10000.0 ** (-np.arange(0, 64, 2, dtype=np.float32) / np.float32(64))).astype(np.float32)
    invf = np.broadcast_to(invf[None, :], (128, 32)).copy()
    j4 = np.arange(4)[None, :, None]
    t4 = np.arange(128)[None, None, :]
    p4 = np.arange(128)[:, None, None]
    cmask4 = np.broadcast_to((t4 // 32) == j4, (128, 4, 128)).astype(np.float32).reshape(128, 512)
    rmask4 = np.broadcast_to((p4 // 32) == j4, (128, 4, 128)).astype(np.float32).reshape(128, 512)
    return {"c_ident": ident, "c_swamask": swamask, "c_sbmask": sbmask, "c_hmask": hmask, "c_invf": invf,
            "c_cmask4": np.ascontiguousarray(cmask4), "c_rmask4": np.ascontiguousarray(rmask4)}


_PROG = {}


def kernel(**inputs):
    n = 8
    key = "full"
    if key not in _PROG:
        _PROG[key] = Prog()
    prog = _PROG[key]
    consts = host_consts()
    per = 32 // n
    in_maps = []
    for c in range(n):
        m = {}
        for k, v in inputs.items():
            v = np.asarray(v)
            if k in ("x", "mem", "positions"):
                m[k] = np.ascontiguousarray(v[c * per:(c + 1) * per])
            else:
                m[k] = np.ascontiguousarray(v)
        m.update(consts)
        in_maps.append(m)
    res = run_bass_kernel_spmd(prog.nc, in_maps, core_ids=list(range(n)))
    return np.concatenate([r["out"] for r in res.results], axis=0).astype(np.float32)
```

```python
import contextlib
import numpy as np
import concourse.bass as bass
import concourse.mybir as mybir
from concourse.bass_utils import run_bass_kernel_spmd

F32 = mybir.dt.float32
BF16 = mybir.dt.bfloat16
I32 = mybir.dt.int32
AF = mybir.ActivationFunctionType
ALU = mybir.AluOpType
AX = mybir.AxisListType

D = 1024
NCH = 8
DFF = 2816
NFC = 22
EPS = 1e-6
N_MEM = 256
TWO_PI = 6.283185307179586
CW1 = 6.28125
CW2 = TWO_PI - 6.28125
MAGIC = 12582912.0
PI_LO = 3.1415925


class Res:
    __slots__ = ("name", "w", "rd", "dsem", "excl")

    def __init__(self, name="", excl=False):
        self.name = name
        self.w = None
        self.rd = {}
        self.dsem = None
        self.excl = excl


class DSem:
    __slots__ = ("h", "cnt", "key")

    def __init__(self, h, key):
        self.h = h
        self.cnt = 0
        self.key = key


class K:
    def __init__(self, nc, es):
        self.nc = nc
        self.es = es
        self.eng = {"pe": nc.tensor, "act": nc.scalar, "dve": nc.vector, "pool": nc.gpsimd, "sp": nc.sync}
        self.sem = {}
        self.semobj = {}
        for k in ("pe", "act", "dve", "pool"):
            h = es.enter_context(nc.semaphore("s_" + k))
            self.sem[k] = h
            self.semobj[k] = h
        self.cnt = {k: 0 for k in self.eng}
        self.seen = {k: {} for k in self.eng}
        self.free_dsems = {"sp": [], "pool": []}
        self.ndsem = 0
        self.bank_rr = 0
        self.reserved = set()
        self.n_ins = 0

    def _dsem(self, q):
        fl = self.free_dsems[q]
        if fl:
            return fl.pop()
        key = "d%d" % self.ndsem
        self.ndsem += 1
        h = self.es.enter_context(self.nc.semaphore(key))
        ds = DSem(h, key)
        self.semobj[key] = h
        return ds

    def release(self, q, res_list):
        for r in res_list:
            if r.dsem is not None and r.dsem[0] == q:
                self.free_dsems[q].append(r.dsem[1])
                r.dsem = None

    def _waits(self, e, reads, writes):
        raw = {}
        oth = {}
        for r in reads:
            if r.w is not None:
                k, v = r.w
                if raw.get(k, 0) < v:
                    raw[k] = v
            if r.excl:
                for k, v in r.rd.items():
                    if k != e and oth.get(k, 0) < v:
                        oth[k] = v
        for w in writes:
            if w.w is not None:
                k, v = w.w
                if oth.get(k, 0) < v:
                    oth[k] = v
            for k, v in w.rd.items():
                if oth.get(k, 0) < v:
                    oth[k] = v
        deps = dict(raw)
        for k, v in oth.items():
            if k == e and e == "pe":
                continue
            if deps.get(k, 0) < v:
                deps[k] = v
        if e == "pe":
            deps.pop("pe", None)
        eng = self.eng[e]
        seen = self.seen[e]
        for k, v in deps.items():
            if seen.get(k, 0) >= v:
                continue
            eng.wait_ge(self.semobj[k], v)
            seen[k] = v
            self.n_ins += 1

    def op(self, e, fn, reads=(), writes=()):
        self._waits(e, reads, writes)
        ins = fn(self.eng[e])
        self.cnt[e] += 1
        c = self.cnt[e]
        ins.then_inc(self.sem[e], 1)
        self.n_ins += 1
        for r in reads:
            if r.rd.get(e, 0) < c:
                r.rd[e] = c
        for w in writes:
            w.w = (e, c)
            w.rd = {}
        return ins

    def dma(self, q, out, in_, reads=(), writes=(), owner=None):
        self._waits(q, reads, writes)
        if owner is None:
            owner = writes[0] if writes else reads[0]
        if owner.dsem is None or owner.dsem[0] != q:
            assert owner.dsem is None
            owner.dsem = (q, self._dsem(q))
        ds = owner.dsem[1]
        ins = self.eng[q].dma_start(out=out, in_=in_)
        ins.then_inc(ds.h, 16)
        ds.cnt += 16
        self.n_ins += 1
        tok = (ds.key, ds.cnt)
        for r in reads:
            if r.rd.get(ds.key, 0) < ds.cnt:
                r.rd[ds.key] = ds.cnt
        for w in writes:
            w.w = tok
            w.rd = {}
        return ins

    def barrier(self):
        ce = ("pe", "act", "dve", "pool")
        for a in ce + ("sp",):
            for b in ce:
                if a == b:
                    continue
                v = self.cnt[b]
                if v and self.seen[a].get(b, 0) < v:
                    self.eng[a].wait_ge(self.sem[b], v)
                    self.seen[a][b] = v
                    self.n_ins += 1

    def wait_all(self, e, res_list):
        self._waits(e, [], res_list)

    def bank(self, k=1):
        for _ in range(16):
            b = self.bank_rr
            if b % k:
                b += k - (b % k)
            if b + k > 8:
                b = 0
            self.bank_rr = (b + k) % 8
            if not any((b + j) in self.reserved for j in range(k)):
                return b
        raise RuntimeError("no free PSUM bank")


class Phase:
    uid = 0

    def __init__(self, kk):
        self.kk = kk
        self.es = contextlib.ExitStack()
        self.res = []

    def __enter__(self):
        self.es.__enter__()
        return self

    def sb(self, name, shape, dtype):
        Phase.uid += 1
        return self.es.enter_context(self.kk.nc.sbuf_tensor("%s_u%d" % (name, Phase.uid), list(shape), dtype))

    def r(self, name=""):
        x = Res(name)
        self.res.append(x)
        return x

    def rs(self, n, name=""):
        return [self.r(name + str(i)) for i in range(n)]

    def __exit__(self, *a):
        kk = self.kk
        kk.barrier()
        for q in ("sp", "pool"):
            kk.release(q, self.res)
        return self.es.__exit__(*a)


def wblk(w2d, kc0, nkc, c0, ncols):
    return w2d.rearrange("(kc p) n -> p kc n", p=128)[:, kc0:kc0 + nkc, c0:c0 + ncols]


class Prog:
    def __init__(self, S=2048, NSEQ=4, layers=(0, 1, 2, 3), dbg=False, parts=("mix", "xa", "ffn")):
        self.S = S
        self.NT = S // 128
        self.NG = S // 512
        self.NSEQ = NSEQ
        self.layers = layers
        self.dbg = dbg
        self.parts = parts
        self.nc = bass.Bass("TRN2", target_bir_lowering=False)
        self.build()

    def din(self, name, shape, dt=F32):
        return self.nc.dram_tensor(name, list(shape), dt, kind="ExternalInput").ap()

    def build(self):
        nc, S, NT, NSEQ = self.nc, self.S, self.NT, self.NSEQ
        self.x_d = self.din("x", [NSEQ, S, D])
        self.mem_d = self.din("mem", [NSEQ, N_MEM, D])
        self.pos_d = self.din("positions", [NSEQ, S], I32)
        self.norm_mix = self.din("norm_mix", [4, D])
        self.norm_cross = self.din("norm_cross", [4, D])
        self.norm_mem = self.din("norm_mem", [4, D])
        self.norm_ffn = self.din("norm_ffn", [4, D])
        self.ab_w_in = self.din("ab_w_in", [2, D, 2304])
        self.ab_w_out = self.din("ab_w_out", [2, D, D])
        self.swa_q_norm = self.din("swa_q_norm", [2, 64])
        self.swa_k_norm = self.din("swa_k_norm", [2, 64])
        self.swa_sinks = self.din("swa_sinks", [2, 8])
        self.hgrn_w_in = self.din("hgrn_w_in", [2, D, 4096])
        self.hgrn_w_out = self.din("hgrn_w_out", [2, D, D])
        self.hgrn_o_norm = self.din("hgrn_o_norm", [2, 128])
        self.hgrn_lb = self.din("hgrn_lb", [4, D])
        self.xa_w_q = self.din("xa_w_q", [4, D, D])
        self.xa_w_kv = self.din("xa_w_kv", [4, D, 2 * D])
        self.xa_w_o = self.din("xa_w_o", [4, D, D])
        self.xa_q_norm = self.din("xa_q_norm", [4, 256])
        self.xa_k_norm = self.din("xa_k_norm", [4, 256])
        self.ffn_w_up = self.din("ffn_w_up", [4, D, 2 * DFF])
        self.ffn_conv_w = self.din("ffn_conv_w", [4, 3, 2 * DFF])
        self.ffn_conv_b = self.din("ffn_conv_b", [4, 2 * DFF])
        self.ffn_w_down = self.din("ffn_w_down", [4, DFF, D])
        self.c_ident = self.din("c_ident", [128, 128])
        self.c_swamask = self.din("c_swamask", [128, 512])
        self.c_sbmask = self.din("c_sbmask", [128, 128])
        self.c_hmask = self.din("c_hmask", [128, 128])
        self.c_invf = self.din("c_invf", [128, 32])
        self.c_sbneg = self.din("c_sbneg", [128, 128])
        self.c_cmask4 = self.din("c_cmask4", [128, 512])
        self.c_rmask4 = self.din("c_rmask4", [128, 512])
        self.out_d = nc.dram_tensor("out", [NSEQ, S, D], F32, kind="ExternalOutput").ap()
        if self.dbg:
            self.dbg_d = nc.dram_tensor("dbg", [16, S, D], F32, kind="ExternalOutput").ap()
            self.ndbg = 0

        with contextlib.ExitStack() as es:
            self.es = es
            kk = self.kk = K(nc, es)
            sb = lambda name, shape, dt: es.enter_context(nc.sbuf_tensor(name, list(shape), dt))
            self.X = sb("X", [128, NT, D], F32)
            self.XR = [Res("X%d" % i) for i in range(NT)]
            self.P = es.enter_context(nc.psum_tensor("P", [128, 8, 512], F32))
            self.PB = [Res("B%d" % i, excl=True) for i in range(8)]
            self.identf = sb("identf", [128, 128], F32)
            self.identb = sb("identb", [128, 128], BF16)
            self.onesb = sb("onesb", [128, 128], BF16)
            self.swamask = sb("swamask", [128, 512], BF16)
            self.sbmask = sb("sbmask", [128, 128], F32)
            self.hmask = sb("hmask", [128, 128], F32)
            self.invf = sb("invf", [128, 32], F32)
            self.lbt = sb("lbt", [128, 32], F32)
            self.omlt = sb("omlt", [128, 32], F32)
            self.nomlt = sb("nomlt", [128, 32], F32)
            self.CR = Res("consts")
            cr = [self.CR]
            kk.dma("sp", self.identf[:], self.c_ident, writes=cr)
            kk.dma("sp", self.sbmask[:], self.c_sbmask, writes=cr)
            kk.dma("sp", self.hmask[:], self.c_hmask, writes=cr)
            kk.dma("sp", self.invf[:], self.c_invf, writes=cr)
            self.CR2 = Res("consts2")
            kk.dma("pool", self.identb[:], self.c_ident, writes=[self.CR2])
            kk.dma("pool", self.swamask[:], self.c_swamask, writes=[self.CR2])
            self.sbneg = sb("sbneg", [128, 128], BF16)
            kk.dma("pool", self.sbneg[:], self.c_sbneg, writes=[self.CR2])
            self.cmask4 = sb("cmask4", [128, 512], BF16)
            self.rmask4 = sb("rmask4", [128, 512], BF16)
            kk.dma("pool", self.cmask4[:], self.c_cmask4, writes=[self.CR2])
            kk.dma("pool", self.rmask4[:], self.c_rmask4, writes=[self.CR2])
            kk.op("dve", lambda e: e.memset(self.onesb[:], 1.0), writes=[self.CR2])
            self.setup_lb()
            for s in range(NSEQ):
                self.run_seq(s)
            kk.wait_all("sp", self.XR)
            kk.barrier()

    def pb(self, b, k=1):
        if k == 1:
            return self.P[:, b, :]
        return self.P[:, b:b + k, :].rearrange("p b f -> p (b f)")

    def pbb(self, b):
        return self.P[:, b, :].bitcast(BF16)

    def setup_lb(self):
        kk = self.kk
        with Phase(kk) as ph:
            raw = ph.sb("lbraw", [32, 128], F32)
            rr = ph.r("lbraw")
            kk.dma("sp", raw[:], self.hgrn_lb.rearrange("l (c p) -> (l c) p", p=128), writes=[rr])
            b = kk.bank()
            kk.op("pe", lambda e: e.transpose(out=self.P[:, b, 0:32], in_=raw[:], identity=self.identf[0:32, 0:32]),
                  reads=[rr, self.CR], writes=[self.PB[b]])
            xs = ph.sb("lbx", [128, 32], F32)
            r2 = ph.r("lbx")
            kk.op("act", lambda e: e.copy(out=xs[:], in_=self.P[:, b, 0:32]), reads=[self.PB[b]], writes=[r2])
            mx = ph.sb("lbmx", [128, 8], F32)
            kk.op("dve", lambda e: e.tensor_max(out=mx[:], in0=xs[:, 0:8], in1=xs[:, 8:16]), reads=[r2], writes=[r2])
            kk.op("dve", lambda e: e.tensor_max(out=mx[:], in0=mx[:], in1=xs[:, 16:24]), reads=[r2], writes=[r2])
            kk.op("dve", lambda e: e.tensor_max(out=mx[:], in0=mx[:], in1=xs[:, 24:32]), reads=[r2], writes=[r2])
            ex = ph.sb("lbex", [128, 32], F32)
            for l in range(4):
                kk.op("dve", lambda e, l=l: e.tensor_sub(out=ex[:, l * 8:(l + 1) * 8], in0=xs[:, l * 8:(l + 1) * 8], in1=mx[:]),
                      reads=[r2], writes=[r2])
            kk.op("act", lambda e: e.activation(out=ex[:], in_=ex[:], func=AF.Exp), reads=[r2], writes=[r2])
            sm = ph.sb("lbsm", [128, 8], F32)
            kk.op("dve", lambda e: e.tensor_add(out=sm[:], in0=ex[:, 0:8], in1=ex[:, 8:16]), reads=[r2], writes=[r2])
            kk.op("dve", lambda e: e.tensor_add(out=sm[:], in0=sm[:], in1=ex[:, 16:24]), reads=[r2], writes=[r2])
            kk.op("dve", lambda e: e.tensor_add(out=sm[:], in0=sm[:], in1=ex[:, 24:32]), reads=[r2], writes=[r2])
            kk.op("dve", lambda e: e.reciprocal(out=sm[:], in_=sm[:]), reads=[r2], writes=[r2])
            for l in range(4):
                kk.op("dve", lambda e, l=l: e.tensor_mul(out=ex[:, l * 8:(l + 1) * 8], in0=ex[:, l * 8:(l + 1) * 8], in1=sm[:]),
                      reads=[r2], writes=[r2])
            lr = self.LBR = Res("lb")
            kk.op("dve", lambda e: e.memset(self.lbt[:, 0:8], 0.0), reads=[r2], writes=[lr])
            kk.op("dve", lambda e: e.tensor_copy(out=self.lbt[:, 8:16], in_=ex[:, 8:16]), reads=[r2, lr], writes=[lr])
            kk.op("dve", lambda e: e.tensor_add(out=self.lbt[:, 16:24], in0=self.lbt[:, 8:16], in1=ex[:, 16:24]), reads=[r2, lr], writes=[lr])
            kk.op("dve", lambda e: e.tensor_add(out=self.lbt[:, 24:32], in0=self.lbt[:, 16:24], in1=ex[:, 24:32]), reads=[r2, lr], writes=[lr])
            kk.op("dve", lambda e: e.tensor_scalar(out=self.omlt[:], in0=self.lbt[:], scalar1=-1.0, scalar2=1.0, op0=ALU.mult, op1=ALU.add),
                  reads=[lr], writes=[lr])
            kk.op("dve", lambda e: e.tensor_scalar(out=self.nomlt[:], in0=self.lbt[:], scalar1=1.0, scalar2=-1.0, op0=ALU.mult, op1=ALU.add),
                  reads=[lr], writes=[lr])

    def load_gain_bc(self, ph, name, row_ap, n):
        t = ph.sb(name, [128, n], F32)
        r = ph.r(name)
        self.kk.dma("sp", t[:], row_ap.partition_broadcast(128), writes=[r])
        return t, r

    def rstd_from_ss(self, ss_ap, rstd_ap, n, res):
        kk = self.kk
        kk.op("act", lambda e: e.activation(out=rstd_ap, in_=ss_ap, func=AF.Ln, bias=self.epsb[:, 0:1], scale=1.0 / n),
              reads=[res, self.CR3], writes=[res])
        kk.op("act", lambda e: e.activation(out=rstd_ap, in_=rstd_ap, func=AF.Exp, scale=-0.5), reads=[res], writes=[res])

    def norm_T(self, ph, tag, srcs, gain, gain_r, dstT, dst_rs, col0=0):
        kk = self.kk
        n = len(srcs)
        ss = ph.sb(tag + "ss", [128, n], F32)
        ssr = ph.r(tag + "ss")
        junk = ph.sb(tag + "junk", [128, D], BF16)
        jr = ph.r(tag + "junk")
        hn = [ph.sb(tag + "hn%d" % j, [128, D], BF16) for j in range(2)]
        hnr = ph.rs(2, tag + "hn")
        for i, (ap, r) in enumerate(srcs):
            kk.op("act", lambda e, ap=ap, i=i: e.activation(out=junk[:], in_=ap, func=AF.Square, accum_out=ss[:, i:i + 1]),
                  reads=[r], writes=[jr, ssr])
        self.rstd_from_ss(ss[:], ss[:], D, ssr)
        for i, (ap, r) in enumerate(srcs):
            h, hr = hn[i % 2], hnr[i % 2]
            kk.op("dve", lambda e, ap=ap, i=i, h=h: e.scalar_tensor_tensor(out=h[:], in0=ap, scalar=ss[:, i:i + 1], in1=gain[:],
                                                                         op0=ALU.mult, op1=ALU.mult),
                  reads=[r, ssr, gain_r], writes=[hr])
            b = kk.bank()
            pv = self.pbb(b)
            for c in range(NCH):
                kk.op("pe", lambda e, c=c, h=h, pv=pv: e.transpose(out=pv[:, c * 128:(c + 1) * 128], in_=h[:, c * 128:(c + 1) * 128],
                                                                  identity=self.identb[:]),
                      reads=[hr, self.CR2], writes=[self.PB[b]])
            kk.op("act", lambda e, i=i, pv=pv: e.copy(out=dstT[:, :, col0 + i * 128: col0 + (i + 1) * 128],
                                                      in_=pv.rearrange("p (c t) -> p c t", c=NCH)),
                  reads=[self.PB[b]], writes=[dst_rs[i]])

    def resid_add(self, i, b, hf):
        kk = self.kk
        xs = self.X[:, i, hf * 512:(hf + 1) * 512]
        kk.op("dve", lambda e: e.tensor_tensor(out=xs, in0=xs, in1=self.P[:, b, :], op=ALU.add),
              reads=[self.PB[b], self.XR[i]], writes=[self.XR[i]])

    def dump(self):
        if not self.dbg:
            return
        k = self.ndbg
        self.ndbg += 1
        for i in range(self.NT):
            self.kk.dma("sp", self.dbg_d[k, i * 128:(i + 1) * 128, :], self.X[:, i, :], reads=[self.XR[i]], owner=self.XR[i])

    def run_seq(self, s):
        kk, NT = self.kk, self.NT
        if s == 0:
            self.epsb = self.es.enter_context(self.nc.sbuf_tensor("epsb", [128, 1], F32))
            self.CR3 = Res("eps")
            kk.op("dve", lambda e: e.memset(self.epsb[:], EPS), writes=[self.CR3])
        for i in range(NT):
            kk.dma("sp", self.X[:, i, :], self.x_d[s, i * 128:(i + 1) * 128, :], writes=[self.XR[i]])
        for l in self.layers:
            if "mix" in self.parts:
                if l % 2 == 0:
                    self.even_mixer(s, l)
                else:
                    self.odd_mixer(s, l)
                self.dump()
            if "xa" in self.parts:
                self.xattn(s, l)
                self.dump()
            if "ffn" in self.parts:
                self.ffn(s, l)
                self.dump()
        for i in range(NT):
            kk.dma("sp", self.out_d[s, i * 128:(i + 1) * 128, :], self.X[:, i, :], reads=[self.XR[i]], owner=self.XR[i])

    def ffn(self, s, l):
        kk, S, NT, NG = self.kk, self.S, self.NT, self.NG
        with Phase(kk) as ph:
            gain, gr = self.load_gain_bc(ph, "fg", self.norm_ffn[l], D)
            hT = ph.sb("fhT", [128, NCH, S], BF16)
            hTr = ph.rs(NT, "fhT")
            self.norm_T(ph, "fn", [(self.X[:, i, :], self.XR[i]) for i in range(NT)], gain, gr, hT, hTr)
            cst = ph.sb("cst", [128, 3, 128], F32)
            cstr = ph.r("cst")
            cw = self.ffn_conv_w[l].rearrange("t (c p) -> (t c) p", p=128)
            cb = self.ffn_conv_b[l].rearrange("(c p) -> c p", p=128)
            kk.dma("sp", cst[:, 0, :], cw[0:128, :], writes=[cstr])
            kk.dma("sp", cst[0:4, 1, :], cw[128:132, :], writes=[cstr])
            kk.dma("sp", cst[0:44, 2, :], cb, writes=[cstr])
            cwb = ph.sb("cwb", [128, 176], F32)
            cwr = ph.r("cwb")
            b = kk.bank()
            kk.op("pe", lambda e: e.transpose(out=self.P[:, b, 0:128], in_=cst[:, 0, :], identity=self.identf[:]),
                  reads=[cstr, self.CR], writes=[self.PB[b]])
            kk.op("pe", lambda e: e.transpose(out=self.P[:, b, 128:132], in_=cst[0:4, 1, :], identity=self.identf[0:4, 0:4]),
                  reads=[cstr, self.CR], writes=[self.PB[b]])
            kk.op("pe", lambda e: e.transpose(out=self.P[:, b, 132:176], in_=cst[0:44, 2, :], identity=self.identf[0:44, 0:44]),
                  reads=[cstr, self.CR], writes=[self.PB[b]])
            kk.op("act", lambda e: e.copy(out=cwb[:], in_=self.P[:, b, 0:176]), reads=[self.PB[b]], writes=[cwr])

            NQ = 4
            qchunks = [list(range(0, 6)), list(range(6, 11)), list(range(11, 17)), list(range(17, 22))]
            yT = ph.sb("yT", [128, 6, S], BF16)
            yTr = [ph.rs(NG, "yT%d_" % j) for j in range(6)]
            NWB = 3
            wup = [ph.sb("wup%d" % j, [128, NCH, 256], BF16) for j in range(NWB)]
            wupr = ph.rs(NWB, "wup")
            wdn = [ph.sb("wdn%d" % j, [128, 6, D], BF16) for j in range(2)]
            wdnr = ph.rs(2, "wdn")
            NU = 3
            U = [ph.sb("U%d" % j, [128, 2, 514], F32) for j in range(NU)]
            Ur = ph.rs(NU, "U")
            TG = [ph.sb("TG%d" % j, [128, 2, 512], F32) for j in range(3)]
            TGr = ph.rs(3, "TG")
            SG = [ph.sb("SG%d" % j, [128, 512], F32) for j in range(2)]
            SGr = ph.rs(2, "SG")
            wup_d = self.ffn_w_up[l]
            wdn_d = self.ffn_w_down[l]
            ui = 0
            wi = 0
            ti = 0
            pend = []
            for qi, chunks in enumerate(qchunks):
                nq = len(chunks)
                wd, wdr = wdn[qi % 2], wdnr[qi % 2]
                kk.dma("pool", wd[:, 0:nq, :], wblk(wdn_d, chunks[0], nq, 0, D), writes=[wdr])
                for ci, c in enumerate(chunks):
                    w, wr = wup[wi % NWB], wupr[wi % NWB]
                    wi += 1
                    kk.dma("pool", w[:, :, 0:128], wblk(wup_d, 0, NCH, c * 128, 128), writes=[wr])
                    kk.dma("pool", w[:, :, 128:256], wblk(wup_d, 0, NCH, DFF + c * 128, 128), writes=[wr])
                    prevU = None
                    for st in range(NG):
                        bg = kk.bank()
                        bu = kk.bank()
                        for (bb, co) in ((bg, 0), (bu, 128)):
                            for kc in range(NCH):
                                kk.op("pe", lambda e, bb=bb, co=co, kc=kc, w=w, st=st: e.matmul(
                                    self.P[:, bb, :], lhsT=w[:, kc, co:co + 128], rhs=hT[:, kc, st * 512:(st + 1) * 512],
                                    start=(kc == 0), stop=(kc == NCH - 1)),
                                    reads=[wr] + hTr[st * 4:(st + 1) * 4], writes=[self.PB[bb]])
                        u, ur = U[ui % NU], Ur[ui % NU]
                        ui += 1
                        kk.op("act", lambda e, u=u, bg=bg: e.copy(out=u[:, 0, 2:514], in_=self.P[:, bg, :]),
                              reads=[self.PB[bg]], writes=[ur])
                        kk.op("act", lambda e, u=u, bu=bu: e.copy(out=u[:, 1, 2:514], in_=self.P[:, bu, :]),
                              reads=[self.PB[bu]], writes=[ur])
                        if prevU is not None:
                            pu, pur = prevU
                            kk.op("act", lambda e, u=u, pu=pu: e.copy(out=u[:, :, 0:2], in_=pu[:, :, 512:514]),
                                  reads=[pur, ur], writes=[ur])
                        else:
                            kk.op("dve", lambda e, u=u: e.memset(u[:, :, 0:2], 0.0), reads=[ur], writes=[ur])
                        prevU = (u, ur)
                        tg, tgr = TG[ti % 3], TGr[ti % 3]
                        sg, sgr = SG[ti % 2], SGr[ti % 2]
                        ti += 1
                        for gi, fc in ((0, c), (1, NFC + c)):
                            w2 = cwb[:, 2 * 44 + fc:2 * 44 + fc + 1]
                            w1 = cwb[:, 1 * 44 + fc:1 * 44 + fc + 1]
                            w0 = cwb[:, 0 * 44 + fc:0 * 44 + fc + 1]
                            bb_ = cwb[:, 3 * 44 + fc:3 * 44 + fc + 1]
                            bsrc = bg if gi == 0 else bu
                            kk.op("act", lambda e, tg=tg, gi=gi, w2=w2, bb_=bb_, bsrc=bsrc: e.activation(
                                out=tg[:, gi, :], in_=self.P[:, bsrc, :], func=AF.Identity, bias=bb_, scale=w2),
                                reads=[self.PB[bsrc], cwr], writes=[tgr])
                            kk.op("dve", lambda e, u=u, tg=tg, gi=gi, w1=w1: e.scalar_tensor_tensor(
                                out=tg[:, gi, :], in0=u[:, gi, 1:513], scalar=w1, in1=tg[:, gi, :], op0=ALU.mult, op1=ALU.add),
                                reads=[ur, cwr, tgr], writes=[tgr])
                            kk.op("dve", lambda e, u=u, tg=tg, gi=gi, w0=w0: e.scalar_tensor_tensor(
                                out=tg[:, gi, :], in0=u[:, gi, 0:512], scalar=w0, in1=tg[:, gi, :], op0=ALU.mult, op1=ALU.add),
                                reads=[ur, cwr, tgr], writes=[tgr])
                        def fin(sg=sg, tg=tg, ci=ci, st=st, sgr=sgr, tgr=tgr):
                            kk.op("act", lambda e: e.activation(out=sg[:], in_=tg[:, 0, :], func=AF.Silu),
                                  reads=[tgr], writes=[sgr])
                            kk.op("dve", lambda e: e.tensor_tensor(
                                out=yT[:, ci, st * 512:(st + 1) * 512], in0=sg[:], in1=tg[:, 1, :], op=ALU.mult),
                                reads=[sgr, tgr], writes=[yTr[ci][st]])
                        while pend:
                            pend.pop(0)()
                        pend.append(fin)
                while pend:
                    pend.pop(0)()
                for i in range(NT):
                    for hf in range(2):
                        b = kk.bank()
                        for ci in range(nq):
                            kk.op("pe", lambda e, b=b, ci=ci, i=i, hf=hf, wd=wd: e.matmul(
                                self.P[:, b, :], lhsT=yT[:, ci, i * 128:(i + 1) * 128], rhs=wd[:, ci, hf * 512:(hf + 1) * 512],
                                start=(ci == 0), stop=(ci == nq - 1)),
                                reads=[wdr, yTr[ci][i // 4]], writes=[self.PB[b]])
                        self.resid_add(i, b, hf)

    def headnorm(self, ph, tag, bank0, nh, hd, gain, gain_r, out, out_r, extra_reads=()):
        kk = self.kk
        nb = (nh * hd) // 512
        src = self.pb(bank0, nb)
        pbr = [self.PB[bank0 + j] for j in range(nb)]
        ss = ph.sb(tag + "ss", [128, nh], F32)
        ssr = ph.r(tag + "ss")
        junk = ph.sb(tag + "jk", [128, hd], BF16)
        jr = ph.r(tag + "jk")
        for h in range(nh):
            kk.op("act", lambda e, h=h: e.activation(out=junk[:], in_=src[:, h * hd:(h + 1) * hd], func=AF.Square,
                                                     accum_out=ss[:, h:h + 1]),
                  reads=pbr, writes=[jr, ssr])
        self.rstd_from_ss(ss[:], ss[:], hd, ssr)
        for h in range(nh):
            kk.op("dve", lambda e, h=h: e.scalar_tensor_tensor(out=out[:, h * hd:(h + 1) * hd], in0=src[:, h * hd:(h + 1) * hd],
                                                               scalar=ss[:, h:h + 1], in1=gain[:], op0=ALU.mult, op1=ALU.mult),
                  reads=pbr + [ssr, gain_r], writes=[out_r])

    def xattn(self, s, l):
        kk, S, NT, NG = self.kk, self.S, self.NT, self.NG
        with Phase(kk) as ph0:
            kT = ph0.sb("xkT", [128, NCH, N_MEM], BF16)
            kTr = ph0.r("xkT")
            Vt = ph0.sb("xV", [128, 2, D], BF16)
            Vr = ph0.r("xV")
            with Phase(kk) as ph:
                gm, gmr = self.load_gain_bc(ph, "gm", self.norm_mem[l], D)
                gk, gkr = self.load_gain_bc(ph, "gk", self.xa_k_norm[l], 256)
                mem = ph.sb("mem", [128, 2, D], F32)
                memr = ph.rs(2, "mem")
                for mt in range(2):
                    kk.dma("sp", mem[:, mt, :], self.mem_d[s, mt * 128:(mt + 1) * 128, :], writes=[memr[mt]])
                memT = ph.sb("memT", [128, NCH, N_MEM], BF16)
                memTr = ph.rs(2, "memT")
                self.norm_T(ph, "mn", [(mem[:, mt, :], memr[mt]) for mt in range(2)], gm, gmr, memT, memTr)
                wkv = [ph.sb("wkv%d" % j, [128, NCH, 512], BF16) for j in range(4)]
                wkvr = ph.rs(4, "wkv")
                for j in range(4):
                    kk.dma("pool", wkv[j][:], wblk(self.xa_w_kv[l], 0, NCH, j * 512, 512), writes=[wkvr[j]])
                kn = ph.sb("kn", [128, D], BF16)
                knr = ph.r("kn")
                for mt in range(2):
                    b0 = kk.bank(2)
                    for hf in range(2):
                        for kc in range(NCH):
                            kk.op("pe", lambda e, hf=hf, kc=kc, mt=mt, b0=b0: e.matmul(
                                self.P[:, b0 + hf, :], lhsT=memT[:, kc, mt * 128:(mt + 1) * 128], rhs=wkv[hf][:, kc, :],
                                start=(kc == 0), stop=(kc == NCH - 1)),
                                reads=[memTr[mt], wkvr[hf]], writes=[self.PB[b0 + hf]])
                    self.headnorm(ph, "kh%d" % mt, b0, 4, 256, gk, gkr, kn, knr)
                    b = kk.bank()
                    pv = self.pbb(b)
                    for c in range(NCH):
                        kk.op("pe", lambda e, c=c, pv=pv: e.transpose(out=pv[:, c * 128:(c + 1) * 128], in_=kn[:, c * 128:(c + 1) * 128],
                                                                      identity=self.identb[:]),
                              reads=[knr, self.CR2], writes=[self.PB[b]])
                    kk.op("act", lambda e, mt=mt, pv=pv: e.copy(out=kT[:, :, mt * 128:(mt + 1) * 128],
                                                              in_=pv.rearrange("p (c t) -> p c t", c=NCH)),
                          reads=[self.PB[b]], writes=[kTr])
                    b1 = kk.bank(2)
                    for hf in range(2):
                        for kc in range(NCH):
                            kk.op("pe", lambda e, hf=hf, kc=kc, mt=mt, b1=b1: e.matmul(
                                self.P[:, b1 + hf, :], lhsT=memT[:, kc, mt * 128:(mt + 1) * 128], rhs=wkv[2 + hf][:, kc, :],
                                start=(kc == 0), stop=(kc == NCH - 1)),
                                reads=[memTr[mt], wkvr[2 + hf]], writes=[self.PB[b1 + hf]])
                    kk.op("act", lambda e, mt=mt, b1=b1: e.copy(out=Vt[:, mt, :], in_=self.pb(b1, 2)),
                          reads=[self.PB[b1], self.PB[b1 + 1]], writes=[Vr])
            with Phase(kk) as ph:
                gc, gcr = self.load_gain_bc(ph, "gc", self.norm_cross[l], D)
                gq, gqr = self.load_gain_bc(ph, "gq", self.xa_q_norm[l], 256)
                wq = ph.sb("wq", [128, NCH, D], BF16)
                wqr = ph.r("wq")
                wo = ph.sb("wo", [128, NCH, D], BF16)
                wor = ph.r("wo")
                for hf in range(2):
                    kk.dma("pool", wq[:, :, hf * 512:(hf + 1) * 512], wblk(self.xa_w_q[l], 0, NCH, hf * 512, 512), writes=[wqr])
                for hf in range(2):
                    kk.dma("pool", wo[:, :, hf * 512:(hf + 1) * 512], wblk(self.xa_w_o[l], 0, NCH, hf * 512, 512), writes=[wor])
                hTg = [ph.sb("xh%d" % j, [128, NCH, 512], BF16) for j in range(2)]
                hTgr = [ph.rs(4, "xh%d_" % j) for j in range(2)]
                qTg = [ph.sb("xq%d" % j, [128, NCH, 512], BF16) for j in range(2)]
                qTgr = [ph.rs(4, "xq%d_" % j) for j in range(2)]
                oTg = [ph.sb("xo%d" % j, [128, NCH, 512], BF16) for j in range(2)]
                oTgr = [ph.rs(NCH, "xo%d_" % j) for j in range(2)]
                qn = [ph.sb("xqn%d" % j, [128, D], BF16) for j in range(2)]
                qnr = ph.rs(2, "xqn")
                PT = [ph.sb("xPT%d" % j, [128, 2, 512], BF16) for j in range(2)]
                PTr = ph.rs(2, "xPT")
                rden = [ph.sb("xrd%d" % j, [128, 512], F32) for j in range(2)]
                rdr = ph.rs(2, "xrd")
                pibox = [0]

                def xa_A(g):
                    hT, hTr = hTg[g % 2], hTgr[g % 2]
                    qT, qTr = qTg[g % 2], qTgr[g % 2]
                    self.norm_T(ph, "xn%d_" % g, [(self.X[:, g * 4 + j, :], self.XR[g * 4 + j]) for j in range(4)], gc, gcr, hT, hTr)
                    for j in range(4):
                        b0 = kk.bank(2)
                        for hf in range(2):
                            for kc in range(NCH):
                                kk.op("pe", lambda e, hf=hf, kc=kc, j=j, b0=b0, hT=hT: e.matmul(
                                    self.P[:, b0 + hf, :], lhsT=hT[:, kc, j * 128:(j + 1) * 128], rhs=wq[:, kc, hf * 512:(hf + 1) * 512],
                                    start=(kc == 0), stop=(kc == NCH - 1)),
                                    reads=[hTr[j], wqr], writes=[self.PB[b0 + hf]])
                        q_, q_r = qn[j % 2], qnr[j % 2]
                        self.headnorm(ph, "qh%d_%d" % (g, j), b0, 4, 256, gq, gqr, q_, q_r)
                        b = kk.bank()
                        pv = self.pbb(b)
                        for c in range(NCH):
                            kk.op("pe", lambda e, c=c, pv=pv, q_=q_: e.transpose(out=pv[:, c * 128:(c + 1) * 128],
                                                                              in_=q_[:, c * 128:(c + 1) * 128], identity=self.identb[:]),
                                  reads=[q_r, self.CR2], writes=[self.PB[b]])
                        kk.op("act", lambda e, j=j, pv=pv, qT=qT: e.copy(out=qT[:, :, j * 128:(j + 1) * 128],
                                                                      in_=pv.rearrange("p (c t) -> p c t", c=NCH)),
                              reads=[self.PB[b]], writes=[qTr[j]])
                def xa_B(g):
                    qT, qTr = qTg[g % 2], qTgr[g % 2]
                    oT, oTr = oTg[g % 2], oTgr[g % 2]
                    for h in range(4):
                        pi = pibox[0]
                        pt, ptr = PT[pi % 2], PTr[pi % 2]
                        rd, rdr_ = rden[pi % 2], rdr[pi % 2]
                        pibox[0] = pi + 1
                        b0 = kk.bank(2)
                        for mt in range(2):
                            for hh in range(2):
                                kk.op("pe", lambda e, mt=mt, hh=hh, h=h, b0=b0, qT=qT: e.matmul(
                                    self.P[:, b0 + mt, :], lhsT=kT[:, h * 2 + hh, mt * 128:(mt + 1) * 128], rhs=qT[:, h * 2 + hh, :],
                                    start=(hh == 0), stop=(hh == 1)),
                                    reads=[kTr] + qTr, writes=[self.PB[b0 + mt]])
                        kk.op("act", lambda e, b0=b0, pt=pt: e.activation(out=pt[:].rearrange("p a b -> p (a b)"), in_=self.pb(b0, 2),
                                                                        func=AF.Exp, scale=1.0 / 16.0),
                              reads=[self.PB[b0], self.PB[b0 + 1]], writes=[ptr])
                        bd = kk.bank()
                        for mt in range(2):
                            kk.op("pe", lambda e, mt=mt, bd=bd, pt=pt: e.matmul(self.P[:, bd, :], lhsT=self.onesb[:], rhs=pt[:, mt, :],
                                                                             start=(mt == 0), stop=(mt == 1)),
                                  reads=[ptr, self.CR2], writes=[self.PB[bd]])
                        kk.op("dve", lambda e, bd=bd, rd=rd: e.reciprocal(out=rd[:], in_=self.P[:, bd, :]),
                              reads=[self.PB[bd]], writes=[rdr_])
                        for hh in range(2):
                            bo = kk.bank()
                            for mt in range(2):
                                kk.op("pe", lambda e, mt=mt, bo=bo, pt=pt, h=h, hh=hh: e.matmul(
                                    self.P[:, bo, :], lhsT=Vt[:, mt, h * 256 + hh * 128:h * 256 + (hh + 1) * 128], rhs=pt[:, mt, :],
                                    start=(mt == 0), stop=(mt == 1)),
                                    reads=[ptr, Vr], writes=[self.PB[bo]])
                            kk.op("dve", lambda e, bo=bo, rd=rd, oT=oT, h=h, hh=hh: e.tensor_tensor(
                                out=oT[:, h * 2 + hh, :], in0=self.P[:, bo, :], in1=rd[:], op=ALU.mult),
                                reads=[self.PB[bo], rdr_], writes=[oTr[h * 2 + hh]])
                    for j in range(4):
                        i = g * 4 + j
                        for hf in range(2):
                            b = kk.bank()
                            for c in range(NCH):
                                kk.op("pe", lambda e, c=c, b=b, j=j, hf=hf, oT=oT: e.matmul(
                                    self.P[:, b, :], lhsT=oT[:, c, j * 128:(j + 1) * 128], rhs=wo[:, c, hf * 512:(hf + 1) * 512],
                                    start=(c == 0), stop=(c == NCH - 1)),
                                    reads=[oTr[c], wor], writes=[self.PB[b]])
                            self.resid_add(i, b, hf)

                xa_A(0)
                for g in range(NG):
                    if g + 1 < NG:
                        xa_A(g + 1)
                    xa_B(g)

    def odd_mixer(self, s, l):
        kk, S, NT, NG = self.kk, self.S, self.NT, self.NG
        o = l // 2
        NCK = NT * 4
        with Phase(kk) as ph:
            gain, gr = self.load_gain_bc(ph, "og", self.norm_mix[l], D)
            go, gor = self.load_gain_bc(ph, "ogo", self.hgrn_o_norm[o], 128)
            hT = ph.sb("ohT", [128, NCH, S], BF16)
            hTr = ph.rs(NT, "ohT")
            self.norm_T(ph, "on", [(self.X[:, i, :], self.XR[i]) for i in range(NT)], gain, gr, hT, hTr)
            rm = ph.sb("orm", [128, 512], BF16)
            rmr = ph.r("orm")
            kk.op("dve", lambda e: e.memset(rm[:], 1.0), writes=[rmr])
            kk.op("dve", lambda e: e.memset(rm[:, 0:512:32], 0.0), writes=[rmr])
            win = [ph.sb("owin%d" % j, [128, NCH, 512], BF16) for j in range(2)]
            winr = ph.rs(2, "owin")
            wout = [ph.sb("owout%d" % j, [128, D], BF16) for j in range(2)]
            woutr = ph.rs(2, "owout")
            qt = ph.sb("oqt", [128, S], BF16)
            qtr = ph.rs(NG, "oqt")
            kt = ph.sb("okt", [128, S], BF16)
            ktr = ph.rs(NG, "okt")
            khT = ph.sb("okhT", [128, NT, 128], BF16)
            khTr = ph.rs(NT, "okhT")
            vtok = ph.sb("ovt", [128, NT, 128], BF16)
            vtr = ph.rs(NT, "ovt")
            sgt = ph.sb("osg", [128, NT, 128], BF16)
            sgr = ph.rs(NT, "osg")
            adec = ph.sb("oadec", [128, NCK], F32)
            adr = ph.rs(NG, "oadec")
            Sbf = ph.sb("oSbf", [128, NCK, 128], BF16)
            Sbfr = ph.rs(NCK, "oSbf")
            Sst = [ph.sb("oSst%d" % j, [128, 128], F32) for j in range(2)]
            Sstr = ph.rs(2, "oSst")
            T1 = [[ph.sb("ot%d_%d" % (a, j), [128, 512], F32) for j in range(2)] for a in range(6)]
            T1r = [ph.rs(2, "ot%d_" % a) for a in range(6)]
            kh = [ph.sb("okh%d" % j, [128, 512], BF16) for j in range(2)]
            khr = ph.rs(2, "okh")
            vm = [ph.sb("ovm%d" % j, [128, 4, 128], BF16) for j in range(2)]
            vmr = ph.rs(2, "ovm")
            qm = [ph.sb("oqm%d" % j, [128, 4, 128], BF16) for j in range(2)]
            qmr = ph.rs(2, "oqm")
            for j in range(2):
                kk.op("dve", lambda e, j=j: e.memset(qm[j][:], 0.0), writes=[qmr[j]])
            scm = [ph.sb("oscm%d" % j, [128, 128], BF16) for j in range(2)]
            scmr = ph.rs(2, "oscm")
            yf = [ph.sb("oyf%d" % j, [128, 128], F32) for j in range(2)]
            yfr = ph.rs(2, "oyf")
            yb = [ph.sb("oyb%d" % j, [128, 128], BF16) for j in range(2)]
            ybr = ph.rs(2, "oyb")
            yT = [ph.sb("oyT%d" % j, [128, 128], BF16) for j in range(2)]
            yTr = ph.rs(2, "oyT")
            oss = ph.sb("oss", [128, 2], F32)
            ossr = ph.rs(2, "oss")
            ojk = ph.sb("ojk", [128, 128], BF16)
            ojr = ph.r("ojk")
            w_in = self.hgrn_w_in[o]
            t1i = 0
            import os
            STG = int(os.environ.get("ODD_STAGE", "9"))
            for hd in range(int(os.environ.get("ODD_HEADS", "8"))):
                if STG < 1:
                    break
                w, wr = win[hd % 2], winr[hd % 2]
                for j in range(4):
                    kk.dma("pool", w[:, :, j * 128:(j + 1) * 128], wblk(w_in, 0, NCH, j * 1024 + hd * 128, 128), writes=[wr])
                wo_, wor_ = wout[hd % 2], woutr[hd % 2]
                kk.dma("pool", wo_[:], self.hgrn_w_out[o][hd * 128:(hd + 1) * 128, :], writes=[wor_])
                col = l * 8 + hd
                lb = self.lbt[:, col:col + 1]
                oml = self.omlt[:, col:col + 1]
                noml = self.nomlt[:, col:col + 1]
                for g in range(NG):
                    tsl = slice(g * 512, (g + 1) * 512)
                    bq = kk.bank()
                    bf = kk.bank()
                    for (bb, co) in ((bq, 0), (bf, 128)):
                        for kc in range(NCH):
                            kk.op("pe", lambda e, bb=bb, co=co, kc=kc, w=w, tsl=tsl: e.matmul(
                                self.P[:, bb, :], lhsT=w[:, kc, co:co + 128], rhs=hT[:, kc, tsl],
                                start=(kc == 0), stop=(kc == NCH - 1)),
                                reads=[wr] + hTr[g * 4:(g + 1) * 4], writes=[self.PB[bb]])
                    p = t1i % 2
                    t1i += 1
                    sig, lf, kf, bb_, eb, enb = [T1[a][p] for a in range(6)]
                    sigr, lfr, kfr, bbr, ebr, enbr = [T1r[a][p] for a in range(6)]
                    kk.op("act", lambda e, sig=sig, bf=bf: e.activation(out=sig[:], in_=self.P[:, bf, :], func=AF.Sigmoid),
                          reads=[self.PB[bf]], writes=[sigr])
                    kk.op("act", lambda e, sig=sig, lf=lf: e.activation(out=lf[:], in_=sig[:], func=AF.Ln, bias=lb, scale=oml),
                          reads=[sigr, self.LBR], writes=[lfr])
                    kk.op("dve", lambda e, sig=sig, kf=kf: e.tensor_scalar(out=kf[:], in0=sig[:], scalar1=noml, scalar2=oml,
                                                                         op0=ALU.mult, op1=ALU.add),
                          reads=[sigr, self.LBR], writes=[kfr])
                    kk.op("dve", lambda e, lf=lf, bb_=bb_: e.tensor_tensor_scan(out=bb_[:], data0=rm[:], data1=lf[:], initial=0.0,
                                                                               op0=ALU.mult, op1=ALU.add),
                          reads=[lfr, rmr], writes=[bbr])
                    kk.op("act", lambda e, bb_=bb_, eb=eb: e.activation(out=eb[:], in_=bb_[:], func=AF.Exp), reads=[bbr], writes=[ebr])
                    kk.op("act", lambda e, bb_=bb_, enb=enb: e.activation(out=enb[:], in_=bb_[:], func=AF.Exp, scale=-1.0),
                          reads=[bbr], writes=[enbr])
                    kk.op("dve", lambda e, eb=eb, bq=bq, tsl=tsl: e.tensor_tensor(out=qt[:, tsl], in0=self.P[:, bq, :], in1=eb[:], op=ALU.mult),
                          reads=[self.PB[bq], ebr], writes=[qtr[g]])
                    kk.op("dve", lambda e, kf=kf, enb=enb, tsl=tsl: e.tensor_tensor(out=kt[:, tsl], in0=kf[:], in1=enb[:], op=ALU.mult),
                          reads=[kfr, enbr], writes=[ktr[g]])
                    kh_, khr_ = kh[p], khr[p]
                    for cc in range(16):
                        kk.op("dve", lambda e, eb=eb, kh_=kh_, cc=cc, g=g: e.tensor_scalar(
                            out=kh_[:, cc * 32:(cc + 1) * 32], in0=kt[:, g * 512 + cc * 32:g * 512 + (cc + 1) * 32],
                            scalar1=eb[:, cc * 32 + 31:cc * 32 + 32], scalar2=None, op0=ALU.mult),
                            reads=[ktr[g], ebr], writes=[khr_])
                    kk.op("act", lambda e, eb=eb, g=g: e.copy(out=adec[:, g * 16:(g + 1) * 16], in_=eb[:, 31:512:32]),
                          reads=[ebr], writes=[adr[g]])
                    b = kk.bank()
                    pv = self.pbb(b)
                    for j in range(4):
                        kk.op("pe", lambda e, j=j, pv=pv, kh_=kh_: e.transpose(out=pv[:, j * 128:(j + 1) * 128],
                                                                            in_=kh_[:, j * 128:(j + 1) * 128], identity=self.identb[:]),
                              reads=[khr_, self.CR2], writes=[self.PB[b]])
                    kk.op("act", lambda e, pv=pv, g=g: e.copy(out=khT[:, g * 4:(g + 1) * 4, :],
                                                              in_=pv[:, 0:512].rearrange("p (c t) -> p c t", c=4)),
                          reads=[self.PB[b]], writes=khTr[g * 4:(g + 1) * 4])
                if STG < 2:
                    continue
                for i in range(NT):
                    b = kk.bank()
                    for kc in range(NCH):
                        kk.op("pe", lambda e, kc=kc, b=b, i=i, w=w: e.matmul(
                            self.P[:, b, 0:256], lhsT=hT[:, kc, i * 128:(i + 1) * 128], rhs=w[:, kc, 256:512],
                            start=(kc == 0), stop=(kc == NCH - 1)),
                            reads=[wr, hTr[i]], writes=[self.PB[b]])
                    kk.op("act", lambda e, b=b, i=i: e.copy(out=vtok[:, i, :], in_=self.P[:, b, 0:128]),
                          reads=[self.PB[b]], writes=[vtr[i]])
                    kk.op("act", lambda e, b=b, i=i: e.activation(out=sgt[:, i, :], in_=self.P[:, b, 128:256], func=AF.Silu),
                          reads=[self.PB[b]], writes=[sgr[i]])
                if STG < 3:
                    continue
                kk.op("dve", lambda e: e.memset(Sst[0][:], 0.0), writes=[Sstr[0]])
                kk.op("dve", lambda e: e.memset(Sbf[:, 0, :], 0.0), writes=[Sbfr[0]])
                for cj in range(NCK - 1):
                    i, j = divmod(cj, 4)
                    if j == 0:
                        vm_, vm_r = vm[i % 2], vmr[i % 2]
                        for jj in range(4):
                            kk.op("dve", lambda e, i=i, vm_=vm_, jj=jj: e.tensor_scalar(
                                out=vm_[:, jj, :], in0=vtok[:, i, :], scalar1=self.rmask4[:, jj * 128:jj * 128 + 1], scalar2=None, op0=ALU.mult),
                                reads=[vtr[i], self.CR2], writes=[vm_r])
                    bd = kk.bank()
                    kk.op("pe", lambda e, bd=bd, i=i, j=j, vm_=vm_: e.matmul(
                        self.P[:, bd, 0:128], lhsT=khT[:, i, :], rhs=vm_[:, j, :], start=True, stop=True),
                        reads=[khTr[i], vm_r], writes=[self.PB[bd]])
                    kk.op("dve", lambda e, bd=bd, j=j, cj=cj: e.scalar_tensor_tensor(
                        out=Sst[(cj + 1) % 2][:], in0=Sst[cj % 2][:], scalar=adec[:, cj:cj + 1], in1=self.P[:, bd, 0:128],
                        op0=ALU.mult, op1=ALU.add),
                        reads=[Sstr[cj % 2], adr[cj // 16], self.PB[bd]], writes=[Sstr[(cj + 1) % 2]])
                    kk.op("act", lambda e, cj=cj: e.copy(out=Sbf[:, cj + 1, :], in_=Sst[(cj + 1) % 2][:]), reads=[Sstr[(cj + 1) % 2]], writes=[Sbfr[cj + 1]])
                if STG < 4:
                    continue
                bobox = {}

                def odd_X(i):
                    p = i % 2
                    bs = kk.bank()
                    kk.op("pe", lambda e, bs=bs, i=i: e.matmul(self.P[:, bs, 0:128], lhsT=kt[:, i * 128:(i + 1) * 128],
                                                              rhs=qt[:, i * 128:(i + 1) * 128], start=True, stop=True),
                          reads=[ktr[i // 4], qtr[i // 4]], writes=[self.PB[bs]])
                    kk.op("dve", lambda e, bs=bs, p=p: e.tensor_tensor(out=scm[p][:], in0=self.P[:, bs, 0:128], in1=self.hmask[:], op=ALU.mult),
                          reads=[self.PB[bs], self.CR], writes=[scmr[p]])
                    bo = kk.bank()
                    kk.op("pe", lambda e, bo=bo, i=i, p=p: e.matmul(self.P[:, bo, 0:128], lhsT=scm[p][:], rhs=vtok[:, i, :],
                                                                 start=True, stop=False),
                          reads=[scmr[p], vtr[i]], writes=[self.PB[bo]])
                    qm_, qm_r = qm[p], qmr[p]
                    for jj in range(4):
                        kk.op("dve", lambda e, i=i, qm_=qm_, jj=jj: e.tensor_copy(
                            out=qm_[:, jj, jj * 32:(jj + 1) * 32], in_=qt[:, i * 128 + jj * 32:i * 128 + (jj + 1) * 32]),
                            reads=[qtr[i // 4]], writes=[qm_r])
                    for j in range(4):
                        kk.op("pe", lambda e, bo=bo, i=i, j=j, qm_=qm_: e.matmul(
                            self.P[:, bo, 0:128], lhsT=qm_[:, j, :], rhs=Sbf[:, 4 * i + j, :], start=False, stop=(j == 3)),
                            reads=[qm_r, Sbfr[4 * i + j]], writes=[self.PB[bo]])
                    bobox[i] = bo
                    kk.reserved.add(bo)

                def odd_Y(i, wo_=wo_, wor_=wor_):
                    p = i % 2
                    bo = bobox.pop(i)
                    kk.op("act", lambda e, bo=bo, p=p: e.activation(out=ojk[:], in_=self.P[:, bo, 0:128], func=AF.Square,
                                                                    accum_out=oss[:, p:p + 1]),
                          reads=[self.PB[bo]], writes=[ojr, ossr[p]])
                    self.rstd_from_ss(oss[:, p:p + 1], oss[:, p:p + 1], 128, ossr[p])
                    kk.op("dve", lambda e, bo=bo, p=p: e.scalar_tensor_tensor(out=yf[p][:], in0=self.P[:, bo, 0:128], scalar=oss[:, p:p + 1],
                                                                             in1=go[:], op0=ALU.mult, op1=ALU.mult),
                          reads=[self.PB[bo], ossr[p], gor], writes=[yfr[p]])
                    kk.reserved.discard(bo)
                    kk.op("dve", lambda e, p=p, i=i: e.tensor_tensor(out=yb[p][:], in0=yf[p][:], in1=sgt[:, i, :], op=ALU.mult),
                          reads=[yfr[p], sgr[i]], writes=[ybr[p]])
                    bt = kk.bank()
                    pv = self.pbb(bt)
                    kk.op("pe", lambda e, pv=pv, p=p: e.transpose(out=pv[:, 0:128], in_=yb[p][:], identity=self.identb[:]),
                          reads=[ybr[p], self.CR2], writes=[self.PB[bt]])
                    kk.op("act", lambda e, pv=pv, p=p: e.copy(out=yT[p][:], in_=pv[:, 0:128]), reads=[self.PB[bt]], writes=[yTr[p]])
                    for hf in range(2):
                        b = kk.bank()
                        kk.op("pe", lambda e, b=b, p=p, hf=hf, wo_=wo_: e.matmul(self.P[:, b, :], lhsT=yT[p][:], rhs=wo_[:, hf * 512:(hf + 1) * 512],
                                                                              start=True, stop=True),
                              reads=[yTr[p], wor_], writes=[self.PB[b]])
                        self.resid_add(i, b, hf)

                odd_X(0)
                for i in range(NT):
                    if i + 1 < NT:
                        odd_X(i + 1)
                    odd_Y(i)

    def rope_tables(self, ph, s):
        kk, NT = self.kk, self.NT
        n = NT * 32
        posi = ph.sb("posi", [128, NT], I32)
        pr = ph.r("pos")
        with self.nc.allow_non_contiguous_dma(reason="positions token-major gather"):
            kk.dma("sp", posi[:], self.pos_d[s].rearrange("(n p) -> p n", p=128), writes=[pr])
        posf = ph.sb("posf", [128, NT], F32)
        kk.op("dve", lambda e: e.tensor_copy(out=posf[:], in_=posi[:]), reads=[pr], writes=[pr])
        ang = ph.sb("ang", [128, NT, 32], F32)
        for t in range(NT):
            kk.op("dve", lambda e, t=t: e.tensor_scalar(out=ang[:, t, :], in0=self.invf[:], scalar1=posf[:, t:t + 1], scalar2=None,
                                                       op0=ALU.mult),
                  reads=[pr, self.CR], writes=[pr])
        a2 = ang[:].rearrange("p a b -> p (a b)")
        kf = ph.sb("ropek", [128, n], F32)
        r_ = ph.sb("roper", [128, n], F32)
        rc = ph.sb("roperc", [128, n], F32)
        m_ = ph.sb("ropem", [128, n], F32)
        kk.op("dve", lambda e: e.tensor_scalar(out=kf[:], in0=a2, scalar1=1.0 / TWO_PI, scalar2=MAGIC, op0=ALU.mult, op1=ALU.add),
              reads=[pr], writes=[pr])
        kk.op("dve", lambda e: e.tensor_scalar(out=kf[:], in0=kf[:], scalar1=-MAGIC, scalar2=None, op0=ALU.add), reads=[pr], writes=[pr])
        kk.op("dve", lambda e: e.scalar_tensor_tensor(out=r_[:], in0=kf[:], scalar=-CW1, in1=a2, op0=ALU.mult, op1=ALU.add),
              reads=[pr], writes=[pr])
        kk.op("dve", lambda e: e.scalar_tensor_tensor(out=r_[:], in0=kf[:], scalar=-CW2, in1=r_[:], op0=ALU.mult, op1=ALU.add),
              reads=[pr], writes=[pr])
        kk.op("dve", lambda e: e.tensor_scalar(out=rc[:], in0=r_[:], scalar1=0.5 * np.pi, scalar2=None, op0=ALU.add), reads=[pr], writes=[pr])
        kk.op("dve", lambda e: e.tensor_scalar(out=m_[:], in0=rc[:], scalar1=float(np.pi), scalar2=None, op0=ALU.is_gt), reads=[pr], writes=[pr])
        kk.op("dve", lambda e: e.scalar_tensor_tensor(out=rc[:], in0=m_[:], scalar=-TWO_PI, in1=rc[:], op0=ALU.mult, op1=ALU.add),
              reads=[pr], writes=[pr])
        for t_ in (r_, rc):
            kk.op("dve", lambda e, t_=t_: e.tensor_scalar(out=t_[:], in0=t_[:], scalar1=-PI_LO, scalar2=PI_LO, op0=ALU.max, op1=ALU.min),
                  reads=[pr], writes=[pr])
        cos3 = ph.sb("cos3", [128, NT, 3, 32], F32)
        sin3 = ph.sb("sin3", [128, NT, 3, 32], F32)
        tr = ph.r("ropetab")
        kk.op("act", lambda e: e.activation(out=sin3[:, :, 0, :], in_=r_[:].rearrange("p (a b) -> p a b", b=32), func=AF.Sin),
              reads=[pr], writes=[tr])
        kk.op("act", lambda e: e.activation(out=cos3[:, :, 0, :], in_=rc[:].rearrange("p (a b) -> p a b", b=32), func=AF.Sin),
              reads=[pr], writes=[tr])
        for j in (1, 2):
            kk.op("dve", lambda e, j=j: e.tensor_copy(out=sin3[:, :, j, :], in_=sin3[:, :, 0, :]), reads=[tr], writes=[tr])
            kk.op("dve", lambda e, j=j: e.tensor_copy(out=cos3[:, :, j, :], in_=cos3[:, :, 0, :]), reads=[tr], writes=[tr])
        return cos3, sin3, tr

    def outproj_partial(self, i, aT_ap, aT_r, wo_, wor_):
        kk = self.kk
        for hf in range(2):
            b = kk.bank()
            kk.op("pe", lambda e, b=b, hf=hf: e.matmul(self.P[:, b, :], lhsT=aT_ap, rhs=wo_[:, hf * 512:(hf + 1) * 512], start=True, stop=True),
                  reads=[aT_r, wor_], writes=[self.PB[b]])
            self.resid_add(i, b, hf)

    def even_mixer(self, s, l):
        kk, S, NT, NG = self.kk, self.S, self.NT, self.NG
        ei = l // 2
        w_in = self.ab_w_in[ei]
        w_out = self.ab_w_out[ei]
        with Phase(kk) as ph:
            hT = ph.sb("ehT", [128, NCH, S], BF16)
            hTr = ph.rs(NT, "ehT")
            with Phase(kk) as pn:
                gain, gr = self.load_gain_bc(pn, "eg", self.norm_mix[l], D)
                self.norm_T(pn, "en", [(self.X[:, i, :], self.XR[i]) for i in range(NT)], gain, gr, hT, hTr)
            wout = [ph.sb("ewout%d" % j, [128, D], BF16) for j in range(2)]
            woutr = ph.rs(2, "ewout")
            Oc = [ph.sb("eOc%d" % j, [128, 128], BF16) for j in range(2)]
            Ocr = ph.rs(2, "eOc")
            aT = [ph.sb("eaT%d" % j, [128, 128], BF16) for j in range(2)]
            aTr = ph.rs(2, "eaT")
            oi = 0
            with Phase(kk) as pa:
                cos3, sin3, tabr = self.rope_tables(pa, s)
                g3 = pa.sb("g3", [128, 3, 64], F32)
                g3r = pa.r("g3")
                kk.dma("sp", g3[:, 0, :], self.swa_q_norm[ei].partition_broadcast(128), writes=[g3r])
                kk.dma("sp", g3[:, 1, :], self.swa_q_norm[ei].partition_broadcast(128), writes=[g3r])
                kk.dma("sp", g3[:, 2, :], self.swa_k_norm[ei].partition_broadcast(128), writes=[g3r])
                esk = pa.sb("esk", [128, 8], F32)
                eskr = pa.r("esk")
                kk.dma("sp", esk[:], self.swa_sinks[ei].partition_broadcast(128), writes=[eskr])
                kk.op("act", lambda e: e.activation(out=esk[:], in_=esk[:], func=AF.Exp), reads=[eskr], writes=[eskr])
                win = [pa.sb("ewa%d" % j, [128, NCH, 256], BF16) for j in range(2)]
                winr = pa.rs(2, "ewa")
                raw = [pa.sb("eraw%d" % j, [128, 256], F32) for j in range(2)]
                rawr = pa.rs(2, "eraw")
                ss3 = [pa.sb("ess%d" % j, [128, 3], F32) for j in range(2)]
                ss3r = pa.rs(2, "ess")
                jk = pa.sb("ejk", [128, 64], BF16)
                jkr = pa.r("ejk")
                xn = [pa.sb("exn%d" % j, [128, 3, 64], F32) for j in range(2)]
                xnr = pa.rs(2, "exn")
                tt = [pa.sb("ett%d" % j, [128, 4, 3, 32], F32) for j in range(2)]
                ttr = pa.rs(2, "ett")
                rp = [pa.sb("erp%d" % j, [128, 3, 64], F32) for j in range(2)]
                rpr = pa.rs(2, "erp")
                stage = [pa.sb("est%d" % j, [128, 2, 128], BF16) for j in range(2)]
                stager = pa.rs(2, "est")
                vst = [pa.sb("evs%d" % j, [128, 65], BF16) for j in range(3)]
                vstr = pa.rs(3, "evs")
                for j in range(3):
                    kk.op("dve", lambda e, j=j: e.memset(vst[j][:, 64:65], 1.0), writes=[vstr[j]])
                qkT = [pa.sb("eqk%d" % j, [128, 2, 128], BF16) for j in range(3)]
                qkTr = pa.rs(3, "eqk")
                PT = [pa.sb("ePT%d" % j, [128, 512], BF16) for j in range(2)]
                PTr = pa.rs(2, "ePT")
                den = [pa.sb("eden%d" % j, [128, 2], F32) for j in range(2)]
                denr = pa.rs(2, "eden")
                for c in range(4):
                    g = c // 2
                    w, wr = win[c % 2], winr[c % 2]
                    kk.dma("pool", w[:, :, 0:128], wblk(w_in, 0, NCH, c * 128, 128), writes=[wr])
                    kk.dma("pool", w[:, :, 128:192], wblk(w_in, 0, NCH, 512 + g * 64, 64), writes=[wr])
                    kk.dma("pool", w[:, :, 192:256], wblk(w_in, 0, NCH, 640 + g * 64, 64), writes=[wr])
                    wo_, wor_ = wout[oi % 2], woutr[oi % 2]
                    kk.dma("pool", wo_[:], w_out[c * 128:(c + 1) * 128, :], writes=[wor_])
                    def swa_A(i, c=c, g=g, w=w, wr=wr):
                        p2, p3 = i % 2, i % 3
                        b = kk.bank()
                        for kc in range(NCH):
                            kk.op("pe", lambda e, kc=kc, b=b, i=i, w=w: e.matmul(
                                self.P[:, b, 0:256], lhsT=hT[:, kc, i * 128:(i + 1) * 128], rhs=w[:, kc, :],
                                start=(kc == 0), stop=(kc == NCH - 1)),
                                reads=[wr, hTr[i]], writes=[self.PB[b]])
                        rw, rwr = raw[p2], rawr[p2]
                        kk.op("act", lambda e, b=b, rw=rw: e.copy(out=rw[:], in_=self.P[:, b, 0:256]), reads=[self.PB[b]], writes=[rwr])
                        s3, s3r = ss3[p2], ss3r[p2]
                        for h in range(3):
                            kk.op("act", lambda e, h=h, rw=rw, s3=s3: e.activation(out=jk[:], in_=rw[:, h * 64:(h + 1) * 64], func=AF.Square,
                                                                              accum_out=s3[:, h:h + 1]),
                                  reads=[rwr], writes=[jkr, s3r])
                        self.rstd_from_ss(s3[:], s3[:], 64, s3r)
                        x_, x_r = xn[p2], xnr[p2]
                        for h in range(3):
                            kk.op("dve", lambda e, h=h, rw=rw, s3=s3, x_=x_: e.scalar_tensor_tensor(
                                out=x_[:, h, :], in0=rw[:, h * 64:(h + 1) * 64], scalar=s3[:, h:h + 1], in1=g3[:, h, :],
                                op0=ALU.mult, op1=ALU.mult),
                                reads=[rwr, s3r, g3r], writes=[x_r])
                        t_, t_r = tt[p2], ttr[p2]
                        r2, r2r = rp[p2], rpr[p2]
                        x1, x2 = x_[:, :, 0:32], x_[:, :, 32:64]
                        cs_, sn_ = cos3[:, i, :, :], sin3[:, i, :, :]
                        for k_, (a_, b_) in enumerate(((x1, cs_), (x2, sn_), (x2, cs_), (x1, sn_))):
                            kk.op("pool", lambda e, k_=k_, a_=a_, b_=b_, t_=t_: e.tensor_tensor(out=t_[:, k_, :, :], in0=a_, in1=b_, op=ALU.mult),
                                  reads=[x_r, tabr], writes=[t_r])
                        kk.op("pool", lambda e, t_=t_, r2=r2: e.tensor_tensor(out=r2[:, :, 0:32], in0=t_[:, 0, :, :], in1=t_[:, 1, :, :], op=ALU.subtract),
                              reads=[t_r], writes=[r2r])
                        kk.op("pool", lambda e, t_=t_, r2=r2: e.tensor_tensor(out=r2[:, :, 32:64], in0=t_[:, 2, :, :], in1=t_[:, 3, :, :], op=ALU.add),
                              reads=[t_r], writes=[r2r])
                        st_, st_r = stage[p2], stager[p2]
                        kk.op("act", lambda e, st_=st_, r2=r2: e.copy(out=st_[:, 0, :].rearrange("p (a b) -> p a b", a=2), in_=r2[:, 0:2, :]),
                              reads=[r2r], writes=[st_r])
                        kk.op("act", lambda e, st_=st_, r2=r2: e.copy(out=st_[:, 1, 0:64], in_=r2[:, 2, :]), reads=[r2r], writes=[st_r])
                        kk.op("act", lambda e, st_=st_, r2=r2: e.copy(out=st_[:, 1, 64:128], in_=r2[:, 2, :]), reads=[r2r], writes=[st_r])
                        vs, vsr = vst[p3], vstr[p3]
                        kk.op("dve", lambda e, vs=vs, rw=rw: e.tensor_copy(out=vs[:, 0:64], in_=rw[:, 192:256]), reads=[rwr], writes=[vsr])
                        bt = kk.bank()
                        pv = self.pbb(bt)
                        for j in range(2):
                            kk.op("pe", lambda e, j=j, pv=pv, st_=st_: e.transpose(out=pv[:, j * 128:(j + 1) * 128], in_=st_[:, j, :],
                                                                                identity=self.identb[:]),
                                  reads=[st_r, self.CR2], writes=[self.PB[bt]])
                        qk, qkr = qkT[p3], qkTr[p3]
                        kk.op("act", lambda e, pv=pv, qk=qk: e.copy(out=qk[:].rearrange("p a b -> p (a b)"), in_=pv[:, 0:256]),
                              reads=[self.PB[bt]], writes=[qkr])
                    def swa_B(i, c=c, wo_=wo_, wor_=wor_):
                        p2, p3 = i % 2, i % 3
                        qk, qkr = qkT[p3], qkTr[p3]
                        oi = oibox[0]
                        blks = [(1, i)] if i == 0 else [(0, i - 1), (1, i)]
                        bs = kk.bank(2)
                        lo = 128 if i == 0 else 0
                        for (blk, ti) in blks:
                            for hh in range(2):
                                kq, kqr = qkT[ti % 3], qkTr[ti % 3]
                                kk.op("pe", lambda e, blk=blk, hh=hh, kq=kq, qk=qk, bs=bs: e.matmul(
                                    self.P[:, bs + hh, blk * 128:(blk + 1) * 128],
                                    lhsT=kq[hh * 64:(hh + 1) * 64, 1, :], rhs=qk[hh * 64:(hh + 1) * 64, 0, :], start=True, stop=True),
                                    reads=[kqr, qkr], writes=[self.PB[bs + hh]])
                        pt, ptr = PT[p2], PTr[p2]
                        pt3 = pt[:].rearrange("p (a b) -> p a b", a=2)
                        mk3 = self.swamask[:].rearrange("p (a b) -> p a b", a=2)
                        kk.op("act", lambda e, pt3=pt3, bs=bs, lo=lo: e.activation(out=pt3[:, :, lo:256], in_=self.P[:, bs:bs + 2, lo:256], func=AF.Exp, scale=0.125),
                              reads=[self.PB[bs], self.PB[bs + 1]], writes=[ptr])
                        kk.op("dve", lambda e, pt3=pt3, mk3=mk3, lo=lo: e.tensor_tensor(out=pt3[:, :, lo:256], in0=pt3[:, :, lo:256], in1=mk3[:, :, lo:256], op=ALU.mult),
                              reads=[ptr, self.CR2], writes=[ptr])
                        bo = kk.bank()
                        for hh in range(2):
                            for bi, (blk, ti) in enumerate(blks):
                                kk.op("pe", lambda e, hh=hh, blk=blk, ti=ti, bi=bi, bo=bo, pt=pt: e.matmul(
                                    self.P[:, bo, hh * 65:(hh + 1) * 65], lhsT=pt[:, (hh * 2 + blk) * 128:(hh * 2 + blk + 1) * 128],
                                    rhs=vst[ti % 3][:, :], start=(bi == 0), stop=(bi == len(blks) - 1)),
                                    reads=[ptr, vstr[ti % 3]], writes=[self.PB[bo]])
                        dn, dnr = den[p2], denr[p2]
                        kk.op("dve", lambda e, dn=dn, bo=bo, c=c: e.tensor_tensor(out=dn[:], in0=self.P[:, bo, 64:130:65], in1=esk[:, 2 * c:2 * c + 2], op=ALU.add),
                              reads=[self.PB[bo], eskr], writes=[dnr])
                        kk.op("dve", lambda e, dn=dn: e.reciprocal(out=dn[:], in_=dn[:]), reads=[dnr], writes=[dnr])
                        oc, ocr = Oc[oi % 2], Ocr[oi % 2]
                        for hh in range(2):
                            kk.op("dve", lambda e, hh=hh, dn=dn, bo=bo, oc=oc: e.tensor_scalar(
                                out=oc[:, hh * 64:(hh + 1) * 64], in0=self.P[:, bo, hh * 65:hh * 65 + 64], scalar1=dn[:, hh:hh + 1], scalar2=None,
                                op0=ALU.mult),
                                reads=[self.PB[bo], dnr], writes=[ocr])
                        bt2 = kk.bank()
                        pv2 = self.pbb(bt2)
                        kk.op("pe", lambda e, pv2=pv2, oc=oc: e.transpose(out=pv2[:, 0:128], in_=oc[:], identity=self.identb[:]),
                              reads=[ocr, self.CR2], writes=[self.PB[bt2]])
                        at, atr = aT[oi % 2], aTr[oi % 2]
                        kk.op("act", lambda e, pv2=pv2, at=at: e.copy(out=at[:], in_=pv2[:, 0:128]), reads=[self.PB[bt2]], writes=[atr])
                        self.outproj_partial(i, at[:], atr, wo_, wor_)
                        oibox[0] = oi + 1

                    oibox = [oi]
                    swa_A(0)
                    for i in range(NT):
                        if i + 1 < NT:
                            swa_A(i + 1)
                        swa_B(i)
                    oi = oibox[0]
            with Phase(kk) as pb_:
                win = [pb_.sb("ewb%d" % j, [128, NCH, 384], BF16) for j in range(2)]
                winr = pb_.rs(2, "ewb")
                qbT = [pb_.sb("eqb%d" % j, [128, S], BF16) for j in range(2)]
                qbTr = [pb_.rs(NG, "eqb%d_" % j) for j in range(2)]
                kbT = [pb_.sb("ekb%d" % j, [128, S], BF16) for j in range(2)]
                kbTr = [pb_.rs(NG, "ekb%d_" % j) for j in range(2)]
                vb = [pb_.sb("evb%d" % j, [128, NT, 128], BF16) for j in range(2)]
                vbr = [pb_.rs(NT, "evb%d_" % j) for j in range(2)]
                ones = pb_.sb("eones", [128, S], BF16)
                onesr = pb_.r("eones")
                kk.op("dve", lambda e: e.memset(ones[:], 1.0), writes=[onesr])
                E = [pb_.sb("eE%d" % j, [128, S], F32) for j in range(2)]
                Er = pb_.rs(2, "eE")
                SPb = [pb_.sb("eSP%d" % j, [128, S], F32) for j in range(2)]
                SPr = pb_.rs(2, "eSP")
                CS = pb_.sb("eCS", [128, S + 1], F32)
                CSr = pb_.r("eCS")
                kk.op("dve", lambda e: e.memset(CS[:, 0:1], 0.0), writes=[CSr])
                ntot = pb_.sb("ent", [128, 1], F32)
                ntr = pb_.r("ent")
                Ab = [pb_.sb("eA%d" % j, [128, S], BF16) for j in range(2)]
                Abr = pb_.rs(2, "eA")
                AT = [pb_.sb("eAT%d" % j, [128, NT, 128], BF16) for j in range(1)]
                ATr = pb_.rs(1, "eAT")
                for c in range(4):
                    w, wr = win[c % 2], winr[c % 2]
                    kk.dma("pool", w[:, :, 0:128], wblk(w_in, 0, NCH, 768 + c * 128, 128), writes=[wr])
                    kk.dma("pool", w[:, :, 128:256], wblk(w_in, 0, NCH, 1280 + c * 128, 128), writes=[wr])
                    kk.dma("pool", w[:, :, 256:384], wblk(w_in, 0, NCH, 1792 + c * 128, 128), writes=[wr])
                    wo_, wor_ = wout[oi % 2], woutr[oi % 2]
                    kk.dma("pool", wo_[:], w_out[512 + c * 128:512 + (c + 1) * 128, :], writes=[wor_])
                    q_, q_r = qbT[c % 2], qbTr[c % 2]
                    k_, k_r = kbT[c % 2], kbTr[c % 2]
                    v_, v_r = vb[c % 2], vbr[c % 2]
                    for g in range(NG):
                        tsl = slice(g * 512, (g + 1) * 512)
                        for (dst, dstr, co) in ((q_, q_r, 0), (k_, k_r, 128)):
                            bb = kk.bank()
                            for kc in range(NCH):
                                kk.op("pe", lambda e, bb=bb, co=co, kc=kc, w=w, tsl=tsl: e.matmul(
                                    self.P[:, bb, :], lhsT=w[:, kc, co:co + 128], rhs=hT[:, kc, tsl],
                                    start=(kc == 0), stop=(kc == NCH - 1)),
                                    reads=[wr] + hTr[g * 4:(g + 1) * 4], writes=[self.PB[bb]])
                            kk.op("act", lambda e, bb=bb, dst=dst, tsl=tsl: e.copy(out=dst[:, tsl], in_=self.P[:, bb, :]),
                                  reads=[self.PB[bb]], writes=[dstr[g]])
                    for i in range(NT):
                        if i % 4 == 0:
                            bv = kk.bank()
                        for kc in range(NCH):
                            kk.op("pe", lambda e, kc=kc, bv=bv, i=i, w=w: e.matmul(
                                self.P[:, bv, (i % 4) * 128:(i % 4 + 1) * 128], lhsT=hT[:, kc, i * 128:(i + 1) * 128], rhs=w[:, kc, 256:384],
                                start=(kc == 0), stop=(kc == NCH - 1)),
                                reads=[wr, hTr[i]], writes=[self.PB[bv]])
                        kk.op("dve", lambda e, bv=bv, i=i, v_=v_: e.tensor_copy(out=v_[:, i, :], in_=self.P[:, bv, (i % 4) * 128:(i % 4 + 1) * 128]),
                              reads=[self.PB[bv]], writes=[v_r[i]])
                    its = [(n, hh) for n in range(NT) for hh in range(2)]
                    state = {}

                    def stA(k, c=c, q_=q_, q_r=q_r, k_=k_, k_r=k_r):
                        n, hh = its[k]
                        L = (n + 1) * 128
                        nb = 1 if L <= 512 else (2 if L <= 1024 else 4)
                        ps_ = slice(hh * 64, (hh + 1) * 64)
                        bz = kk.bank(nb)
                        Z = self.pb(bz, nb)
                        zr = [self.PB[bz + j] for j in range(nb)]
                        nj = (L + 511) // 512
                        for j in range(nj):
                            c0, c1 = j * 512, min(L, (j + 1) * 512)
                            last = (j == nj - 1)
                            kk.op("pe", lambda e, c0=c0, c1=c1, last=last: e.matmul(
                                Z[:, c0:c1], lhsT=q_[ps_, n * 128:(n + 1) * 128], rhs=k_[ps_, c0:c1], start=True, stop=not last),
                                reads=[q_r[n // 4], k_r[j]], writes=[self.PB[bz + j]])
                        kk.op("pe", lambda e: e.matmul(Z[:, L - 128:L], lhsT=self.identb[:], rhs=self.sbneg[:], start=False, stop=True),
                              reads=[self.CR2], writes=[self.PB[bz + nj - 1]])
                        gi = state.setdefault("gi", 0)
                        state["gi"] = gi + 1
                        e_, e_r = E[gi % 2], Er[gi % 2]
                        sp_, sp_r = SPb[gi % 2], SPr[gi % 2]
                        ab, abr = Ab[gi % 2], Abr[gi % 2]
                        state[k] = (L, e_, e_r, sp_, sp_r, ab, abr)
                        kk.op("act", lambda e: e.activation(out=e_[:, 0:L], in_=Z[:, 0:L], func=AF.Exp, scale=0.125),
                              reads=zr[0:nj], writes=[e_r])
                        kk.op("act", lambda e: e.activation(out=sp_[:, 0:L], in_=e_[:, 0:L], func=AF.Ln, bias=1.0, scale=1.0),
                              reads=[e_r], writes=[sp_r])

                    def stB(k):
                        L, e_, e_r, sp_, sp_r, ab, abr = state[k]
                        kk.op("dve", lambda e: e.tensor_tensor_scan(out=CS[:, 1:L + 1], data0=ones[:, 0:L], data1=sp_[:, 0:L], initial=0.0,
                                                                     op0=ALU.mult, op1=ALU.add),
                              reads=[sp_r, onesr], writes=[CSr])
                        kk.op("dve", lambda e: e.tensor_scalar(out=ntot[:], in0=CS[:, L:L + 1], scalar1=-1.0, scalar2=None, op0=ALU.mult),
                              reads=[CSr], writes=[ntr])
                        kk.op("act", lambda e: e.activation(out=sp_[:, 0:L], in_=CS[:, 0:L], func=AF.Exp, bias=ntot[:, 0:1], scale=1.0),
                              reads=[CSr, ntr], writes=[sp_r])
                        kk.op("pool", lambda e: e.tensor_tensor(out=ab[:, 0:L], in0=e_[:, 0:L], in1=sp_[:, 0:L], op=ALU.mult),
                              reads=[e_r, sp_r], writes=[abr])

                    def stC(k, c=c, v_=v_, v_r=v_r, wo_=wo_, wor_=wor_):
                        n, hh = its[k]
                        L, e_, e_r, sp_, sp_r, ab, abr = state.pop(k)
                        if hh == 0:
                            state["bo"] = kk.bank()
                            kk.reserved.add(state["bo"])
                        bo = state["bo"]
                        at_, at_r = AT[0], ATr[0]
                        for k0 in range(0, n + 1, 8):
                            k1 = min(n + 1, k0 + 8)
                            bt = kk.bank()
                            while bt == bo:
                                bt = kk.bank()
                            pv = self.pbb(bt)
                            for kb in range(k0, k1):
                                kk.op("pe", lambda e, kb=kb, k0=k0, pv=pv: e.transpose(
                                    out=pv[:, (kb - k0) * 128:(kb - k0 + 1) * 128], in_=ab[:, kb * 128:(kb + 1) * 128], identity=self.identb[:]),
                                    reads=[abr, self.CR2], writes=[self.PB[bt]])
                            kk.op("act", lambda e, k0=k0, k1=k1, pv=pv: e.copy(
                                out=at_[:, k0:k1, :], in_=pv[:, 0:(k1 - k0) * 128].rearrange("p (a b) -> p a b", b=128)),
                                reads=[self.PB[bt]], writes=[at_r])
                        for kb in range(n + 1):
                            kk.op("pe", lambda e, kb=kb: e.matmul(
                                self.P[:, bo, hh * 64:(hh + 1) * 64], lhsT=at_[:, kb, :], rhs=v_[:, kb, hh * 64:(hh + 1) * 64],
                                start=(kb == 0), stop=(kb == n)),
                                reads=[at_r, v_r[kb]], writes=[self.PB[bo]])
                        if hh == 1:
                            oi = state["oi"]
                            oc, ocr = Oc[oi % 2], Ocr[oi % 2]
                            kk.op("dve", lambda e: e.tensor_copy(out=oc[:], in_=self.P[:, bo, 0:128]), reads=[self.PB[bo]], writes=[ocr])
                            kk.reserved.discard(bo)
                            bt2 = kk.bank()
                            pv2 = self.pbb(bt2)
                            kk.op("pe", lambda e: e.transpose(out=pv2[:, 0:128], in_=oc[:], identity=self.identb[:]),
                                  reads=[ocr, self.CR2], writes=[self.PB[bt2]])
                            at, atr = aT[oi % 2], aTr[oi % 2]
                            kk.op("act", lambda e: e.copy(out=at[:], in_=pv2[:, 0:128]), reads=[self.PB[bt2]], writes=[atr])
                            self.outproj_partial(n, at[:], atr, wo_, wor_)
                            state["oi"] = oi + 1

                    state["oi"] = oi
                    NI = len(its)
                    for t in range(NI + 2):
                        if 0 <= t - 2 < NI:
                            stC(t - 2)
                        if 0 <= t - 1 < NI:
                            stB(t - 1)
                        if t < NI:
                            stA(t)
                    oi = state["oi"]


def host_consts():
    p = np.arange(128)[:, None]
    i = np.arange(128)[None, :]
    ident = np.eye(128, dtype=np.float32)
    mprev = (i < p).astype(np.float32)
    mcur = (i >= p).astype(np.float32)
    swamask = np.stack([mprev, mcur, mprev, mcur], axis=1).reshape(128, 512).astype(np.float32)
    sbmask = (p > i).astype(np.float32)
    hmask = ((i >= p) & ((i // 32) == (p // 32))).astype(np.float32)
    invf = (10000.0 ** (-np.arange(0, 64, 2, dtype=np.float32) / np.float32(64))).astype(np.float32)
    invf = np.broadcast_to(invf[None, :], (128, 32)).copy()
    j4 = np.arange(4)[None, :, None]
    t4 = np.arange(128)[None, None, :]
    p4 = np.arange(128)[:, None, None]
    cmask4 = np.broadcast_to((t4 // 32) == j4, (128, 4, 128)).astype(np.float32).reshape(128, 512)
    rmask4 = np.broadcast_to((p4 // 32) == j4, (128, 4, 128)).astype(np.float32).reshape(128, 512)
    return {"c_ident": ident, "c_swamask": swamask, "c_sbmask": sbmask, "c_hmask": hmask, "c_invf": invf,
            "c_cmask4": np.ascontiguousarray(cmask4), "c_rmask4": np.ascontiguousarray(rmask4),
            "c_sbneg": np.ascontiguousarray((1.0 - sbmask) * np.float32(-262144.0))}


_PROG = {}


def kernel(**inputs):
    n = 8
    key = "full"
    if key not in _PROG:
        _PROG[key] = Prog()
    prog = _PROG[key]
    consts = host_consts()
    per = 32 // n
    in_maps = []
    for c in range(n):
        m = {}
        for k, v in inputs.items():
            v = np.asarray(v)
            if k in ("x", "mem", "positions"):
                m[k] = np.ascontiguousarray(v[c * per:(c + 1) * per])
            else:
                m[k] = np.ascontiguousarray(v)
        m.update(consts)
        in_maps.append(m)
    res = run_bass_kernel_spmd(prog.nc, in_maps, core_ids=list(range(n)))
    return np.concatenate([r["out"] for r in res.results], axis=0).astype(np.float32)
```
